# Optimizing a Trainium2 kernel written in Bass

```python
import math
import jax, jax.numpy as jnp
from jax import lax
import numpy as np

D_MODEL = 1024
BATCH = 4
SEQ = 4096
DEPTH = 2

GRID_W = 64
CTX_LEN = 256
EPS = 1e-6
N_HEADS = 8
Q_LORA = 384
KV_LORA = 256
QK_NOPE = 64
QK_ROPE = 32
V_HEAD = 64
ROPE_BASE = 10000.0
Q_BLOCK = 128
ATTN_SCALE = (QK_NOPE + QK_ROPE) ** -0.5
HY_WIDTH = 512
HY_EMB = 17
HY_FILTER_HIDDEN = 64
HY_DECAY_MIN = -math.log(1e-2) / 1.5
HY_DECAY_MAX = -math.log(1e-2) / 0.3
D_FF = 2816
N_EXPERTS = 8
TOP_K = 2
N_DENSE = (DEPTH + 1) // 2
N_MOE = DEPTH // 2
KV_START = Q_LORA
HY_START = KV_START + KV_LORA + QK_ROPE
GATE_START = HY_START + 3 * HY_WIDTH
IN_COLS = GATE_START + 2 * D_MODEL

kernel_name = 'hybrid_mla_hyena_moe_dit_trunk'


def rmsnorm(x, g):
    xf = x.astype(jnp.float32)
    xf = xf * lax.rsqrt(jnp.mean(xf * xf, axis=-1, keepdims=True) + EPS)
    return xf.astype(x.dtype) * g


def adaln(cond, w, b, n_chunks):
    return jnp.split(jax.nn.silu(cond) @ w + b, n_chunks, axis=-1)


def axial_rope_tables(rows, dtype):
    n_freq = QK_ROPE // 4
    inv_freq = ROPE_BASE ** (-jnp.arange(n_freq, dtype=jnp.float32) / n_freq)
    r = jnp.repeat(jnp.arange(rows, dtype=jnp.float32), GRID_W)
    col = jnp.tile(jnp.arange(GRID_W, dtype=jnp.float32), rows)
    ang = jnp.concatenate([r[:, None] * inv_freq, col[:, None] * inv_freq], axis=-1)
    return jnp.cos(ang).astype(dtype), jnp.sin(ang).astype(dtype)


def apply_rope(v, cos, sin):
    half = QK_ROPE // 2
    a, b = v[..., :half], v[..., half:]
    return jnp.concatenate([a * cos - b * sin, b * cos + a * sin], axis=-1)


def mla_queries(p_q, q_norm_g, w_uq, rope):
    bsz, n, _ = p_q.shape
    q = (rmsnorm(p_q, q_norm_g) @ w_uq).reshape(bsz, n, N_HEADS, QK_NOPE + QK_ROPE)
    if rope is None:
        return q
    cos, sin = rope
    return jnp.concatenate([q[..., :QK_NOPE], apply_rope(q[..., QK_NOPE:], cos[:, None], sin[:, None])], axis=-1)


def mla_keys_values(p_kv, kv_norm_g, w_uk, w_uv, rope):
    bsz, n, _ = p_kv.shape
    c_kv = rmsnorm(p_kv[..., :KV_LORA], kv_norm_g)
    k_rope = p_kv[..., KV_LORA:]
    if rope is not None:
        k_rope = apply_rope(k_rope, *rope)
    k_nope = (c_kv @ w_uk).reshape(bsz, n, N_HEADS, QK_NOPE)
    v = (c_kv @ w_uv).reshape(bsz, n, N_HEADS, V_HEAD)
    k = jnp.concatenate([k_nope, jnp.broadcast_to(k_rope[:, :, None, :], (bsz, n, N_HEADS, QK_ROPE))], axis=-1)
    return k, v


def attend(q, k, v):
    s = jnp.einsum('bqhd,bkhd->bhqk', q, k).astype(jnp.float32) * ATTN_SCALE
    p = jax.nn.softmax(s, axis=-1).astype(v.dtype)
    return jnp.einsum('bhqk,bkhd->bqhd', p, v)


def attend_blocked(q, k, v):
    bsz, n, h, dk = q.shape
    qb = q.reshape(bsz, n // Q_BLOCK, Q_BLOCK, h, dk).transpose(1, 0, 2, 3, 4)
    o = lax.map(lambda qi: attend(qi, k, v), qb)
    return o.transpose(1, 0, 2, 3, 4).reshape(bsz, n, h * V_HEAD)


def hyena_filter(n, w1, b1, w2, b2, w3, freq, decay):
    f32 = jnp.float32
    w1, b1, w2, b2, w3, freq, decay = (a.astype(f32) for a in (w1, b1, w2, b2, w3, freq, decay))
    bands = (HY_EMB - 1) // 2
    t = jnp.linspace(0.0, 1.0, n, dtype=f32)[:, None]
    phase = (2.0 * math.pi / n) * jnp.arange(n, dtype=f32)[:, None] * jnp.linspace(1e-4, bands - 1, bands, dtype=f32)
    z = jnp.concatenate([t, jnp.cos(phase), -jnp.sin(phase)], axis=-1)
    h = jnp.sin(freq * (z @ w1 + b1))
    h = jnp.sin(freq * (h @ w2 + b2))
    h = (h @ w3).reshape(n, 2, HY_WIDTH) * jnp.exp(-t[:, :, None] * jnp.abs(decay))
    k = jnp.concatenate([h[:, 0], jnp.zeros((1, HY_WIDTH), f32), h[:0:-1, 1]], axis=0)
    return k / jnp.sum(jnp.abs(k), axis=0, keepdims=True)


def hyena(p, short_w, short_b, w1, b1, w2, b2, w3, freq, decay, bias):
    n = p.shape[1]
    pp = jnp.pad(p, ((0, 0), (1, 1), (0, 0)))
    u = pp[:, :-2] * short_w[0] + pp[:, 1:-1] * short_w[1] + pp[:, 2:] * short_w[2] + short_b
    v, x1, x0 = jnp.split(u, 3, axis=-1)
    z = v * x1
    k = hyena_filter(n, w1, b1, w2, b2, w3, freq, decay)
    zf = z.astype(jnp.float32)
    y = jnp.fft.irfft(jnp.fft.rfft(zf, n=2 * n, axis=1) * jnp.fft.rfft(k, axis=0)[None], n=2 * n, axis=1)[:, :n]
    y = (y + zf * bias.astype(jnp.float32)).astype(z.dtype)
    return y * x0


def token_mixer(p, k_all, v_all, rope, q_norm_g, w_uq, hy, w_br_attn, w_br_hy, w_out):
    bsz, n, _ = p.shape
    q = mla_queries(p[..., :Q_LORA], q_norm_g, w_uq, rope)
    if rope is None:
        o_attn = attend(q, k_all, v_all).reshape(bsz, n, N_HEADS * V_HEAD)
    else:
        o_attn = attend_blocked(q, k_all, v_all)
    o_hy = hyena(p[..., HY_START:GATE_START], *hy)
    g_attn, g_hy = jnp.split(jax.nn.sigmoid(p[..., GATE_START:]), 2, axis=-1)
    merged = g_attn * (o_attn @ w_br_attn) + g_hy * (o_hy @ w_br_hy)
    return merged @ w_out


def swiglu(h, w_gate, w_up, w_down):
    return (jax.nn.silu(h @ w_gate) * (h @ w_up)) @ w_down


def moe_swiglu(h, router, w_gate, w_up, w_down):
    bsz, n, d = h.shape
    t = h.reshape(-1, d)
    logits = (t @ router).astype(jnp.float32)
    top_val, top_idx = lax.top_k(logits, TOP_K)
    top_w = jax.nn.softmax(top_val, axis=-1)
    combine = jnp.sum(jax.nn.one_hot(top_idx, N_EXPERTS, dtype=jnp.float32) * top_w[..., None], axis=1).astype(h.dtype)
    out = jnp.zeros_like(t)
    for e in range(N_EXPERTS):
        out = out + combine[:, e:e + 1] * swiglu(t, w_gate[e], w_up[e], w_down[e])
    return out.reshape(bsz, n, d)


def channel_mixer(h, layer, ffn_w_gate, ffn_w_up, ffn_w_down, moe_router, moe_w_gate, moe_w_up, moe_w_down):
    i = layer // 2
    if layer % 2 == 0:
        return swiglu(h, ffn_w_gate[i], ffn_w_up[i], ffn_w_down[i])
    return moe_swiglu(h, moe_router[i], moe_w_gate[i], moe_w_up[i], moe_w_down[i])


def setup_inputs(seed: int = 0) -> dict:
    key = jax.random.key(seed)
    ks = iter(jax.random.split(key, 48))
    f32 = jnp.float32
    D, L = D_MODEL, DEPTH

    def nrm(shape, fan_in, gain=1.0):
        return jax.random.normal(next(ks), shape, f32) * (gain * fan_in ** -0.5)

    def gain_vec(shape):
        return 1.0 + 0.02 * jax.random.normal(next(ks), shape, f32)

    def small(shape, scale):
        return scale * jax.random.normal(next(ks), shape, f32)

    return {
        'x': jax.random.normal(next(ks), (BATCH, SEQ, D), f32),
        'c': jax.random.normal(next(ks), (BATCH, D), f32),
        'ctx': jax.random.normal(next(ks), (BATCH, CTX_LEN, D), f32),
        'c_ctx': jax.random.normal(next(ks), (D,), f32),
        'w_mod': nrm((L, D, 6 * D), D, 0.5),
        'b_mod': small((L, 6 * D), 0.02),
        'norm1_g': gain_vec((L, D)),
        'norm2_g': gain_vec((L, D)),
        'w_in': nrm((L, D, IN_COLS), D),
        'q_norm_g': gain_vec((L, Q_LORA)),
        'kv_norm_g': gain_vec((L, KV_LORA)),
        'w_uq': nrm((L, Q_LORA, N_HEADS * (QK_NOPE + QK_ROPE)), Q_LORA),
        'w_uk': nrm((L, KV_LORA, N_HEADS * QK_NOPE), KV_LORA),
        'w_uv': nrm((L, KV_LORA, N_HEADS * V_HEAD), KV_LORA),
        'hy_short_w': nrm((L, 3, 3 * HY_WIDTH), 3),
        'hy_short_b': small((L, 3 * HY_WIDTH), 0.02),
        'hy_w1': nrm((L, HY_EMB, HY_FILTER_HIDDEN), HY_EMB),
        'hy_b1': small((L, HY_FILTER_HIDDEN), 0.1),
        'hy_w2': nrm((L, HY_FILTER_HIDDEN, HY_FILTER_HIDDEN), HY_FILTER_HIDDEN),
        'hy_b2': small((L, HY_FILTER_HIDDEN), 0.1),
        'hy_w3': nrm((L, HY_FILTER_HIDDEN, 2 * HY_WIDTH), HY_FILTER_HIDDEN),
        'hy_freq': gain_vec((L, HY_FILTER_HIDDEN)),
        'hy_decay': jax.random.uniform(next(ks), (L, 2, HY_WIDTH), f32, minval=HY_DECAY_MIN, maxval=HY_DECAY_MAX),
        'hy_bias': jax.random.normal(next(ks), (L, HY_WIDTH), f32),
        'w_br_attn': nrm((L, N_HEADS * V_HEAD, D), N_HEADS * V_HEAD),
        'w_br_hy': nrm((L, HY_WIDTH, D), HY_WIDTH),
        'w_out': nrm((L, D, D), D),
        'ffn_w_gate': nrm((N_DENSE, D, D_FF), D),
        'ffn_w_up': nrm((N_DENSE, D, D_FF), D),
        'ffn_w_down': nrm((N_DENSE, D_FF, D), D_FF),
        'moe_router': nrm((N_MOE, D, N_EXPERTS), D),
        'moe_w_gate': nrm((N_MOE, N_EXPERTS, D, D_FF), D),
        'moe_w_up': nrm((N_MOE, N_EXPERTS, D, D_FF), D),
        'moe_w_down': nrm((N_MOE, N_EXPERTS, D_FF, D), D_FF),
        'final_g': gain_vec((D,)),
    }


def reference(x, c, ctx, c_ctx, w_mod, b_mod, norm1_g, norm2_g, w_in, q_norm_g, kv_norm_g, w_uq, w_uk, w_uv,
              hy_short_w, hy_short_b, hy_w1, hy_b1, hy_w2, hy_b2, hy_w3, hy_freq, hy_decay, hy_bias,
              w_br_attn, w_br_hy, w_out, ffn_w_gate, ffn_w_up, ffn_w_down,
              moe_router, moe_w_gate, moe_w_up, moe_w_down, final_g):
    rows = x.shape[1] // GRID_W
    rope = axial_rope_tables(rows, x.dtype)
    for layer in range(DEPTH):
        last = layer == DEPTH - 1
        sh1, sc1, g1, sh2, sc2, g2 = (m[:, None, :] for m in adaln(c, w_mod[layer], b_mod[layer], 6))
        hy = (hy_short_w[layer], hy_short_b[layer], hy_w1[layer], hy_b1[layer], hy_w2[layer], hy_b2[layer],
              hy_w3[layer], hy_freq[layer], hy_decay[layer], hy_bias[layer])

        h_lat = rmsnorm(x, norm1_g[layer]) * (1 + sc1) + sh1
        p_lat = h_lat @ w_in[layer]
        k_lat, v_lat = mla_keys_values(p_lat[..., KV_START:HY_START], kv_norm_g[layer], w_uk[layer], w_uv[layer], rope)

        if last:
            csh1, csc1 = adaln(c_ctx, w_mod[layer][:, :2 * D_MODEL], b_mod[layer][:2 * D_MODEL], 2)
            h_ctx = rmsnorm(ctx, norm1_g[layer]) * (1 + csc1) + csh1
            p_ctx_kv = h_ctx @ w_in[layer][:, KV_START:HY_START]
            k_ctx, v_ctx = mla_keys_values(p_ctx_kv, kv_norm_g[layer], w_uk[layer], w_uv[layer], None)
        else:
            csh1, csc1, cg1, csh2, csc2, cg2 = adaln(c_ctx, w_mod[layer], b_mod[layer], 6)
            h_ctx = rmsnorm(ctx, norm1_g[layer]) * (1 + csc1) + csh1
            p_ctx = h_ctx @ w_in[layer]
            k_ctx, v_ctx = mla_keys_values(p_ctx[..., KV_START:HY_START], kv_norm_g[layer], w_uk[layer], w_uv[layer], None)
            mix_ctx = token_mixer(p_ctx, k_ctx, v_ctx, None, q_norm_g[layer], w_uq[layer], hy,
                                  w_br_attn[layer], w_br_hy[layer], w_out[layer])

        k_all = jnp.concatenate([k_ctx, k_lat], axis=1)
        v_all = jnp.concatenate([v_ctx, v_lat], axis=1)
        mix_lat = token_mixer(p_lat, k_all, v_all, rope, q_norm_g[layer], w_uq[layer], hy,
                              w_br_attn[layer], w_br_hy[layer], w_out[layer])
        x = x + g1 * mix_lat
        x = x + g2 * channel_mixer(rmsnorm(x, norm2_g[layer]) * (1 + sc2) + sh2, layer,
                                   ffn_w_gate, ffn_w_up, ffn_w_down, moe_router, moe_w_gate, moe_w_up, moe_w_down)
        if not last:
            ctx = ctx + cg1 * mix_ctx
            ctx = ctx + cg2 * channel_mixer(rmsnorm(ctx, norm2_g[layer]) * (1 + csc2) + csh2, layer,
                                            ffn_w_gate, ffn_w_up, ffn_w_down, moe_router, moe_w_gate, moe_w_up, moe_w_down)
    return rmsnorm(x, final_g)
```

```python
from concourse.bass_utils import run_bass_kernel_spmd
import math
import numpy as np
from contextlib import ExitStack
import concourse.bass as bass
import concourse.mybir as mybir

F32 = mybir.dt.float32
BF16 = mybir.dt.bfloat16
AF = mybir.ActivationFunctionType
ALU = mybir.AluOpType
AX = mybir.AxisListType

import os
ENGS = ("pe", "act", "dve", "pool", "sp")
SKIP_SAME = set(os.environ.get("MK_SKIP_SAME", "").split(",")) - {""}
NDMA = 8

D = 1024
SEQ = 4096
CTX = 256
OWN = 2048
NH = 8
QL = 384
KVL = 256
ROPE = 32
HYW = 512
DFF = 2816
NE = 8
EPS = 1e-6
ATT_SCALE = (64 + 32) ** -0.5
TB = 512
ARENA_WORDS = 44500


class Res:
    __slots__ = ("name", "w", "r")

    def __init__(self, name="r"):
        self.name = name
        self.w = None
        self.r = {}


class Prog:
    def __init__(self, nc):
        self.nc = nc
        self.q = {k: [] for k in ENGS}
        self.cnt = {}
        self.known = {k: {} for k in ENGS}
        self.semnames = list(ENGS) + ["ccsem"]
        for q in ("sp", "pool", "act"):
            for i in range(NDMA):
                self.semnames.append(f"d_{q}_{i}")
        for s in self.semnames:
            self.cnt[s] = 0
        self.dma_rr = {"sp": 0, "pool": 0, "act": 0}
        self.sems = {}
        self.n_ops = 0

    def _need(self, eng, deps):
        best = {}
        for (s, t) in deps:
            if t > best.get(s, 0):
                best[s] = t
        for s, t in best.items():
            if eng == "pe" and s == "pe":
                continue
            if s == eng and eng in SKIP_SAME:
                continue
            if self.known[eng].get(s, 0) >= t:
                continue
            self.known[eng][s] = t
            self.q[eng].append(("wait", s, t))

    def _collect(self, reads, writes):
        deps = []
        assert not isinstance(reads, Res) and not isinstance(writes, Res)
        for r in reads:
            if r.w is not None:
                deps.append(r.w)
        for w in writes:
            if w.w is not None:
                deps.append(w.w)
            for s, t in w.r.items():
                deps.append((s, t))
        return deps

    def _mark(self, semkey, ticket, reads, writes):
        for r in reads:
            if r.r.get(semkey, 0) < ticket:
                r.r[semkey] = ticket
        for w in writes:
            w.w = (semkey, ticket)
            w.r = {}

    def op(self, eng, fn, reads=(), writes=(), inc=True):
        self.n_ops += 1
        self._need(eng, self._collect(reads, writes))
        ticket = self.cnt[eng] + 1
        if inc:
            self.cnt[eng] = ticket
        self.q[eng].append(("op", fn, inc))
        self._mark(eng, ticket, reads, writes)

    def dma(self, q, out, in_, reads=(), writes=(), **kw):
        self.n_ops += 1
        i = self.dma_rr[q]
        self.dma_rr[q] = (i + 1) % NDMA
        s = f"d_{q}_{i}"
        deps = self._collect(reads, writes)
        if self.cnt[s] > 0:
            deps.append((s, self.cnt[s]))
        self._need(q, deps)
        ticket = self.cnt[s] + 16
        self.cnt[s] = ticket
        self.q[q].append(("dma", out, in_, s, kw))
        self._mark(s, ticket, reads, writes)

    def cc(self, fn, reads=(), writes=()):
        q = "pool"
        s = "ccsem"
        deps = self._collect(reads, writes)
        if self.cnt[s] > 0:
            deps.append((s, self.cnt[s]))
        self._need(q, deps)
        ticket = self.cnt[s] + 1
        self.cnt[s] = ticket
        self.q[q].append(("cc", fn, s))
        self._mark(s, ticket, reads, writes)

    def barrier(self, with_cc=False):
        for e in ENGS:
            deps = [(s, c) for s, c in self.cnt.items() if c > 0 and (s != "ccsem" or with_cc)]
            self._need(e, deps)

    def emit(self, block):
        sems = self.sems

        def run(eng_name, engine):
            for item in self.q[eng_name]:
                if item[0] == "wait":
                    engine.wait_ge(sems[item[1]], item[2])
                elif item[0] == "op":
                    ins = item[1](engine)
                    if item[2]:
                        ins.then_inc(sems[eng_name], 1)
                elif item[0] == "cc":
                    item[1](engine).then_inc(sems[item[2]])
                else:
                    _, out, in_, s, kw = item
                    engine.dma_start(out=out, in_=in_, **kw).then_inc(sems[s], 16)

        @block.sync
        def _(e):
            run("sp", e)

        @block.scalar
        def _(e):
            run("act", e)

        @block.vector
        def _(e):
            run("dve", e)

        @block.gpsimd
        def _(e):
            run("pool", e)

        @block.tensor
        def _(e):
            run("pe", e)


class Arena:
    def __init__(self, t):
        self.t = t
        self.top = 0
        self.hi = 0

    def mark(self):
        return self.top

    def release(self, m):
        self.top = m

    def f32(self, words, shape=None):
        a = self.t[:, self.top:self.top + words]
        self.top += words
        self.hi = max(self.hi, self.top)
        assert self.top <= ARENA_WORDS, f"arena overflow {self.top}"
        return a

    def bf16(self, elems):
        words = (elems + 1) // 2
        a = self.t[:, self.top:self.top + words].bitcast(BF16)
        self.top += words
        self.hi = max(self.hi, self.top)
        assert self.top <= ARENA_WORDS, f"arena overflow {self.top}"
        return a


class Buf:
    def __init__(self, ap, name="b", nres=1):
        self.ap = ap
        self.res = [Res(f"{name}{i}") for i in range(nres)]

    @property
    def r(self):
        return self.res[0]


def _bind(f, *a, **k):
    return lambda e: f(e, *a, **k)


SHARED_INPUTS = {"ropeC", "ropeS", "zfT_f", "zfT_b", "t_f", "t_b", "CS1a", "CS1b", "W3r", "W3i", "W3n", "T1", "T2",
                 "ICr", "ICi", "zfT_c", "tc_bc", "cond", "xT", "cxT", "smask"}


class LayerK:
    def __init__(self, layer, taps=(), upto=None):
        self.layer = layer
        self.L0 = layer == 0
        self.last = layer == 1
        self.taps = set(taps)
        self.upto = upto
        self.nc = bass.Bass("TRN2", target_bir_lowering=False)
        self.es = ExitStack()
        self.in_shapes = {}
        self.out_shapes = {}
        nc = self.nc
        self.arena_t = self.es.enter_context(nc.sbuf_tensor("arena", [128, ARENA_WORDS], F32))
        self.A = Arena(self.arena_t)
        self.P = Prog(nc)
        for s in self.P.semnames:
            self.P.sems[s] = self.es.enter_context(nc.semaphore(s))
        self.ps = []
        for i in range(8):
            t = self.es.enter_context(nc.psum_tensor(f"ps{i}", [128, 512], F32))
            self.ps.append(Buf(t[:, :], f"ps{i}"))
        self.tapq = 0

    def din(self, name, shape, dt=F32):
        if not hasattr(self, "_in_aps"):
            self._in_aps = {}
        key = name if (name in SHARED_INPUTS or not getattr(self, "fused", False)) else f"L{self.layer}_{name}"
        if key in self._in_aps:
            assert self.in_shapes[key] == tuple(shape), (key, shape)
            self._in_aps[name] = self._in_aps[key]
            return self._in_aps[key]
        t = self.nc.dram_tensor(key, list(shape), dt, kind="ExternalInput").ap()
        self.in_shapes[key] = tuple(shape)
        self._in_aps[key] = t
        self._in_aps[name] = t
        return t

    def dout(self, name, shape, dt=F32):
        t = self.nc.dram_tensor(name, list(shape), dt, kind="ExternalOutput").ap()
        self.out_shapes[name] = tuple(shape)
        return t

    def tap(self, name, ap, res, dt=F32):
        if name not in self.taps:
            return
        shape = list(ap.shape)
        d = self.dout("tap_" + name, shape, dt)
        self.P.dma("sp", d, ap, reads=res)

    def mm(self, out, lhsT, rhs, start, stop, reads, writes, inc=None):
        if inc is None:
            inc = stop
        self.P.op("pe", lambda e: e.matmul(out, lhsT=lhsT, rhs=rhs, start=start, stop=stop),
                  reads, writes, inc)

    def tr(self, out, in_, ident, reads, writes):
        self.P.op("pe", lambda e: e.transpose(out, in_, ident), reads, writes, True)

    def act(self, out, in_, func, reads, writes, scale=1.0, bias=0.0):
        self.P.op("act", lambda e: e.activation(out=out, in_=in_, func=func, bias=bias, scale=scale),
                  reads, writes)

    def tt(self, eng, out, in0, in1, op, reads, writes):
        self.P.op(eng, lambda e: e.tensor_tensor(out=out, in0=in0, in1=in1, op=op), reads, writes)

    def ts(self, eng, out, in0, s1, s2, op0, op1, reads, writes):
        if op1 is None:
            self.P.op(eng, lambda e: e.tensor_scalar(out=out, in0=in0, scalar1=s1, scalar2=None, op0=op0),
                      reads, writes)
        else:
            self.P.op(eng, lambda e: e.tensor_scalar(out=out, in0=in0, scalar1=s1, scalar2=s2, op0=op0, op1=op1),
                      reads, writes)

    def stt(self, out, in0, scalar, in1, op0, op1, reads, writes):
        self.P.op("dve", lambda e: e.scalar_tensor_tensor(out=out, in0=in0, scalar=scalar, in1=in1, op0=op0, op1=op1),
                  reads, writes)

    def cp(self, eng, out, in_, reads, writes):
        if eng == "act":
            self.P.op("act", lambda e: e.copy(out=out, in_=in_), reads, writes)
        else:
            self.P.op(eng, lambda e: e.tensor_copy(out=out, in_=in_), reads, writes)

    def memset(self, eng, ap, val, writes):
        self.P.op(eng, lambda e: e.memset(ap, val), (), writes)

    def recip(self, out, in_, reads, writes):
        self.P.op("dve", lambda e: e.reciprocal(out=out, in_=in_), reads, writes)

    def load(self, q, out, in_, writes, **kw):
        self.P.dma(q, out, in_, writes=writes, **kw)

    def loadw_bf16(self, dst3, src2, kchunks, res):
        for k in range(kchunks):
            self.P.dma("pool", dst3[:, k, :], src2[k * 128:(k + 1) * 128, :], writes=[res])

    def setup_consts(self):
        A = self.A
        self.ones_f = Buf(A.f32(128), "ones")
        self.memset("pool", self.ones_f.ap, 1.0, [self.ones_f.r])
        self.ident_f = Buf(A.f32(128), "identf")
        self.memset("pool", self.ident_f.ap, 0.0, [self.ident_f.r])
        self.P.op("pool", lambda e: e.affine_select(out=self.ident_f.ap, in_=self.ident_f.ap, pattern=[[-1, 128]],
                                                    base=0, channel_multiplier=1, compare_op=ALU.not_equal, fill=1.0),
                  [self.ident_f.r], [self.ident_f.r])
        self.ident_b = Buf(A.bf16(128), "identb")
        self.cp("dve", self.ident_b.ap, self.ident_f.ap, [self.ident_f.r], [self.ident_b.r])

    def rms_bcast(self, src3, C, n, dim, rstd, sq3, src_res, rstd_res, sq_res, psb):
        self.act(sq3, src3, AF.Square, [src_res], [sq_res])
        ss = rstd
        self.P.op("dve", lambda e: e.tensor_reduce(out=ss, in_=sq3.rearrange("p c t -> p t c"), axis=AX.X, op=ALU.add),
                  [sq_res], [rstd_res])
        self.mm(psb.ap[:, 0:n], self.ones_f.ap, ss, True, True, [rstd_res, self.ones_f.r], [psb.r])
        self.act(rstd, psb.ap[:, 0:n], AF.Sqrt, [psb.r], [rstd_res], scale=1.0 / dim, bias=EPS)
        self.recip(rstd, rstd, [rstd_res], [rstd_res])

    def norm_mod(self, xb3, n, gs, sh, col, hT3, tmp3, rstd, xb_res, h_res, tmp_res, rstd_res, psb, hf3=None, hf_res=None):
        self.rms_bcast(xb3, 8, n, D, rstd, tmp3, xb_res, rstd_res, tmp_res, psb)
        for k in range(8):
            self.stt(tmp3[:, k, :], xb3[:, k, :], gs[:, k, col:col + 1], rstd, ALU.mult, ALU.mult,
                     [xb_res, rstd_res, self.small_res], [tmp_res])
        for k in range(8):
            self.act(hT3[:, k, :], tmp3[:, k, :], AF.Identity, [tmp_res, self.small_res], [h_res],
                     bias=sh[:, k, col:col + 1])
            if hf3 is not None:
                self.act(hf3[:, k, :], tmp3[:, k, :], AF.Identity, [tmp_res, self.small_res], [hf_res],
                         bias=sh[:, k, col:col + 1])

    def phase_mod(self):
        A, P = self.A, self.P
        l = self.layer
        self.small_res = Res("small")
        sm = self.small_res
        cond_d = self.din("cond", [128, 8, 2])
        wmod_d = self.din("w_mod", [D, 6 * D])
        bmod_d = self.din("b_mod2", [128, 48, 2])
        n1g_d = self.din("n1g2", [128, 8, 2])
        n2g_d = self.din("n2g2", [128, 8, 2])
        self.mod = A.f32(96).rearrange("p (j c) -> p j c", c=2)
        self.gs1 = A.f32(16).rearrange("p (j c) -> p j c", c=2)
        self.gs2 = A.f32(16).rearrange("p (j c) -> p j c", c=2)
        bmod = A.f32(96).rearrange("p (j c) -> p j c", c=2)
        n1g = A.f32(16).rearrange("p (j c) -> p j c", c=2)
        n2g = A.f32(16).rearrange("p (j c) -> p j c", c=2)
        scond = A.f32(16).rearrange("p (j c) -> p j c", c=2)
        m0 = A.mark()
        sc_res = Res("scond")
        self.load("sp", scond, cond_d, [sc_res])
        self.load("sp", bmod, bmod_d, [sm])
        self.load("sp", n1g, n1g_d, [sm])
        self.load("sp", n2g, n2g_d, [sm])
        self.act(scond, scond, AF.Silu, [sc_res], [sc_res])
        wb = [Buf(A.f32(8 * 512).rearrange("p (k c) -> p k c", k=8), f"wm{i}") for i in range(2)]
        psb = self.ps[0]
        wview = wmod_d.rearrange("(k p) c -> p k c", p=128)
        for blk in range(12):
            w = wb[blk % 2]
            self.load("sp" if blk % 2 == 0 else "act", w.ap, wview[:, :, blk * 512:(blk + 1) * 512], [w.r])
            for jj in range(4):
                j = blk * 4 + jj
                for k in range(8):
                    self.mm(psb.ap[:, 2 * j:2 * j + 2], w.ap[:, k, jj * 128:(jj + 1) * 128], scond[:, k, :],
                            k == 0, k == 7, [w.r, sc_res], [psb.r])
        self.tt("dve", self.mod, psb.ap[:, 0:96].rearrange("p (j c) -> p j c", c=2), bmod, ALU.add, [psb.r, sm], [sm])
        self.ts("dve", self.gs1, self.mod[:, 8:16, :], 1.0, None, ALU.add, None, [sm], [sm])
        self.tt("dve", self.gs1, self.gs1, n1g, ALU.mult, [sm], [sm])
        self.ts("dve", self.gs2, self.mod[:, 32:40, :], 1.0, None, ALU.add, None, [sm], [sm])
        self.tt("dve", self.gs2, self.gs2, n2g, ALU.mult, [sm], [sm])
        self.sh1 = self.mod[:, 0:8, :]
        self.g1 = self.mod[:, 16:24, :]
        self.sh2 = self.mod[:, 24:32, :]
        self.g2 = self.mod[:, 40:48, :]
        self.tap("mod", self.mod, [sm])
        self.tap("gs1", self.gs1, [sm])
        P.barrier()
        A.release(m0)
        self.mark_ffn = A.mark()

    def xblock_src(self, bi):
        if bi < 8:
            return self.xT_v[:, :, bi * TB:(bi + 1) * TB], TB, 0
        return self.cxT_v, CTX, 1

    def phase_kvq(self):
        A, P = self.A, self.P
        wkvq_d = self.din("w_kvq", [D, 7 * 128])
        gkv_d = self.din("g_kv", [128, 2])
        gq_d = self.din("g_q", [128, 3])
        ropeC_d = self.din("ropeC", [128, SEQ])
        ropeS_d = self.din("ropeS", [128, SEQ])
        NK = CTX + SEQ
        self.NK = NK
        nq = OWN + (CTX if self.L0 else 0)
        self.nq = nq
        nq = OWN + (CTX if self.L0 else 0)
        self.nq = nq
        self.oT = Buf(A.bf16(4 * nq).rearrange("p (k t) -> p k t", k=4), "oT")
        self.mark_att = A.mark()
        self.ckvT = Buf(A.bf16(2 * NK).rearrange("p (k t) -> p k t", k=2), "ckvT")
        self.kT = [Buf(A.bf16(NK), f"kT{i}") for i in range(2)]
        self.qnT = Buf(A.bf16(3 * nq).rearrange("p (k t) -> p k t", k=3), "qnT")
        gkv = A.f32(2)
        gq = A.f32(3)
        m0 = A.mark()
        self.load("sp", gkv, gkv_d, [self.small_res])
        self.load("sp", gq, gq_d, [self.small_res])
        w = Buf(A.bf16(8 * 896).rearrange("p (k c) -> p k c", k=8), "wkvq")
        self.loadw_bf16(w.ap, wkvq_d, 8, w.r)
        xb = [Buf(A.f32(8 * TB).rearrange("p (k t) -> p k t", k=8), f"xb{i}") for i in range(1)]
        tmp = Buf(A.f32(8 * TB).rearrange("p (k t) -> p k t", k=8), "tmp")
        hTs = [Buf(A.bf16(8 * TB).rearrange("p (k t) -> p k t", k=8), f"hT{i}") for i in range(2)]
        rstd = Buf(A.f32(TB), "rstd")
        pp = Buf(A.f32(3 * TB).rearrange("p (k t) -> p k t", k=3), "pp")
        sq = Buf(A.f32(3 * TB).rearrange("p (k t) -> p k t", k=3), "sq")
        rs2 = Buf(A.f32(TB), "rs2")
        tabC = Buf(A.f32(TB), "tabC")
        tabS = Buf(A.f32(TB), "tabS")
        t1 = Buf(sq.ap[:, 0, :], "t1")
        t2 = Buf(sq.ap[:, 1, :], "t2")
        t1.res = sq.res
        t2.res = sq.res
        sm = self.small_res
        order = [8] + list(range(8))

        def prep(it_):
            bi_ = order[it_]
            src_, n_, col_ = self.xblock_src(bi_)
            hT_ = hTs[it_ % 2]
            x_ = xb[0]
            self.load("sp", x_.ap[:, :, 0:n_], src_, [x_.r])
            self.norm_mod(x_.ap[:, :, 0:n_], n_, self.gs1, self.sh1, col_, hT_.ap[:, :, 0:n_], tmp.ap[:, :, 0:n_],
                          rstd.ap[:, 0:n_], x_.r, hT_.r, tmp.r, rstd.r, self.ps[0])

        prep(0)
        for it, bi in enumerate(order):
            src, n, col = self.xblock_src(bi)
            hT = hTs[it % 2]
            if it + 1 < len(order):
                prep(it + 1)
            if bi == 0:
                self.tap("hT0", hT.ap, [hT.r], BF16)
            koff = 0 if bi == 8 else CTX + bi * TB
            for g in range(2):
                psb = self.ps[1 + g]
                for k in range(8):
                    self.mm(psb.ap[:, 0:n], w.ap[:, k, g * 128:(g + 1) * 128], hT.ap[:, k, 0:n], k == 0, k == 7,
                            [w.r, hT.r], [psb.r])
                self.cp("act", pp.ap[:, g, 0:n], psb.ap[:, 0:n], [psb.r], [pp.r])
            self.rms_bcast(pp.ap[:, 0:2, 0:n], 2, n, KVL, rs2.ap[:, 0:n], sq.ap[:, 0:2, 0:n], pp.r, rs2.r, sq.r, self.ps[3])
            for g in range(2):
                self.stt(self.ckvT.ap[:, g, koff:koff + n], pp.ap[:, g, 0:n], gkv[:, g:g + 1], rs2.ap[:, 0:n],
                         ALU.mult, ALU.mult, [pp.r, rs2.r, sm], [self.ckvT.r])
            psA, psB = self.ps[4], self.ps[5]
            for k in range(8):
                self.mm(psA.ap[:, 0:n], w.ap[:, k, 256:384], hT.ap[:, k, 0:n], k == 0, k == 7, [w.r, hT.r], [psA.r])
            if bi == 8:
                self.cp("act", self.kT[0].ap[64:96, koff:koff + n], psA.ap[64:96, 0:n], [psA.r], [self.kT[0].r])
            else:
                for k in range(8):
                    self.mm(psB.ap[:, 0:n], w.ap[:, k, 384:512], hT.ap[:, k, 0:n], k == 0, k == 7, [w.r, hT.r], [psB.r])
                self.load("sp", tabC.ap[64:96, 0:n], ropeC_d[64:96, bi * TB:bi * TB + n], [tabC.r])
                self.load("sp", tabS.ap[64:96, 0:n], ropeS_d[64:96, bi * TB:bi * TB + n], [tabS.r])
                self.tt("dve", t1.ap[64:96, 0:n], psA.ap[64:96, 0:n], tabC.ap[64:96, 0:n], ALU.mult, [psA.r, tabC.r], [t1.r])
                self.tt("dve", t2.ap[64:96, 0:n], psB.ap[64:96, 0:n], tabS.ap[64:96, 0:n], ALU.mult, [psB.r, tabS.r], [t2.r])
                self.tt("dve", self.kT[0].ap[64:96, koff:koff + n], t1.ap[64:96, 0:n], t2.ap[64:96, 0:n], ALU.add,
                        [t1.r, t2.r], [self.kT[0].r])
            self.cp("act", self.kT[1].ap[64:96, koff:koff + n], self.kT[0].ap[64:96, koff:koff + n],
                    [self.kT[0].r], [self.kT[1].r])
            isq = (bi < 4) or (bi == 8 and self.L0)
            if isq:
                qoff = bi * TB if bi < 4 else OWN
                for g in range(3):
                    psb = self.ps[6 + (g % 2)]
                    for k in range(8):
                        self.mm(psb.ap[:, 0:n], w.ap[:, k, 512 + g * 128:512 + (g + 1) * 128], hT.ap[:, k, 0:n],
                                k == 0, k == 7, [w.r, hT.r], [psb.r])
                    self.cp("act", pp.ap[:, g, 0:n], psb.ap[:, 0:n], [psb.r], [pp.r])
                self.rms_bcast(pp.ap[:, 0:3, 0:n], 3, n, QL, rs2.ap[:, 0:n], sq.ap[:, 0:3, 0:n], pp.r, rs2.r, sq.r, self.ps[3])
                for g in range(3):
                    self.stt(self.qnT.ap[:, g, qoff:qoff + n], pp.ap[:, g, 0:n], gq[:, g:g + 1], rs2.ap[:, 0:n],
                             ALU.mult, ALU.mult, [pp.r, rs2.r, sm], [self.qnT.r])
        self.tap("ckvT", self.ckvT.ap, [self.ckvT.r], BF16)
        self.tap("kT", self.kT[0].ap[64:96, :], [self.kT[0].r], BF16)
        self.tap("qnT", self.qnT.ap, [self.qnT.r], BF16)
        P.barrier()
        A.release(m0)

    def phase_att(self):
        A, P = self.A, self.P
        NK, nq = self.NK, self.nq
        wuqA_d = self.din("w_uqA", [QL, NH * 96])
        wuqB_d = self.din("w_uqB", [QL, NH * 96])
        wuk_d = self.din("w_uk", [KVL, 512])
        wuv_d = self.din("w_uv", [KVL, 512])
        ropeC_d = self.in_ap("ropeC")
        ropeS_d = self.in_ap("ropeS")
        m0 = A.mark()
        wA = Buf(A.bf16(3 * 768).rearrange("p (k c) -> p k c", k=3), "wA")
        wB = Buf(A.bf16(3 * 768).rearrange("p (k c) -> p k c", k=3), "wB")
        wk = Buf(A.bf16(2 * 512).rearrange("p (k c) -> p k c", k=2), "wk")
        wv = Buf(A.bf16(2 * 512).rearrange("p (k c) -> p k c", k=2), "wv")
        self.loadw_bf16(wA.ap, wuqA_d, 3, wA.r)
        self.loadw_bf16(wB.ap, wuqB_d, 3, wB.r)
        self.loadw_bf16(wk.ap, wuk_d, 2, wk.r)
        self.loadw_bf16(wv.ap, wuv_d, 2, wv.r)
        tabC = Buf(A.f32(TB), "qtabC")
        tabS = Buf(A.f32(TB), "qtabS")
        qh = [Buf(A.bf16(nq), f"qh{i}") for i in range(2)]
        NKC = NK // 128
        vT = [Buf(A.bf16(NKC * 128).rearrange("p (c m) -> p c m", m=128), f"vT{i}") for i in range(2)]
        self.memset("pool", vT[0].ap[:, :, 64:128], 1.0, [vT[0].r])
        self.memset("pool", vT[1].ap[:, :, 0:64], 1.0, [vT[1].r])
        Pt = [Buf(A.bf16(TB), f"Pt{i}") for i in range(4)]
        t1 = Buf(A.f32(TB), "at1")
        t2 = Buf(A.f32(TB), "at2")
        rec = Buf(A.f32(TB), "rec")
        qblocks = [(i * TB, TB, True) for i in range(4)]
        if self.L0:
            qblocks.append((OWN, CTX, False))
        kblocks = [(0, CTX)] + [(CTX + i * TB, TB) for i in range(8)]
        psS = self.ps[0:4]
        psO = self.ps[4:6]
        psQ = [self.ps[6], self.ps[7], self.ps[6]]
        oi = 0
        for h in range(NH):
            par = h % 2
            q = qh[par]
            kT = self.kT[par]
            v = vT[par]
            for bq, (c0, n, lat) in enumerate(qblocks):
                pa = psQ[0]
                for k in range(3):
                    self.mm(pa.ap[0:96, 0:n], wA.ap[:, k, h * 96:(h + 1) * 96], self.qnT.ap[:, k, c0:c0 + n],
                            k == 0, k == 2, [wA.r, self.qnT.r], [pa.r])
                self.cp("act", q.ap[0:64, c0:c0 + n], pa.ap[0:64, 0:n], [pa.r], [q.r])
                if lat:
                    self.load("sp", tabC.ap[64:96, 0:n], ropeC_d[64:96, c0:c0 + n], [tabC.r])
                    self.load("sp", tabS.ap[64:96, 0:n], ropeS_d[64:96, c0:c0 + n], [tabS.r])
                    pb = psQ[1]
                    for k in range(3):
                        self.mm(pb.ap[0:96, 0:n], wB.ap[:, k, h * 96:(h + 1) * 96], self.qnT.ap[:, k, c0:c0 + n],
                                k == 0, k == 2, [wB.r, self.qnT.r], [pb.r])
                    self.tt("dve", t1.ap[64:96, 0:n], pa.ap[64:96, 0:n], tabC.ap[64:96, 0:n], ALU.mult,
                            [pa.r, tabC.r], [t1.r])
                    self.tt("dve", t2.ap[64:96, 0:n], pb.ap[64:96, 0:n], tabS.ap[64:96, 0:n], ALU.mult,
                            [pb.r, tabS.r], [t2.r])
                    self.tt("dve", q.ap[64:96, c0:c0 + n], t1.ap[64:96, 0:n], t2.ap[64:96, 0:n], ALU.add,
                            [t1.r, t2.r], [q.r])
                else:
                    self.cp("act", q.ap[64:96, c0:c0 + n], pa.ap[64:96, 0:n], [pa.r], [q.r])
            for (c0, n) in kblocks:
                pk = psQ[2]
                for k in range(2):
                    self.mm(pk.ap[0:64, 0:n], wk.ap[:, k, h * 64:(h + 1) * 64], self.ckvT.ap[:, k, c0:c0 + n],
                            k == 0, k == 1, [wk.r, self.ckvT.r], [pk.r])
                self.cp("act", kT.ap[0:64, c0:c0 + n], pk.ap[0:64, 0:n], [pk.r], [kT.r])
            voff = 0 if par == 0 else 64
            for g0 in range(0, NKC, 8):
                gn = min(8, NKC - g0)
                pv = psQ[(g0 // 8) % 2]
                for c in range(gn):
                    kc = g0 + c
                    for k in range(2):
                        self.mm(pv.ap[:, c * 64:(c + 1) * 64], self.ckvT.ap[:, k, kc * 128:(kc + 1) * 128],
                                wv.ap[:, k, h * 64:(h + 1) * 64], k == 0, k == 1, [wv.r, self.ckvT.r], [pv.r],
                                inc=(k == 1 and c == gn - 1))
                self.cp("act", v.ap[:, g0:g0 + gn, voff:voff + 64],
                        pv.ap[:, 0:gn * 64].rearrange("p (c m) -> p c m", m=64), [pv.r], [v.r])
            if h == 0:
                self.tap("qh0", q.ap[0:96, :], [q.r], BF16)
                self.tap("kh0", kT.ap[0:96, :], [kT.r], BF16)
                self.tap("vh0", v.ap, [v.r], BF16)
            for (c0, n, lat) in qblocks:
                kcs = list(range(NKC)) if lat else [0, 1]
                po = psO[oi % 2]
                oi += 1

                def S(i):
                    kc = kcs[i]
                    ps = psS[i % 4]
                    self.mm(ps.ap[:, 0:n], kT.ap[0:96, kc * 128:(kc + 1) * 128], q.ap[0:96, c0:c0 + n], True, True,
                            [kT.r, q.r], [ps.r])

                S(0)
                if len(kcs) > 1:
                    S(1)
                if len(kcs) > 2:
                    S(2)
                for i, kc in enumerate(kcs):
                    ps = psS[i % 4]
                    pt = Pt[i % 4]
                    self.act(pt.ap[:, 0:n], ps.ap[:, 0:n], AF.Exp, [ps.r], [pt.r], scale=ATT_SCALE)
                    self.mm(po.ap[:, 0:n], v.ap[:, kc, :], pt.ap[:, 0:n], i == 0, i == len(kcs) - 1,
                            [v.r, pt.r], [po.r])
                    if i + 3 < len(kcs):
                        S(i + 3)
                pair = h // 2
                if par == 0:
                    self.recip(rec.ap[0:64, 0:n], po.ap[64:128, 0:n], [po.r], [rec.r])
                    self.tt("dve", self.oT.ap[0:64, pair, c0:c0 + n], po.ap[0:64, 0:n], rec.ap[0:64, 0:n], ALU.mult,
                            [po.r, rec.r], [self.oT.r])
                else:
                    self.recip(rec.ap[64:128, 0:n], po.ap[0:64, 0:n], [po.r], [rec.r])
                    self.tt("dve", self.oT.ap[64:128, pair, c0:c0 + n], po.ap[64:128, 0:n], rec.ap[64:128, 0:n],
                            ALU.mult, [po.r, rec.r], [self.oT.r])
        self.tap("oT", self.oT.ap, [self.oT.r], BF16)
        P.barrier()
        A.release(m0)

    def in_ap(self, name):
        return self._in_aps[name]

    def finish(self):
        P = self.P
        P.barrier()
        with self.nc.Block() as block:
            P.emit(block)
        self.es.close()
        return self.nc

MAGIC = 12582912.0
TWO_PI = 2.0 * math.pi


class LayerK2(LayerK):
    def _filter_mlp(self, zf_d, ncols, h2T, h2_res, w1, w2, fcol, fb1, fb2, wres):
        A = self.A
        m0 = A.mark()
        zf = Buf(A.f32(512), "zf")
        a1 = Buf(A.f32(512), "a1")
        a2 = Buf(A.f32(512), "a2")
        h1 = Buf(A.f32(512), "h1")
        ps1, ps2 = self.ps[0], self.ps[1]
        for c0 in range(0, ncols, 512):
            n = min(512, ncols - c0)
            self.load("sp", zf.ap[0:17, 0:n], zf_d[:, c0:c0 + n], [zf.r])
            self.mm(ps1.ap[0:64, 0:n], w1[0:17, :], zf.ap[0:17, 0:n], True, True, [wres, zf.r], [ps1.r])
            for (ps, fb, dst, dres) in ((ps1, fb1, h1.ap[0:64, 0:n], h1.r), (ps2, fb2, h2T[0:64, c0:c0 + n], h2_res)):
                if ps is ps2:
                    self.mm(ps2.ap[0:64, 0:n], w2[0:64, :], h1.ap[0:64, 0:n], True, True, [wres, h1.r], [ps2.r])
                self.ts("dve", a1.ap[0:64, 0:n], ps.ap[0:64, 0:n], fcol, fb, ALU.mult, ALU.add, [ps.r, wres], [a1.r])
                self.ts("dve", a2.ap[0:64, 0:n], a1.ap[0:64, 0:n], 1.0 / TWO_PI, MAGIC, ALU.mult, ALU.add, [a1.r], [a2.r])
                self.ts("dve", a2.ap[0:64, 0:n], a2.ap[0:64, 0:n], MAGIC, TWO_PI, ALU.subtract, ALU.mult, [a2.r], [a2.r])
                self.tt("dve", a1.ap[0:64, 0:n], a1.ap[0:64, 0:n], a2.ap[0:64, 0:n], ALU.subtract, [a1.r, a2.r], [a1.r])
                self.act(dst, a1.ap[0:64, 0:n], AF.Sin, [a1.r], [dres])
        A.release(m0)

    def _cmul(self, src, t1, t2, out_r, out_i, conj, m1, m2, src_res, tab_res, out_res):
        i = self._cm_i = getattr(self, "_cm_i", 0) + 1
        m1 = m1[i % len(m1)]
        m2 = m2[i % len(m2)]
        self.tt("dve", m1.ap, src, t1, ALU.mult, src_res + tab_res, [m1.r])
        self.tt("dve", m2.ap, src, t2, ALU.mult, src_res + tab_res, [m2.r])
        if not conj:
            self.tt("pool", out_r, m1.ap[:, 0:128], m2.ap[:, 128:256], ALU.subtract, [m1.r, m2.r], out_res)
            self.tt("pool", out_i, m2.ap[:, 0:128], m1.ap[:, 128:256], ALU.add, [m1.r, m2.r], out_res)
        else:
            self.tt("pool", out_r, m1.ap[:, 0:128], m2.ap[:, 128:256], ALU.add, [m1.r, m2.r], out_res)
            self.tt("pool", out_i, m1.ap[:, 128:256], m2.ap[:, 0:128], ALU.subtract, [m1.r, m2.r], out_res)

    def load_fft_tables(self):
        A = self.A
        self.ft_res = Res("fft_tabs")
        r = self.ft_res
        d = {}
        for nm in ("CS1a", "CS1b"):
            d[nm] = self.din(nm, [128, 256])
        for nm in ("W3r", "W3i", "W3n"):
            d[nm] = self.din(nm, [128, 128])
        for nm in ("T1", "T2"):
            d[nm] = self.din(nm, [128, 256])
        for nm in ("ICr", "ICi"):
            d[nm] = self.din(nm, [128, 64])
        self.CS1a = A.bf16(256); self.CS1b = A.bf16(256)
        self.W3r = A.bf16(128); self.W3i = A.bf16(128); self.W3n = A.bf16(128)
        self.T1 = A.f32(256); self.T2 = A.f32(256)
        self.ICr = A.bf16(64); self.ICi = A.bf16(64)
        for nm in ("CS1a", "CS1b", "W3r", "W3i", "W3n", "ICr", "ICi"):
            self.P.dma("pool", getattr(self, nm), d[nm], writes=[r])
        for nm in ("T1", "T2"):
            self.P.dma("sp", getattr(self, nm), d[nm], writes=[r])

    def _fft_s1(self, lhs_list, cs_list, lhs_res, psA, m1, m2, Bt):
        r = self.ft_res
        nl = len(lhs_list)
        for i, (lh, cs) in enumerate(zip(lhs_list, cs_list)):
            self.mm(psA.ap[:, 0:256], lh, cs, i == 0, i == nl - 1, lhs_res + [r], [psA.r])
        self._cmul(psA.ap[:, 0:256], self.T1, self.T2, Bt.ap[:, 0:128], Bt.ap[:, 128:256], False, m1, m2,
                   [psA.r], [r], [Bt.r])

    def _fft_s3(self, Bt, psZ):
        r = self.ft_res
        self.mm(psZ.ap[:, 0:128], self.W3r, Bt.ap[:, 0:128], True, False, [r, Bt.r], [psZ.r])
        self.mm(psZ.ap[:, 0:128], self.W3n, Bt.ap[:, 128:256], False, True, [r, Bt.r], [psZ.r])
        self.mm(psZ.ap[:, 128:256], self.W3i, Bt.ap[:, 0:128], True, False, [r, Bt.r], [psZ.r])
        self.mm(psZ.ap[:, 128:256], self.W3r, Bt.ap[:, 128:256], False, True, [r, Bt.r], [psZ.r])

    def phase_fg(self):
        A, P = self.A, self.P
        sm = self.small_res
        zff_d = self.din("zfT_f", [17, SEQ])
        zfb_d = self.din("zfT_b", [17, SEQ])
        tf_d = self.din("t_f", [1, SEQ])
        tb_d = self.din("t_b", [1, SEQ])
        w1_d = self.din("hy_w1", [17, 64])
        w2_d = self.din("hy_w2", [64, 64])
        w3_d = self.din("hy_w3", [64, 1024])
        fq_d = self.din("hy_fq", [64, 3])
        dec_d = self.din("hy_decay", [1, 1024])
        bias_d = self.din("hy_bias", [1, 512])
        self.Kh_d = self.nc.dram_tensor(f"Kh_scr{self.layer}", [128, 128, 512], F32, kind="Internal").ap()
        self.kh_res = Res("kh")
        self.x0T = Buf(A.bf16(4 * (OWN + TB)).rearrange("p (c t) -> p c t", c=4), "x0T")
        self.ohyT = Buf(self.x0T.ap[:, :, 1:OWN + 1], "ohyT")
        self.ohyT.res = self.x0T.res
        if self.L0:
            self.x0cT = Buf(A.bf16(4 * (CTX + 2)).rearrange("p (c t) -> p c t", c=4), "x0cT")
            self.ohycT = Buf(self.x0cT.ap[:, :, 1:CTX + 1], "ohycT")
            self.ohycT.res = self.x0cT.res
        self.rL1 = A.f32(4)
        self.mark_hyw = A.mark()
        self.load_fft_tables()
        if self.L0:
            self.zcT = Buf(A.f32(4 * (CTX + 1)).rearrange("p (c t) -> p c t", c=4), "zcT")
        self.fw_res = Res("fw")
        fw = self.fw_res
        self.hw1 = A.f32(64); self.hw2 = A.f32(64); self.hw3 = A.f32(1024); self.hfq = A.f32(3)
        self.hfb = A.f32(2)
        self.negdec = A.f32(1024)
        self.hbias = A.f32(512)
        self.load("sp", self.hw1[0:17, :], w1_d, [fw])
        self.load("sp", self.hw2[0:64, :], w2_d, [fw])
        self.load("sp", self.hw3[0:64, :], w3_d, [fw])
        self.load("sp", self.hfq[0:64, :], fq_d, [fw])
        self.load("sp", self.negdec[64:65, :], dec_d, [fw])
        self.load("sp", self.hbias[0:1, :], bias_d, [fw])
        self.ts("dve", self.hfb[0:64, 0:1], self.hfq[0:64, 1:2], self.hfq[0:64, 0:1], None, ALU.mult, None, [fw], [fw])
        self.ts("dve", self.hfb[0:64, 1:2], self.hfq[0:64, 2:3], self.hfq[0:64, 0:1], None, ALU.mult, None, [fw], [fw])
        self.act(self.negdec[64:65, :], self.negdec[64:65, :], AF.Abs, [fw], [fw])
        self.ts("dve", self.negdec[64:65, :], self.negdec[64:65, :], -1.0, None, ALU.mult, None, [fw], [fw])
        m0 = A.mark()
        h2 = [Buf(A.f32(SEQ), f"h2_{i}") for i in range(2)]
        trow = [Buf(h2[i].ap, f"trow{i}") for i in range(2)]
        self.load("sp", trow[0].ap[64:65, :], tf_d, [trow[0].r])
        self.load("sp", trow[1].ap[64:65, :], tb_d, [trow[1].r])
        fcol = self.hfq[0:64, 0:1]
        for d, zd in enumerate((zff_d, zfb_d)):
            self._filter_mlp(zd, SEQ, h2[d].ap, h2[d].r, self.hw1, self.hw2, fcol, self.hfb[0:64, 0:1],
                             self.hfb[0:64, 1:2], fw)
        kf = [Buf(A.bf16(16384).rearrange("p (g n c) -> p g n c", g=128, n=32), f"kfft{i}") for i in range(2)]
        acc = Buf(A.f32(512), "l1acc")
        self.memset("dve", acc.ap, 0.0, [acc.r])
        Es = [Buf(A.f32(512), f"E{i}") for i in range(2)]
        Ab = [Buf(A.f32(512), f"Ab{i}") for i in range(2)]
        kts = [Buf(A.f32(512), f"kt{i}") for i in range(2)]
        k0 = Buf(A.f32(512), "k0")
        psKs, psEs = [self.ps[2], self.ps[4]], [self.ps[3], self.ps[5]]
        itk = 0
        for d in range(2):
            for n2 in range(32):
                E, kt, ab = Es[itk % 2], kts[itk % 2], Ab[itk % 2]
                psK, psE = psKs[itk % 2], psEs[itk % 2]
                itk += 1
                lh = h2[d].ap.rearrange("p (a b) -> p a b", b=32)[0:64, :, n2]
                self.mm(psK.ap[:, :], lh, self.hw3[0:64, d * 512:(d + 1) * 512], True, True, [h2[d].r, fw], [psK.r])
                self.mm(psE.ap[:, :], trow[d].ap.rearrange("p (a b) -> p a b", b=32)[64:65, :, n2], self.negdec[64:65, d * 512:(d + 1) * 512], True, True,
                        [trow[d].r, fw], [psE.r])
                self.act(E.ap, psE.ap, AF.Exp, [psE.r], [E.r])
                sgn = 1.0 if d == 0 else -1.0
                self.stt(kt.ap, psK.ap, sgn, E.ap, ALU.mult, ALU.mult, [psK.r, E.r], [kt.r])
                if d == 1 and n2 == 0:
                    self.memset("dve", kt.ap[0:1, :], 0.0, [kt.r])
                if d == 0 and n2 == 0:
                    self.cp("dve", k0.ap[0:1, :], kt.ap[0:1, :], [kt.r], [k0.r])
                self.act(ab.ap, kt.ap, AF.Abs, [kt.r], [ab.r])
                self.tt("pool", acc.ap, acc.ap, ab.ap, ALU.add, [ab.r], [acc.r])
                self.cp("act", kf[d].ap[:, :, n2, :], kt.ap.rearrange("p (g c) -> p g c", c=4), [kt.r], [kf[d].r])
        kt = kts[0]
        psL = self.ps[4]
        self.mm(psL.ap[:, :], self.ones_f.ap, acc.ap, True, True, [acc.r, self.ones_f.r], [psL.r])
        self.tt("dve", kt.ap[0:1, :], psL.ap[0:1, :], self.hbias[0:1, :], ALU.mult, [psL.r, fw], [kt.r])
        self.tt("dve", kt.ap[0:1, :], kt.ap[0:1, :], k0.ap[0:1, :], ALU.add, [kt.r, k0.r], [kt.r])
        self.cp("act", kf[0].ap[0:1, :, 0, :], kt.ap[0:1, :].rearrange("p (g c) -> p g c", c=4), [kt.r], [kf[0].r])
        psC = self.ps[5]
        for c in range(4):
            self.mm(psC.ap[:, c:c + 1], acc.ap[:, c * 128:(c + 1) * 128], self.ones_f.ap[:, 0:1], True, True,
                    [acc.r, self.ones_f.r], [psC.r])
        self.recip(self.rL1, psC.ap[:, 0:4], [psC.r], [sm])
        self.tap("rL1", self.rL1, [sm])
        self.tap("kf0", kf[0].ap, [kf[0].r], BF16)
        self.tap("kf1", kf[1].ap, [kf[1].r], BF16)
        m1 = [Buf(A.f32(256), f"m1_{i}") for i in range(3)]; m2 = [Buf(A.f32(256), f"m2_{i}") for i in range(3)]
        Bt = [Buf(A.bf16(256), f"Bt{i}") for i in range(2)]
        Ks = [Buf(A.f32(512), f"Ks{i}") for i in range(2)]
        psAs, psZs = [self.ps[4], self.ps[5]], [self.ps[6], self.ps[7]]
        for it in range(128 + 1):
            g = it
            if g < 128:
                self._fft_s1([kf[0].ap[:, g, :, :].rearrange("p n c -> p (n c)"),
                              kf[1].ap[:, g, :, :].rearrange("p n c -> p (n c)")],
                             [self.CS1a, self.CS1b], [kf[0].r, kf[1].r], psAs[g % 2], m1, m2, Bt[g % 2])
            g = it - 1
            if 0 <= g < 128:
                psZ = psZs[g % 2]
                self._fft_s3(Bt[g % 2], psZ)
                ks = Ks[g % 2]
                self.cp("act", ks.ap[:, 0:512].rearrange("p (a b c) -> p a b c", a=2, b=2)[:, :, 0, :],
                        psZ.ap[:, 0:256].rearrange("p (a c) -> p a c", a=2), [psZ.r], [ks.r])
                self.cp("act", ks.ap[:, 0:512].rearrange("p (a b c) -> p a b c", a=2, b=2)[:, :, 1, :],
                        psZ.ap[:, 0:256].rearrange("p (a c) -> p a c", a=2), [psZ.r], [ks.r])
                self.P.dma("sp", self.Kh_d[g], ks.ap, reads=[ks.r], writes=[self.kh_res])
        P.barrier()
        A.release(m0)

    def phase_h(self):
        A, P = self.A, self.P
        sm = self.small_res
        why_d = self.din("w_hy", [D, 1536])
        sw_d = self.din("hy_sw", [128, 12, 4])
        sw = A.f32(48).rearrange("p (g j) -> p g j", j=4)
        self.load("sp", sw, sw_d, [sm])
        if self.L0:
            swc_d = self.din("hy_swc", [128, 12, 4])
            swc = A.f32(48).rearrange("p (g j) -> p g j", j=4)
            self.load("sp", swc, swc_d, [sm])
        self.zT = Buf(A.bf16(4 * (SEQ + 1)).rearrange("p (c t) -> p c t", c=4), "zT")
        m0 = A.mark()
        w = Buf(A.bf16(8 * 1536).rearrange("p (k c) -> p k c", k=8), "why")
        self.loadw_bf16(w.ap, why_d, 8, w.r)
        xb = [Buf(A.f32(8 * TB).rearrange("p (k t) -> p k t", k=8), f"xb{i}") for i in range(1)]
        tmp = Buf(A.f32(8 * TB).rearrange("p (k t) -> p k t", k=8), "tmp")
        hTs = [Buf(A.bf16(8 * TB).rearrange("p (k t) -> p k t", k=8), f"hT{i}") for i in range(2)]
        rstd = Buf(A.f32(TB), "rstd")
        R = [Buf(A.bf16(TB + 2), f"R{g}") for g in range(12)]
        ua = [Buf(A.f32(TB), f"ua{i}") for i in range(2)]
        ub = [Buf(A.f32(TB), f"ub{i}") for i in range(2)]

        cur = {"sw": sw}

        def conv(g, n, out_ap, out_res, eng_tmp):
            r = R[g]
            sw = cur["sw"]
            self.ts("dve", eng_tmp.ap[:, 0:n], r.ap[:, 0:n], sw[:, g, 0:1], sw[:, g, 3:4], ALU.mult, ALU.add,
                    [r.r, sm], [eng_tmp.r])
            self.stt(eng_tmp.ap[:, 0:n], r.ap[:, 1:n + 1], sw[:, g, 1:2], eng_tmp.ap[:, 0:n], ALU.mult, ALU.add,
                     [r.r, sm], [eng_tmp.r])
            self.stt(out_ap, r.ap[:, 2:n + 2], sw[:, g, 2:3], eng_tmp.ap[:, 0:n], ALU.mult, ALU.add,
                     [r.r, sm, eng_tmp.r], out_res)

        def reset_R():
            for g in range(12):
                self.memset("pool", R[g].ap[:, 0:2], 0.0, [R[g].r])

        order = ([8] if self.L0 else []) + list(range(8))
        reset_R()

        def prep(it_):
            bi_ = order[it_]
            src_, n_, col_ = self.xblock_src(bi_)
            hT_ = hTs[it_ % 2]
            x_ = xb[0]
            self.load("sp", x_.ap[:, :, 0:n_], src_, [x_.r])
            self.norm_mod(x_.ap[:, :, 0:n_], n_, self.gs1, self.sh1, col_, hT_.ap[:, :, 0:n_], tmp.ap[:, :, 0:n_],
                          rstd.ap[:, 0:n_], x_.r, hT_.r, tmp.r, rstd.r, self.ps[0])

        prep(0)
        for it, bi in enumerate(order):
            src, n, col = self.xblock_src(bi)
            cur["sw"] = swc if bi == 8 else sw
            hT = hTs[it % 2]
            if it + 1 < len(order):
                prep(it + 1)
            need_x0 = (bi == 8) or (bi <= 4)
            groups = list(range(12)) if need_x0 else list(range(8))
            for g in groups:
                psb = self.ps[1 + (g % 4)]
                for k in range(8):
                    self.mm(psb.ap[:, 0:n], w.ap[:, k, g * 128:(g + 1) * 128], hT.ap[:, k, 0:n], k == 0, k == 7,
                            [w.r, hT.r], [psb.r])
                self.cp("act", R[g].ap[:, 2:n + 2], psb.ap[:, 0:n], [psb.r], [R[g].r])
            base = 0 if bi == 8 else bi * TB
            zdst = self.zcT if bi == 8 else self.zT
            x0dst = self.x0cT if bi == 8 else self.x0T
            T4 = [ua[0], ub[0], ua[1], ub[1]]

            def conv_batch(gl, outs):
                sw_ = cur["sw"]
                for i, g in enumerate(gl):
                    t_ = T4[i]
                    self.ts("dve", t_.ap[:, 0:n], R[g].ap[:, 0:n], sw_[:, g, 0:1], sw_[:, g, 3:4], ALU.mult, ALU.add,
                            [R[g].r, sm], [t_.r])
                for i, g in enumerate(gl):
                    t_ = T4[i]
                    self.stt(t_.ap[:, 0:n], R[g].ap[:, 1:n + 1], sw_[:, g, 1:2], t_.ap[:, 0:n], ALU.mult, ALU.add,
                             [R[g].r, sm], [t_.r])
                for i, g in enumerate(gl):
                    t_ = T4[i]
                    oap, ores = outs[i]
                    self.stt(oap, R[g].ap[:, 2:n + 2], sw_[:, g, 2:3], t_.ap[:, 0:n], ALU.mult, ALU.add,
                             [R[g].r, sm, t_.r], ores)

            for c2 in range(0, 4, 2):
                gl = [c2, 4 + c2, c2 + 1, 4 + c2 + 1]
                conv_batch(gl, [(T4[i].ap[:, 0:n], [T4[i].r]) for i in range(4)])
                self.tt("dve", zdst.ap[:, c2, base:base + n], T4[0].ap[:, 0:n], T4[1].ap[:, 0:n], ALU.mult,
                        [T4[0].r, T4[1].r], [zdst.r])
                self.tt("dve", zdst.ap[:, c2 + 1, base:base + n], T4[2].ap[:, 0:n], T4[3].ap[:, 0:n], ALU.mult,
                        [T4[2].r, T4[3].r], [zdst.r])
            if need_x0:
                conv_batch([8, 9, 10, 11], [(x0dst.ap[:, c, base:base + n], [x0dst.r]) for c in range(4)])
            for g in groups:
                self.cp("pool", R[g].ap[:, 0:2], R[g].ap[:, n:n + 2], [R[g].r], [R[g].r])
            last_of_seq = (bi == 8) or (bi == 7)
            if last_of_seq:
                fl = base + n
                fgroups = list(range(12)) if bi == 8 else list(range(8))
                for g in fgroups:
                    self.memset("pool", R[g].ap[:, 2:3], 0.0, [R[g].r])
                for c in range(4):
                    a, b_ = ua[c % 2], ub[c % 2]
                    conv(c, 1, a.ap[:, 0:1], [a.r], a)
                    conv(4 + c, 1, b_.ap[:, 0:1], [b_.r], b_)
                    self.tt("dve", zdst.ap[:, c, fl:fl + 1], a.ap[:, 0:1], b_.ap[:, 0:1], ALU.mult, [a.r, b_.r], [zdst.r])
                    if bi == 8:
                        conv(8 + c, 1, x0dst.ap[:, c, fl:fl + 1], [x0dst.r], a)
                reset_R()
        self.tap("zT", self.zT.ap, [self.zT.r], BF16)
        self.tap("x0T", self.x0T.ap, [self.x0T.r], BF16)
        if self.L0:
            self.tap("zcT", self.zcT.ap, [self.zcT.r])
            self.tap("x0cT", self.x0cT.ap, [self.x0cT.r], BF16)
        P.barrier()
        A.release(m0)

    def phase_f(self):
        A, P = self.A, self.P
        sm = self.small_res
        r = self.ft_res
        m0 = A.mark()
        ztm = Buf(A.bf16(16384).rearrange("p (g n c) -> p g n c", g=128, n=32), "ztm")
        zv = self.zT.ap[:, :, 1:SEQ + 1].rearrange("p c (a b) -> p c a b", b=32)
        psT = [self.ps[0], self.ps[1]]
        ti = 0
        for cc in range(4):
            for n20 in range(0, 32, 4):
                pt = psT[ti % 2]
                ti += 1
                ptb = pt.ap.bitcast(BF16)
                for j in range(4):
                    self.tr(ptb[:, j * 128:(j + 1) * 128], zv[:, cc, :, n20 + j], self.ident_b.ap,
                            [self.zT.r, self.ident_b.r], [pt.r])
                self.cp("act" if ti % 2 else "dve",
                        ztm.ap[:, cc * 32:(cc + 1) * 32, n20:n20 + 4, :].rearrange("p g n c -> p n g c"),
                        ptb[:, 0:512].rearrange("p (n g c) -> p n g c", n=4, c=4), [pt.r], [ztm.r])
        self.tap("ztm", ztm.ap, [ztm.r], BF16)
        m1 = [Buf(A.f32(256), f"m1_{i}") for i in range(3)]; m2 = [Buf(A.f32(256), f"m2_{i}") for i in range(3)]
        Bt = [Buf(A.bf16(256), f"Bt{i}") for i in range(2)]
        Ks = [Buf(A.f32(512), f"Ks{i}") for i in range(2)]
        Yt = [Buf(A.bf16(256), f"Yt{i}") for i in range(2)]
        Gp = [Buf(A.bf16(256), f"Gp{i}") for i in range(2)]
        GT = [Buf(A.bf16(1024).rearrange("p (r c) -> p r c", r=2), f"GT{i}") for i in range(2)]
        yall = Buf(A.f32(32 * 128), "yall")
        yv = yall.ap.rearrange("p (n g c) -> p n g c", n=32, g=32)
        y3 = yall.ap.rearrange("p (n m) -> p n m", n=32)
        psAs, psZs, psGs = [self.ps[0], self.ps[1]], [self.ps[2], self.ps[3]], [self.ps[4], self.ps[5]]
        psM, psYT = self.ps[6], self.ps[7]
        psGT_res, psY_res = Res("psGT"), Res("psY")
        pgb = psM.ap.bitcast(BF16)
        psY = psM.ap[0:64, 256:512]
        GT2 = [Buf(A.bf16(512).rearrange("p (r c) -> p r c", r=2), f"GTp{i}") for i in range(2)]

        def stage_a(g):
            ks = Ks[g % 2]
            self.P.dma("sp", ks.ap, self.Kh_d[g], reads=[self.kh_res], writes=[ks.r])
            self._fft_s1([ztm.ap[:, g, :, :].rearrange("p n c -> p (n c)")], [self.CS1a], [ztm.r],
                         psAs[g % 2], m1, m2, Bt[g % 2])

        def stage_b(g):
            ks = Ks[g % 2]
            psZ = psZs[g % 2]
            self._fft_s3(Bt[g % 2], psZ)
            yt = Yt[g % 2]
            self._cmul(psZ.ap[:, 0:256], ks.ap[:, 0:256], ks.ap[:, 256:512], yt.ap[:, 0:128], yt.ap[:, 128:256],
                       False, m1, m2, [psZ.r], [ks.r], [yt.r])

        def stage_c(g):
            yt = Yt[g % 2]
            psG = psGs[g % 2]
            self.mm(psG.ap[:, 0:128], self.W3r, yt.ap[:, 0:128], True, False, [r, yt.r], [psG.r])
            self.mm(psG.ap[:, 0:128], self.W3i, yt.ap[:, 128:256], False, True, [r, yt.r], [psG.r])
            self.mm(psG.ap[:, 128:256], self.W3n, yt.ap[:, 0:128], True, False, [r, yt.r], [psG.r])
            self.mm(psG.ap[:, 128:256], self.W3r, yt.ap[:, 128:256], False, True, [r, yt.r], [psG.r])
            gp = Gp[g % 2]
            self._cmul(psG.ap[:, 0:256], self.T1, self.T2, gp.ap[:, 0:128], gp.ap[:, 128:256], True, m1, m2,
                       [psG.r], [r], [gp.r])

        def stage_d(g):
            gp = Gp[g % 2]
            gt = GT2[(g // 2) % 2]
            gl = g % 2
            self.tr(pgb[:, 0:128], gp.ap[:, 0:128], self.ident_b.ap, [gp.r, self.ident_b.r], [psGT_res])
            self.tr(pgb[:, 128:256], gp.ap[:, 128:256], self.ident_b.ap, [gp.r, self.ident_b.r], [psGT_res])
            self.cp("act", gt.ap[:, :, gl * 128:(gl + 1) * 128], pgb[:, 0:256].rearrange("p (r c) -> p r c", r=2),
                    [psGT_res], [gt.r])
            if gl == 1:
                self.mm(psY, self.ICr, gt.ap[:, 0, :], True, False, [r, gt.r], [psY_res])
                self.mm(psY, self.ICi, gt.ap[:, 1, :], False, True, [r, gt.r], [psY_res])
                gq = (g // 2) % 16
                self.cp("act", yv[0:64, :, gq * 2:(gq + 1) * 2, :].rearrange("p n g c -> p g n c"),
                        psY.rearrange("p (g n c) -> p g n c", g=2, n=32), [psY_res], [yall.r])
            if g % 32 == 31:
                cc = g // 32
                if cc == 0:
                    self.tap("yall0", yall.ap[0:64, :], [yall.r])
                x0v = self.x0T.ap[:, cc, 1:OWN + 1].rearrange("p (a b) -> p a b", b=32)
                ov = self.ohyT.ap[:, cc, :].rearrange("p (a b) -> p a b", b=32)
                for n20 in range(0, 32, 8):
                    for j in range(8):
                        n2 = n20 + j
                        self.tr(psYT.ap[:, j * 64:(j + 1) * 64], y3[0:64, n2, :], self.ident_f.ap[0:64, 0:64],
                                [yall.r, self.ident_f.r], [psYT.r])
                    self.stt(ov[:, :, n20:n20 + 8].rearrange("p a b -> p b a"),
                             psYT.ap[:, 0:512].rearrange("p (b a) -> p b a", b=8), self.rL1[:, cc:cc + 1],
                             x0v[:, :, n20:n20 + 8].rearrange("p a b -> p b a"), ALU.mult, ALU.mult,
                             [psYT.r, self.x0T.r, sm], [self.ohyT.r])

        for it in range(128 + 3):
            if it < 128:
                stage_a(it)
            if 0 <= it - 1 < 128:
                stage_b(it - 1)
            if 0 <= it - 2 < 128:
                stage_c(it - 2)
            if 0 <= it - 3 < 128:
                stage_d(it - 3)
        self.tap("ohyT", self.ohyT.ap, [self.ohyT.r], BF16)
        P.barrier()
        A.release(m0)

    def phase_ctxhy(self):
        A, P = self.A, self.P
        sm = self.small_res
        fw = self.fw_res
        zfc_d = self.din("zfT_c", [17, CTX])
        tc_d = self.din("tc_bc", [128, CTX])
        w3c_d = self.din("hyc_w3", [64, 1024])
        dec_d = self.din("hyc_dec", [128, 8])
        bias_d = self.din("hyc_bias", [128, 4])
        m0 = A.mark()
        w3c = A.f32(1024)
        nd = A.f32(8)
        bc = A.f32(4)
        tcb = A.f32(CTX)
        cw = Res("ctxw")
        self.load("sp", w3c[0:64, :], w3c_d, [cw])
        self.load("sp", nd, dec_d, [cw])
        self.load("sp", bc, bias_d, [cw])
        self.load("sp", tcb, tc_d, [cw])
        self.act(nd, nd, AF.Abs, [cw], [cw])
        self.ts("dve", nd, nd, -1.0, None, ALU.mult, None, [cw], [cw])
        h2c = Buf(A.f32(CTX), "h2c")
        self._filter_mlp(zfc_d, CTX, h2c.ap, h2c.r, self.hw1, self.hw2, self.hfq[0:64, 0:1], self.hfb[0:64, 0:1],
                         self.hfb[0:64, 1:2], fw)
        kc = [Buf(A.f32(4 * CTX).rearrange("p (c t) -> p c t", c=4), f"kc{d}") for d in range(2)]
        E = Buf(A.f32(CTX), "Ec")
        l1 = Buf(A.f32(8), "l1c")
        for d in range(2):
            for c in range(4):
                ps = self.ps[(d * 4 + c) % 2]
                self.mm(ps.ap[:, 0:CTX], w3c[0:64, d * 512 + c * 128:d * 512 + (c + 1) * 128], h2c.ap[0:64, :], True, True,
                        [cw, h2c.r], [ps.r])
                j = d * 4 + c
                self.act(E.ap, tcb, AF.Exp, [cw], [E.r], scale=nd[:, j:j + 1])
                self.tt("dve", kc[d].ap[:, c, :], ps.ap[:, 0:CTX], E.ap, ALU.mult, [ps.r, E.r], [kc[d].r])
                src = kc[d].ap[:, c, :] if d == 0 else kc[d].ap[:, c, 1:CTX]
                self.P.op("dve", lambda e, o=l1.ap[:, j:j + 1], s=src: e.tensor_reduce(
                    out=o, in_=s, axis=AX.X, op=ALU.add, apply_absolute_value=True), [kc[d].r], [l1.r])
        self.tt("dve", l1.ap[:, 0:4], l1.ap[:, 0:4], l1.ap[:, 4:8], ALU.add, [l1.r], [l1.r])
        self.recip(l1.ap[:, 0:4], l1.ap[:, 0:4], [l1.r], [l1.r])
        for d in range(2):
            for c in range(4):
                self.ts("dve", kc[d].ap[:, c, :], kc[d].ap[:, c, :], l1.ap[:, c:c + 1], None, ALU.mult, None,
                        [l1.r, kc[d].r], [kc[d].r])
        for c in range(4):
            self.tt("dve", kc[0].ap[:, c, 0:1], kc[0].ap[:, c, 0:1], bc[:, c:c + 1], ALU.add, [kc[0].r, cw], [kc[0].r])
        self.tap("kc0", kc[0].ap, [kc[0].r])
        y = [Buf(A.f32(CTX), f"yc{c}") for c in range(4)]
        z = self.zcT
        for c in range(4):
            self.ts("dve", y[c].ap, z.ap[:, c, 1:CTX + 1], kc[0].ap[:, c, 0:1], None, ALU.mult, None,
                    [z.r, kc[0].r], [y[c].r])
        ptmp = [Buf(A.f32(CTX), f"ptmp{i}") for i in range(4)]
        pi = 0
        for dlag in range(1, CTX):
            m = CTX - dlag
            for c in range(4):
                if c < 4:
                    self.stt(y[c].ap[:, dlag:CTX], z.ap[:, c, 1:1 + m], kc[0].ap[:, c, dlag:dlag + 1], y[c].ap[:, dlag:CTX],
                             ALU.mult, ALU.add, [z.r, kc[0].r], [y[c].r])
                    self.stt(y[c].ap[:, 0:m], z.ap[:, c, 1 + dlag:CTX + 1], kc[1].ap[:, c, dlag:dlag + 1], y[c].ap[:, 0:m],
                             ALU.mult, ALU.add, [z.r, kc[1].r], [y[c].r])
                else:
                    for (ys, zs, kk) in ((y[c].ap[:, dlag:CTX], z.ap[:, c, 1:1 + m], kc[0]),
                                         (y[c].ap[:, 0:m], z.ap[:, c, 1 + dlag:CTX + 1], kc[1])):
                        pt_ = ptmp[pi % 4]
                        pi += 1
                        self.ts("pool", pt_.ap[:, 0:m], zs, kk.ap[:, c, dlag:dlag + 1], None, ALU.mult, None,
                                [z.r, kk.r], [pt_.r])
                        self.tt("pool", ys, ys, pt_.ap[:, 0:m], ALU.add, [pt_.r], [y[c].r])
        for c in range(4):
            self.tt("dve", self.ohycT.ap[:, c, :], y[c].ap, self.x0cT.ap[:, c, 1:CTX + 1], ALU.mult,
                    [y[c].r], [self.x0cT.r])
        self.tap("ohycT", self.ohycT.ap, [self.x0cT.r], BF16)
        P.barrier()
        A.release(m0)

    def phase_m(self):
        A, P = self.A, self.P
        sm = self.small_res
        wg_d = self.din("w_gate", [D, 2048])
        wba_d = self.din("w_br_attn", [512, D])
        wbh_d = self.din("w_br_hy", [512, D])
        wo_d = self.din("w_out", [D, D])
        NT = OWN + (CTX if self.L0 else 0)
        self.NT = NT
        self.xmid_d = self.nc.dram_tensor(f"xmid_scr{self.layer}", [D, NT], F32, kind="Internal").ap()
        self.xmid_v = self.xmid_d.rearrange("(k p) c -> p k c", p=128)
        self.xmid_res = Res("xmid")
        m0 = A.mark()
        wg = Buf(A.bf16(8 * 2048).rearrange("p (k c) -> p k c", k=8), "wg")
        wba = Buf(A.bf16(4 * D).rearrange("p (k c) -> p k c", k=4), "wba")
        wbh = Buf(A.bf16(4 * D).rearrange("p (k c) -> p k c", k=4), "wbh")
        wo = Buf(A.bf16(8 * D).rearrange("p (k c) -> p k c", k=8), "wo")
        self.loadw_bf16(wg.ap, wg_d, 8, wg.r)
        self.loadw_bf16(wba.ap, wba_d, 4, wba.r)
        self.loadw_bf16(wbh.ap, wbh_d, 4, wbh.r)
        self.loadw_bf16(wo.ap, wo_d, 8, wo.r)
        xb = Buf(A.f32(8 * TB).rearrange("p (k t) -> p k t", k=8), "xb")
        tmp = Buf(A.f32(8 * TB).rearrange("p (k t) -> p k t", k=8), "tmp")
        hT = Buf(A.bf16(8 * TB).rearrange("p (k t) -> p k t", k=8), "hT")
        rstd = Buf(A.f32(TB), "rstd")
        mg = Buf(A.bf16(8 * TB).rearrange("p (k t) -> p k t", k=8), "mg")
        ga = [Buf(A.f32(TB), f"ga{i}") for i in range(2)]
        gh = [Buf(A.f32(TB), f"gh{i}") for i in range(2)]
        blocks = [0, 1, 2, 3] + ([8] if self.L0 else [])
        for bi in blocks:
            src, n, col = self.xblock_src(bi)
            c0 = bi * TB if bi < 8 else OWN
            self.load("sp", xb.ap[:, :, 0:n], src, [xb.r])
            self.norm_mod(xb.ap[:, :, 0:n], n, self.gs1, self.sh1, col, hT.ap[:, :, 0:n], tmp.ap[:, :, 0:n],
                          rstd.ap[:, 0:n], xb.r, hT.r, tmp.r, rstd.r, self.ps[0])
            oa = self.oT.ap[:, :, c0:c0 + n]
            if bi < 8:
                oh = self.ohyT.ap[:, :, c0:c0 + n]
                oh_res = self.ohyT.r
            else:
                oh = self.ohycT.ap[:, :, 0:n]
                oh_res = self.ohycT.r
            for c in range(8):
                pga, pgh, pba, pbh = self.ps[(c % 2) * 4:(c % 2) * 4 + 4]
                for k in range(8):
                    self.mm(pga.ap[:, 0:n], wg.ap[:, k, c * 128:(c + 1) * 128], hT.ap[:, k, 0:n], k == 0, k == 7,
                            [wg.r, hT.r], [pga.r])
                for k in range(8):
                    self.mm(pgh.ap[:, 0:n], wg.ap[:, k, 1024 + c * 128:1024 + (c + 1) * 128], hT.ap[:, k, 0:n],
                            k == 0, k == 7, [wg.r, hT.r], [pgh.r])
                for k in range(4):
                    self.mm(pba.ap[:, 0:n], wba.ap[:, k, c * 128:(c + 1) * 128], oa[:, k, :], k == 0, k == 3,
                            [wba.r, self.oT.r], [pba.r])
                for k in range(4):
                    self.mm(pbh.ap[:, 0:n], wbh.ap[:, k, c * 128:(c + 1) * 128], oh[:, k, :], k == 0, k == 3,
                            [wbh.r, oh_res], [pbh.r])
                a_, h_ = ga[c % 2], gh[c % 2]
                self.act(a_.ap[:, 0:n], pga.ap[:, 0:n], AF.Sigmoid, [pga.r], [a_.r])
                self.act(h_.ap[:, 0:n], pgh.ap[:, 0:n], AF.Sigmoid, [pgh.r], [h_.r])
                self.tt("dve", a_.ap[:, 0:n], a_.ap[:, 0:n], pba.ap[:, 0:n], ALU.mult, [a_.r, pba.r], [a_.r])
                self.tt("dve", h_.ap[:, 0:n], h_.ap[:, 0:n], pbh.ap[:, 0:n], ALU.mult, [h_.r, pbh.r], [h_.r])
                self.tt("dve", mg.ap[:, c, 0:n], a_.ap[:, 0:n], h_.ap[:, 0:n], ALU.add, [a_.r, h_.r], [mg.r])
            if bi == 0:
                self.tap("mg0", mg.ap, [mg.r], BF16)
            for c in range(8):
                po = self.ps[c % 2]
                for k in range(8):
                    self.mm(po.ap[:, 0:n], wo.ap[:, k, c * 128:(c + 1) * 128], mg.ap[:, k, 0:n], k == 0, k == 7,
                            [wo.r, mg.r], [po.r])
                self.stt(xb.ap[:, c, 0:n], po.ap[:, 0:n], self.g1[:, c, col:col + 1], xb.ap[:, c, 0:n],
                         ALU.mult, ALU.add, [po.r, sm], [xb.r])
            self.P.dma("sp", self.xmid_v[:, :, c0:c0 + n], xb.ap[:, :, 0:n], reads=[xb.r], writes=[self.xmid_res])
        P.barrier()
        A.release(m0)

    def phase_ffn(self):
        A, P = self.A, self.P
        sm = self.small_res
        moe = not self.L0
        NT = self.NT
        nexp = NE if moe else 1
        if moe:
            wgd = self.din("moe_w_gate", [NE, D, DFF])
            wud = self.din("moe_w_up", [NE, D, DFF])
            wdd = self.din("moe_w_down", [NE, DFF, D])
            rt_d = self.din("moe_router", [128, 8, NE])
            fg_d = self.din("final_g", [128, 8])
        else:
            wgd = self.din("ffn_w_gate", [1, D, DFF])
            wud = self.din("ffn_w_up", [1, D, DFF])
            wdd = self.din("ffn_w_down", [1, DFF, D])
        fused0 = self.L0 and getattr(self, "fused", False)
        if not fused0:
            xout_d = self.dout("xout", [D, OWN])
            xout_v = xout_d.rearrange("(k p) c -> p k c", p=128)
        if self.L0 and not fused0:
            cout_d = self.dout("cout", [D, CTX])
            cout_v = cout_d.rearrange("(k p) c -> p k c", p=128)
        xT = Buf(A.f32(8 * NT).rearrange("p (k t) -> p k t", k=8), "xT", nres=5)
        h2T = Buf(A.bf16(8 * NT).rearrange("p (k t) -> p k t", k=8), "h2T", nres=5)
        blocks = [(i * TB, TB, 0) for i in range(4)] + ([(OWN, CTX, 1)] if self.L0 else [])
        if moe:
            cb = Buf(A.bf16(NE * OWN).rearrange("p (e t) -> p e t", e=NE), "cb", nres=4)
            rt = A.f32(8 * NE).rearrange("p (k e) -> p k e", k=8)
            fgc = A.f32(8)
            self.load("sp", rt, rt_d, [sm])
            self.load("sp", fgc, fg_d, [sm])
        m0 = A.mark()
        tmp = Buf(A.f32(8 * TB).rearrange("p (k t) -> p k t", k=8), "tmp")
        rstd = Buf(A.f32(TB), "rstd")
        if moe:
            hf = tmp
            lg = Buf(A.f32(8), "lg"); m8 = Buf(A.f32(8), "m8"); wv = Buf(A.f32(8), "wv"); nv1 = Buf(A.f32(2), "nv1")
            dg = Buf(A.f32(128), "dg")
        for bidx, (c0, n, col) in enumerate(blocks):
            self.load("sp", xT.ap[:, :, c0:c0 + n], self.xmid_v[:, :, c0:c0 + n], [xT.res[bidx]])
            self.norm_mod(xT.ap[:, :, c0:c0 + n], n, self.gs2, self.sh2, col, h2T.ap[:, :, c0:c0 + n], tmp.ap[:, :, 0:n],
                          rstd.ap[:, 0:n], xT.res[bidx], h2T.res[bidx], tmp.r, rstd.r, self.ps[0],
                          hf3=(hf.ap[:, :, 0:n] if moe else None), hf_res=(hf.r if moe else None))
            if bidx == 0:
                self.tap("h2T0", h2T.ap[:, :, 0:TB], [h2T.res[0]], BF16)
            if moe:
                for t in range(n // 128):
                    pl = self.ps[1 + (t % 2)]
                    for k in range(8):
                        self.mm(pl.ap[:, 0:NE], hf.ap[:, k, t * 128:(t + 1) * 128], rt[:, k, :], k == 0, k == 7,
                                [hf.r, sm], [pl.r])
                    self.cp("act", lg.ap, pl.ap[:, 0:NE], [pl.r], [lg.r])
                    self.P.op("dve", lambda e, o=m8.ap, i=lg.ap: e.max(out=o, in_=i), [lg.r], [m8.r])
                    self.ts("dve", wv.ap, lg.ap, m8.ap[:, 1:2], None, ALU.is_ge, None, [lg.r, m8.r], [wv.r])
                    self.ts("dve", nv1.ap[:, 0:1], m8.ap[:, 0:1], -1.0, None, ALU.mult, None, [m8.r], [nv1.r])
                    self.act(lg.ap, lg.ap, AF.Exp, [lg.r, nv1.r], [lg.r], bias=nv1.ap[:, 0:1])
                    self.tt("dve", wv.ap, wv.ap, lg.ap, ALU.mult, [wv.r, lg.r], [wv.r])
                    self.P.op("dve", lambda e, o=nv1.ap[:, 1:2], i=wv.ap: e.tensor_reduce(out=o, in_=i, axis=AX.X, op=ALU.add),
                              [wv.r], [nv1.r])
                    self.recip(nv1.ap[:, 1:2], nv1.ap[:, 1:2], [nv1.r], [nv1.r])
                    self.ts("dve", wv.ap, wv.ap, nv1.ap[:, 1:2], None, ALU.mult, None, [wv.r, nv1.r], [wv.r])
                    if bidx == 0 and t == 0:
                        self.tap("comb0", wv.ap, [wv.r])
                    for e_ in range(NE):
                        self.ts("dve", dg.ap, self.ident_f.ap, wv.ap[:, e_:e_ + 1], None, ALU.mult, None,
                                [wv.r, self.ident_f.r], [dg.r])
                        pc = self.ps[3 + (e_ % 4)]
                        self.mm(pc.ap[:, 0:128], self.ones_f.ap, dg.ap, True, True, [dg.r, self.ones_f.r], [pc.r])
                        self.cp("act", cb.ap[:, e_, c0 + t * 128:c0 + (t + 1) * 128], pc.ap[:, 0:128], [pc.r],
                                [cb.res[bidx]])
        P.barrier()
        A.release(m0)
        NG = DFF // 256
        W = []
        for i in range(2):
            W.append((Buf(A.bf16(8 * 256).rearrange("p (k c) -> p k c", k=8), f"wg{i}"),
                      Buf(A.bf16(8 * 256).rearrange("p (k c) -> p k c", k=8), f"wu{i}"),
                      Buf(A.bf16(2 * D).rearrange("p (k c) -> p k c", k=2), f"wd{i}")))
        sg = [Buf(A.bf16(TB), f"sg{i}") for i in range(2)]
        hid = [Buf(A.bf16(2 * TB).rearrange("p (j t) -> p j t", j=2), f"hid{i}") for i in range(2)]
        gi = 0
        for e_ in range(nexp):
            for g in range(NG):
                wg_, wu_, wd_ = W[gi % 2]
                f0 = g * 256
                for k in range(8):
                    self.P.dma("pool", wg_.ap[:, k, :], wgd[e_, k * 128:(k + 1) * 128, f0:f0 + 256], writes=[wg_.r])
                    self.P.dma("pool", wu_.ap[:, k, :], wud[e_, k * 128:(k + 1) * 128, f0:f0 + 256], writes=[wu_.r])
                for j in range(2):
                    self.P.dma("pool", wd_.ap[:, j, :], wdd[e_, f0 + j * 128:f0 + (j + 1) * 128, :], writes=[wd_.r])
                for bidx, (c0, n, col) in enumerate(blocks):
                    hd = hid[bidx % 2]
                    for j in range(2):
                        pg, pu = self.ps[2 * j], self.ps[2 * j + 1]
                        for k in range(8):
                            self.mm(pg.ap[:, 0:n], wg_.ap[:, k, j * 128:(j + 1) * 128], h2T.ap[:, k, c0:c0 + n],
                                    k == 0, k == 7, [wg_.r, h2T.res[bidx]], [pg.r])
                        for k in range(8):
                            self.mm(pu.ap[:, 0:n], wu_.ap[:, k, j * 128:(j + 1) * 128], h2T.ap[:, k, c0:c0 + n],
                                    k == 0, k == 7, [wu_.r, h2T.res[bidx]], [pu.r])
                        s_ = sg[j]
                        self.act(s_.ap[:, 0:n], pg.ap[:, 0:n], AF.Silu, [pg.r], [s_.r])
                        if moe:
                            self.tt("dve", s_.ap[:, 0:n], s_.ap[:, 0:n], cb.ap[:, e_, c0:c0 + n], ALU.mult,
                                    [s_.r, cb.res[bidx]], [s_.r])
                        self.tt("dve", hd.ap[:, j, 0:n], s_.ap[:, 0:n], pu.ap[:, 0:n], ALU.mult, [s_.r, pu.r], [hd.r])
                    for dc in range(8):
                        py = self.ps[4 + (dc % 4)]
                        for j in range(2):
                            self.mm(py.ap[:, 0:n], wd_.ap[:, j, dc * 128:(dc + 1) * 128], hd.ap[:, j, 0:n],
                                    j == 0, j == 1, [wd_.r, hd.r], [py.r])
                        self.stt(xT.ap[:, dc, c0:c0 + n], py.ap[:, 0:n], self.g2[:, dc, col:col + 1],
                                 xT.ap[:, dc, c0:c0 + n], ALU.mult, ALU.add, [py.r, sm], [xT.res[bidx]])
                gi += 1
        self.out_res = Res("out")
        if self.L0 and getattr(self, "fused", False):
            P.barrier()
            A.release(m0)
            sc = [Buf(A.f32(8 * TB).rearrange("p (k t) -> p k t", k=8), f"sc{i}") for i in range(2)]
            bv = self.bounce_d.rearrange("(s k p) c -> s p k c", s=2, p=128)
            for bidx, (c0, n, col) in enumerate(blocks):
                if col == 0:
                    self.P.dma("sp", self.x1T_v[:, :, c0:c0 + n], xT.ap[:, :, c0:c0 + n], reads=[xT.res[bidx]],
                               writes=[self.out_res])
                    for sl in range(2):
                        self.ts("dve", sc[sl].ap[:, :, 0:n], xT.ap[:, :, c0:c0 + n], self.smask[:, sl:sl + 1], None,
                                ALU.mult, None, [xT.res[bidx], sm], [sc[sl].r])
                        self.P.dma("sp", bv[sl][:, :, c0:c0 + n], sc[sl].ap[:, :, 0:n], reads=[sc[sl].r],
                                   writes=[self.out_res])
                else:
                    self.P.dma("sp", self.c1T_v, xT.ap[:, :, c0:c0 + n], reads=[xT.res[bidx]], writes=[self.out_res])
        elif self.L0:
            for bidx, (c0, n, col) in enumerate(blocks):
                if col == 0:
                    self.P.dma("sp", xout_v[:, :, c0:c0 + n], xT.ap[:, :, c0:c0 + n], reads=[xT.res[bidx]], writes=[self.out_res])
                else:
                    self.P.dma("sp", cout_v, xT.ap[:, :, c0:c0 + n], reads=[xT.res[bidx]], writes=[self.out_res])
        else:
            P.barrier()
            A.release(m0)
            tmp = Buf(A.f32(8 * TB).rearrange("p (k t) -> p k t", k=8), "ftmp")
            rstd = Buf(A.f32(TB), "frstd")
            for bidx, (c0, n, col) in enumerate(blocks):
                self.rms_bcast(xT.ap[:, :, c0:c0 + n], 8, n, D, rstd.ap[:, 0:n], tmp.ap[:, :, 0:n], xT.res[bidx], rstd.r,
                               tmp.r, self.ps[0])
                for k in range(8):
                    self.stt(tmp.ap[:, k, 0:n], xT.ap[:, k, c0:c0 + n], fgc[:, k:k + 1], rstd.ap[:, 0:n], ALU.mult, ALU.mult,
                             [xT.res[bidx], rstd.r, sm], [tmp.r])
                self.P.dma("sp", xout_v[:, :, c0:c0 + n], tmp.ap[:, :, 0:n], reads=[tmp.r], writes=[self.out_res])
        P.barrier()


def build_layer(layer, taps=()):
    K = LayerK2(layer, taps=taps)
    K.setup_consts()
    K.phase_mod()
    K.xT_d = K.din("xT", [D, SEQ])
    K.cxT_d = K.din("cxT", [D, CTX])
    K.xT_v = K.xT_d.rearrange("(k p) c -> p k c", p=128)
    K.cxT_v = K.cxT_d.rearrange("(k p) c -> p k c", p=128)
    K.phase_fg()
    K.phase_h()
    K.phase_f()
    if K.L0:
        K.phase_ctxhy()
    K.A.release(K.mark_hyw)
    K.phase_kvq()
    K.phase_att()
    K.A.release(K.mark_att)
    K.phase_m()
    K.A.release(K.mark_ffn)
    K.phase_ffn()
    nc = K.finish()
    return K, nc


class FusedK(LayerK2):
    def __init__(self, taps=()):
        super().__init__(0, taps=taps)
        self.fused = True

    def set_layer(self, l):
        self.layer = l
        self.L0 = l == 0
        self.last = l == 1

    def setup_exchange(self):
        nc = self.nc
        A = self.A
        self.bounce_d = nc.dram_tensor("xbounce", [2 * D, OWN], F32, kind="Internal").ap()
        self.gath_d = nc.dram_tensor("xgath", [2 * D, OWN], F32, kind="Internal").ap()
        self.x1T_d = nc.dram_tensor("x1T_scr", [D, SEQ], F32, kind="Internal").ap()
        self.c1T_d = nc.dram_tensor("c1T_scr", [D, CTX], F32, kind="Internal").ap()
        self.x1T_v = self.x1T_d.rearrange("(k p) c -> p k c", p=128)
        self.c1T_v = self.c1T_d.rearrange("(k p) c -> p k c", p=128)
        sm_d = self.din("smask", [128, 2])
        self.smask = A.f32(2)
        self.load("sp", self.smask, sm_d, [self.small_res_c])
        self.J = Buf(A.f32(128), "J")
        self.memset("pool", self.J.ap, 0.0, [self.J.r])
        self.P.op("pool", lambda e: e.affine_select(out=self.J.ap, in_=self.J.ap, pattern=[[1, 128]], base=-127,
                                                    channel_multiplier=1, compare_op=ALU.not_equal, fill=1.0),
                  [self.J.r], [self.J.r])

    def exchange_start(self):
        P = self.P
        self.gr = []
        nchunk = 8
        rr = (2 * D) // nchunk
        order = [0, 4, 1, 5, 2, 6, 3, 7]
        self.gr = {}
        for i in order:
            g = Res(f"gath{i}")
            self.gr[i] = g
            P.cc(lambda e, i=i: e.collective_compute(
                "AllReduce", ALU.add, replica_groups=[[0, 1], [2, 3], [4, 5], [6, 7]],
                ins=[self.bounce_d[i * rr:(i + 1) * rr, :].opt()], outs=[self.gath_d[i * rr:(i + 1) * rr, :].opt()]),
                reads=[self.out_res], writes=[g])

    def exchange(self):
        A, P = self.A, self.P
        m0 = A.mark()
        a = [Buf(A.f32(TB), f"xa{i}") for i in range(2)]
        b = [Buf(A.f32(TB), f"xb{i}") for i in range(2)]
        Tt = [Buf(A.f32(TB), f"xT{i}") for i in range(2)]
        o = [Buf(A.f32(TB), f"xo{i}") for i in range(2)]
        sres = self.small_res_c
        it = 0
        for k in range(8):
            for jb in range(4):
                i2 = it % 2
                it += 1
                self.P.dma("sp", a[i2].ap, self.gath_d[k * 128:(k + 1) * 128, jb * TB:(jb + 1) * TB],
                           reads=[self.gr[k // 2]], writes=[a[i2].r])
                self.P.dma("act", b[i2].ap, self.gath_d[D + k * 128:D + (k + 1) * 128, jb * TB:(jb + 1) * TB],
                           reads=[self.gr[4 + k // 2]], writes=[b[i2].r])
                self.ts("dve", a[i2].ap, a[i2].ap, self.smask[:, 1:2], None, ALU.mult, None, [sres], [a[i2].r])
                self.stt(a[i2].ap, b[i2].ap, self.smask[:, 0:1], a[i2].ap, ALU.mult, ALU.add, [b[i2].r, sres], [a[i2].r])
                pt, po = self.ps[2 * i2], self.ps[2 * i2 + 1]
                for q in range(4):
                    self.tr(pt.ap[:, q * 128:(q + 1) * 128], a[i2].ap[:, q * 128:(q + 1) * 128], self.ident_f.ap,
                            [a[i2].r, self.ident_f.r], [pt.r])
                self.cp("act", Tt[i2].ap, pt.ap, [pt.r], [Tt[i2].r])
                for q in range(4):
                    self.mm(po.ap[:, (3 - q) * 128:(4 - q) * 128], Tt[i2].ap[:, q * 128:(q + 1) * 128], self.J.ap,
                            True, True, [Tt[i2].r, self.J.r], [po.r], inc=(q == 3))
                self.cp("dve", o[i2].ap, po.ap, [po.r], [o[i2].r])
                self.P.dma("sp", self.x1T_d[k * 128:(k + 1) * 128, OWN + (3 - jb) * TB:OWN + (4 - jb) * TB], o[i2].ap,
                           reads=[o[i2].r], writes=[Res("x1w")])
        P.barrier(with_cc=True)
        A.release(m0)


def build_fused(taps=()):
    K = FusedK(taps=taps)
    K.setup_consts()
    K.small_res_c = Res("smallc")
    K.setup_exchange()
    mark_base = K.A.mark()
    for l in range(2):
        K.set_layer(l)
        K.A.release(mark_base)
        K.phase_mod()
        if l == 0:
            K.xT_d = K.din("xT", [D, SEQ])
            K.cxT_d = K.din("cxT", [D, CTX])
            K.xT_v = K.xT_d.rearrange("(k p) c -> p k c", p=128)
            K.cxT_v = K.cxT_d.rearrange("(k p) c -> p k c", p=128)
        else:
            K.xT_v = K.x1T_v
            K.cxT_v = K.c1T_v
        K.phase_fg()
        if l == 1:
            K.exchange()
        K.phase_h()
        K.phase_f()
        if K.L0:
            K.phase_ctxhy()
        K.A.release(K.mark_hyw)
        K.phase_kvq()
        K.phase_att()
        K.A.release(K.mark_att)
        K.phase_m()
        K.A.release(K.mark_ffn)
        K.phase_ffn()
        if l == 0:
            K.exchange_start()
    nc = K.finish()
    return K, nc

GRID_W = 64
KV_START = 384
HY_START = 384 + 256 + 32
GATE_START = HY_START + 1536


def chunked(vec, nchunk):
    return np.ascontiguousarray(vec.reshape(nchunk, 128).T)


def rope_tables(hf):
    n_freq = 8
    inv_freq = (10000.0 ** (-np.arange(n_freq, dtype=np.float32) / n_freq)).astype(np.float32)
    t = np.arange(SEQ)
    pos = t if hf == 0 else (SEQ - 1 - t)
    r = (pos // GRID_W).astype(np.float32)
    c = (pos % GRID_W).astype(np.float32)
    ang = np.concatenate([r[:, None] * inv_freq, c[:, None] * inv_freq], axis=-1).astype(np.float32)
    cos = np.cos(ang).astype(np.float32).T
    sin = np.sin(ang).astype(np.float32).T
    C = np.zeros((128, SEQ), np.float32)
    S = np.zeros((128, SEQ), np.float32)
    C[64:80] = cos; C[80:96] = cos
    S[64:80] = -sin; S[80:96] = sin
    return C, S


def layer_inputs(inp, l, b, hf, xT_b, cxT_b, names):
    out = {}
    f = np.float32
    w_in = inp["w_in"][l]

    def need(n):
        return n in names

    if need("xT"):
        out["xT"] = np.ascontiguousarray(xT_b if hf == 0 else xT_b[:, ::-1])
    if need("cxT"):
        out["cxT"] = np.ascontiguousarray(cxT_b)
    if need("cond"):
        out["cond"] = np.ascontiguousarray(np.stack([chunked(inp["c"][b], 8), chunked(inp["c_ctx"], 8)], axis=-1))
    if need("w_mod"):
        out["w_mod"] = np.ascontiguousarray(inp["w_mod"][l])
    if need("b_mod2"):
        bm = chunked(inp["b_mod"][l], 48)
        out["b_mod2"] = np.ascontiguousarray(np.stack([bm, bm], axis=-1))
    for nm, key in (("n1g2", "norm1_g"), ("n2g2", "norm2_g")):
        if need(nm):
            g = chunked(inp[key][l], 8)
            out[nm] = np.ascontiguousarray(np.stack([g, g], axis=-1))
    if need("w_kvq"):
        w = np.zeros((D, 7 * 128), f)
        w[:, 0:256] = w_in[:, KV_START:KV_START + 256]
        ra = w_in[:, KV_START + 256:KV_START + 272]
        rb = w_in[:, KV_START + 272:KV_START + 288]
        w[:, 256 + 64:256 + 80] = ra; w[:, 256 + 80:256 + 96] = rb
        w[:, 384 + 64:384 + 80] = rb; w[:, 384 + 80:384 + 96] = ra
        w[:, 512:896] = w_in[:, 0:384]
        out["w_kvq"] = w
    if need("g_kv"):
        out["g_kv"] = chunked(inp["kv_norm_g"][l], 2)
    if need("g_q"):
        out["g_q"] = chunked(inp["q_norm_g"][l], 3)
    if need("ropeC") or need("ropeS"):
        C, S = rope_tables(hf)
        out["ropeC"] = C; out["ropeS"] = S
    if need("w_uqA"):
        wq = inp["w_uq"][l].reshape(QL, NH, 96)
        A = wq.copy()
        B = np.zeros_like(wq)
        B[:, :, 64:80] = wq[:, :, 80:96]
        B[:, :, 80:96] = wq[:, :, 64:80]
        out["w_uqA"] = np.ascontiguousarray(A.reshape(QL, NH * 96))
        out["w_uqB"] = np.ascontiguousarray(B.reshape(QL, NH * 96))
    if need("w_uk"):
        out["w_uk"] = np.ascontiguousarray(inp["w_uk"][l])
    if need("w_uv"):
        out["w_uv"] = np.ascontiguousarray(inp["w_uv"][l])
    return out


def zfeat(n):
    bands = 8
    t = np.linspace(0.0, 1.0, n, dtype=np.float32)[:, None]
    phase = (np.float32(2.0 * math.pi / n) * np.arange(n, dtype=np.float32)[:, None]
             * np.linspace(1e-4, bands - 1, bands, dtype=np.float32)).astype(np.float32)
    z = np.concatenate([t, np.cos(phase), -np.sin(phase)], -1).astype(np.float32)
    return z, t[:, 0]


_FFT = {}


def fft_tables():
    if _FFT:
        return _FFT
    N = 8192
    n1 = np.arange(128)[:, None].astype(np.float64)
    f1 = (np.arange(128)[None, :] + 0.5)
    th = 2 * np.pi * n1 * f1 / 256
    _FFT["CS1a"] = np.concatenate([np.cos(th), -np.sin(th)], 1).astype(np.float32)
    th = 2 * np.pi * (n1 + 128) * f1 / 256
    _FFT["CS1b"] = np.concatenate([np.cos(th), -np.sin(th)], 1).astype(np.float32)
    p = np.arange(128)
    n2 = (p // 4)[:, None].astype(np.float64)
    th = 2 * np.pi * n2 * f1 / N
    Tr, Ti = np.cos(th), -np.sin(th)
    _FFT["T1"] = np.concatenate([Tr, Tr], 1).astype(np.float32)
    _FFT["T2"] = np.concatenate([Ti, Ti], 1).astype(np.float32)
    a = (p // 4); c = p % 4
    same = (c[:, None] == c[None, :]).astype(np.float64)
    th = 2 * np.pi * a[:, None] * a[None, :] / 32
    _FFT["W3r"] = (same * np.cos(th)).astype(np.float32)
    _FFT["W3i"] = (same * -np.sin(th)).astype(np.float32)
    _FFT["W3n"] = (same * np.sin(th)).astype(np.float32)
    n1p = np.arange(64)[None, :].astype(np.float64)
    f1c = (np.arange(128)[:, None] + 0.5)
    th = 2 * np.pi * n1p * f1c / 256
    _FFT["ICr"] = (2.0 / N * np.cos(th)).astype(np.float32)
    _FFT["ICi"] = (-2.0 / N * np.sin(th)).astype(np.float32)
    return _FFT


def hyena_inputs(inp, l, hf, names):
    out = {}
    f = np.float32
    w_in = inp["w_in"][l]
    if "zfT_f" in names:
        z, t = zfeat(SEQ)
        out["zfT_f"] = np.ascontiguousarray(z.T)
        idx = (SEQ - np.arange(SEQ)) % SEQ
        out["zfT_b"] = np.ascontiguousarray(z[idx].T)
        out["t_f"] = np.ascontiguousarray(t[None, :])
        out["t_b"] = np.ascontiguousarray(t[idx][None, :])
    if "hy_w1" in names:
        out["hy_w1"] = np.ascontiguousarray(inp["hy_w1"][l])
        out["hy_w2"] = np.ascontiguousarray(inp["hy_w2"][l])
        w3 = inp["hy_w3"][l].reshape(64, 2, HYW)
        dec = inp["hy_decay"][l]
        if hf == 1:
            w3 = w3[:, ::-1, :]
            dec = dec[::-1]
        out["hy_w3"] = np.ascontiguousarray(w3.reshape(64, 1024))
        out["hy_decay"] = np.ascontiguousarray(dec.reshape(1, 1024))
        out["hy_fq"] = np.ascontiguousarray(np.stack([inp["hy_freq"][l], inp["hy_b1"][l], inp["hy_b2"][l]], -1))
        out["hy_bias"] = np.ascontiguousarray(inp["hy_bias"][l][None, :])
    if "w_hy" in names:
        out["w_hy"] = np.ascontiguousarray(w_in[:, HY_START:GATE_START])
        sw = inp["hy_short_w"][l]
        if hf == 1:
            sw = sw[::-1]
        sb = inp["hy_short_b"][l]
        arr = np.concatenate([sw, sb[None]], 0)
        out["hy_sw"] = np.ascontiguousarray(arr.reshape(4, 12, 128).transpose(2, 1, 0))
    T = fft_tables()
    for k in T:
        if k in names:
            out[k] = T[k]
    return out


def rest_inputs(inp, l, hf, names):
    out = {}
    w_in = inp["w_in"][l]
    if "zfT_c" in names:
        z, t = zfeat(CTX)
        out["zfT_c"] = np.ascontiguousarray(z.T)
        out["tc_bc"] = np.ascontiguousarray(np.broadcast_to(t[None, :], (128, CTX)))
        out["hyc_w3"] = np.ascontiguousarray(inp["hy_w3"][l])
        dec = inp["hy_decay"][l]
        out["hyc_dec"] = np.ascontiguousarray(dec.reshape(2, 4, 128).transpose(2, 0, 1).reshape(128, 8))
        out["hyc_bias"] = chunked(inp["hy_bias"][l], 4)
    if "hy_swc" in names:
        sw = inp["hy_short_w"][l]
        sb = inp["hy_short_b"][l]
        arr = np.concatenate([sw, sb[None]], 0)
        out["hy_swc"] = np.ascontiguousarray(arr.reshape(4, 12, 128).transpose(2, 1, 0))
    if "w_gate" in names:
        out["w_gate"] = np.ascontiguousarray(w_in[:, GATE_START:])
        out["w_br_attn"] = np.ascontiguousarray(inp["w_br_attn"][l])
        out["w_br_hy"] = np.ascontiguousarray(inp["w_br_hy"][l])
        out["w_out"] = np.ascontiguousarray(inp["w_out"][l])
    i = l // 2
    if "ffn_w_gate" in names:
        out["ffn_w_gate"] = inp["ffn_w_gate"][i:i + 1]
        out["ffn_w_up"] = inp["ffn_w_up"][i:i + 1]
        out["ffn_w_down"] = inp["ffn_w_down"][i:i + 1]
    if "moe_w_gate" in names:
        out["moe_w_gate"] = inp["moe_w_gate"][i]
        out["moe_w_up"] = inp["moe_w_up"][i]
        out["moe_w_down"] = inp["moe_w_down"][i]
        out["moe_router"] = np.ascontiguousarray(inp["moe_router"][i].reshape(8, 128, NE).transpose(1, 0, 2))
        out["final_g"] = chunked(inp["final_g"], 8)
    return out


def all_inputs(inp, l, b, hf, xT_b, cxT_b, names):
    im = layer_inputs(inp, l, b, hf, xT_b, cxT_b, names)
    im.update(hyena_inputs(inp, l, hf, names))
    im.update(rest_inputs(inp, l, hf, names))
    return im


_FCACHE = {}


def _get_fused():
    if "k" not in _FCACHE:
        _FCACHE["k"] = build_fused()
    return _FCACHE["k"]


def kernel(**inputs):
    inp = {k: np.asarray(v, dtype=np.float32) for k, v in inputs.items()}
    B = inp["x"].shape[0]
    K, nc = _get_fused()
    all_names = set(K.in_shapes)
    shared = {n for n in all_names if not (n.startswith("L0_") or n.startswith("L1_"))}
    lnames = {l: {n[3:] for n in all_names if n.startswith(f"L{l}_")} for l in range(2)}
    xT = [np.ascontiguousarray(inp["x"][b].T) for b in range(B)]
    cxT = [np.ascontiguousarray(inp["ctx"][b].T) for b in range(B)]
    in_maps = []
    for core in range(8):
        b, hf = core // 2, core % 2
        m = {}
        im = all_inputs(inp, 0, b, hf, xT[b], cxT[b], shared | lnames[0])
        for n in shared:
            if n == "smask":
                sm = np.zeros((128, 2), np.float32)
                sm[:, hf] = 1.0
                m[n] = sm
            else:
                m[n] = im[n]
        for n in lnames[0]:
            m["L0_" + n] = im[n]
        im1 = all_inputs(inp, 1, b, hf, xT[b], cxT[b], lnames[1])
        for n in lnames[1]:
            m["L1_" + n] = im1[n]
        for k, s in K.in_shapes.items():
            assert m[k].shape == s, (k, m[k].shape, s)
            m[k] = np.ascontiguousarray(m[k], dtype=np.float32)
        in_maps.append(m)
    res = run_bass_kernel_spmd(nc, in_maps, core_ids=list(range(8)))
    r1 = res.results
    out = np.empty((B, SEQ, D), np.float32)
    for b in range(B):
        lo = np.asarray(r1[2 * b]["xout"])
        hi = np.asarray(r1[2 * b + 1]["xout"])[:, ::-1]
        out[b] = np.concatenate([lo, hi], axis=1).T
    return out
```

```python
from concourse.bass_utils import run_bass_kernel_spmd
import math
import numpy as np
from contextlib import ExitStack
import concourse.bass as bass
import concourse.mybir as mybir

F32 = mybir.dt.float32
BF16 = mybir.dt.bfloat16
AF = mybir.ActivationFunctionType
ALU = mybir.AluOpType
AX = mybir.AxisListType

import os
ENGS = ("pe", "act", "dve", "pool", "sp")
SKIP_SAME = set(os.environ.get("MK_SKIP_SAME", "").split(",")) - {""}
NDMA = 8

D = 1024
SEQ = 4096
CTX = 256
OWN = 2048
NH = 8
QL = 384
KVL = 256
ROPE = 32
HYW = 512
DFF = 2816
NE = 8
EPS = 1e-6
ATT_SCALE = (64 + 32) ** -0.5
TB = 512
ARENA_WORDS = 44500


class Res:
    __slots__ = ("name", "w", "r")

    def __init__(self, name="r"):
        self.name = name
        self.w = None
        self.r = {}


class Prog:
    def __init__(self, nc):
        self.nc = nc
        self.q = {k: [] for k in ENGS}
        self.cnt = {}
        self.known = {k: {} for k in ENGS}
        self.semnames = list(ENGS) + ["ccsem"]
        for q in ("sp", "pool", "act"):
            for i in range(NDMA):
                self.semnames.append(f"d_{q}_{i}")
        for s in self.semnames:
            self.cnt[s] = 0
        self.dma_rr = {"sp": 0, "pool": 0, "act": 0}
        self.sems = {}
        self.n_ops = 0

    def _need(self, eng, deps):
        best = {}
        for (s, t) in deps:
            if t > best.get(s, 0):
                best[s] = t
        for s, t in best.items():
            if eng == "pe" and s == "pe":
                continue
            if s == eng and eng in SKIP_SAME:
                continue
            if self.known[eng].get(s, 0) >= t:
                continue
            self.known[eng][s] = t
            self.q[eng].append(("wait", s, t))

    def _collect(self, reads, writes):
        deps = []
        assert not isinstance(reads, Res) and not isinstance(writes, Res)
        for r in reads:
            if r.w is not None:
                deps.append(r.w)
        for w in writes:
            if w.w is not None:
                deps.append(w.w)
            for s, t in w.r.items():
                deps.append((s, t))
        return deps

    def _mark(self, semkey, ticket, reads, writes):
        for r in reads:
            if r.r.get(semkey, 0) < ticket:
                r.r[semkey] = ticket
        for w in writes:
            w.w = (semkey, ticket)
            w.r = {}

    def op(self, eng, fn, reads=(), writes=(), inc=True):
        self.n_ops += 1
        self._need(eng, self._collect(reads, writes))
        ticket = self.cnt[eng] + 1
        if inc:
            self.cnt[eng] = ticket
        self.q[eng].append(("op", fn, inc))
        self._mark(eng, ticket, reads, writes)

    def dma(self, q, out, in_, reads=(), writes=(), **kw):
        self.n_ops += 1
        i = self.dma_rr[q]
        self.dma_rr[q] = (i + 1) % NDMA
        s = f"d_{q}_{i}"
        deps = self._collect(reads, writes)
        if self.cnt[s] > 0:
            deps.append((s, self.cnt[s]))
        self._need(q, deps)
        ticket = self.cnt[s] + 16
        self.cnt[s] = ticket
        self.q[q].append(("dma", out, in_, s, kw))
        self._mark(s, ticket, reads, writes)

    def cc(self, fn, reads=(), writes=()):
        q = "pool"
        s = "ccsem"
        deps = self._collect(reads, writes)
        if self.cnt[s] > 0:
            deps.append((s, self.cnt[s]))
        self._need(q, deps)
        ticket = self.cnt[s] + 1
        self.cnt[s] = ticket
        self.q[q].append(("cc", fn, s))
        self._mark(s, ticket, reads, writes)

    def barrier(self, with_cc=False):
        for e in ENGS:
            deps = [(s, c) for s, c in self.cnt.items() if c > 0 and (s != "ccsem" or with_cc)]
            self._need(e, deps)

    def emit(self, block):
        sems = self.sems

        def run(eng_name, engine):
            for item in self.q[eng_name]:
                if item[0] == "wait":
                    engine.wait_ge(sems[item[1]], item[2])
                elif item[0] == "op":
                    ins = item[1](engine)
                    if item[2]:
                        ins.then_inc(sems[eng_name], 1)
                elif item[0] == "cc":
                    item[1](engine).then_inc(sems[item[2]])
                else:
                    _, out, in_, s, kw = item
                    engine.dma_start(out=out, in_=in_, **kw).then_inc(sems[s], 16)

        @block.sync
        def _(e):
            run("sp", e)

        @block.scalar
        def _(e):
            run("act", e)

        @block.vector
        def _(e):
            run("dve", e)

        @block.gpsimd
        def _(e):
            run("pool", e)

        @block.tensor
        def _(e):
            run("pe", e)


class Arena:
    def __init__(self, t):
        self.t = t
        self.top = 0
        self.hi = 0

    def mark(self):
        return self.top

    def release(self, m):
        self.top = m

    def f32(self, words, shape=None):
        a = self.t[:, self.top:self.top + words]
        self.top += words
        self.hi = max(self.hi, self.top)
        assert self.top <= ARENA_WORDS, f"arena overflow {self.top}"
        return a

    def bf16(self, elems):
        words = (elems + 1) // 2
        a = self.t[:, self.top:self.top + words].bitcast(BF16)
        self.top += words
        self.hi = max(self.hi, self.top)
        assert self.top <= ARENA_WORDS, f"arena overflow {self.top}"
        return a


class Buf:
    def __init__(self, ap, name="b", nres=1):
        self.ap = ap
        self.res = [Res(f"{name}{i}") for i in range(nres)]

    @property
    def r(self):
        return self.res[0]


def _bind(f, *a, **k):
    return lambda e: f(e, *a, **k)


SHARED_INPUTS = {"ropeC", "ropeS", "zfT_f", "zfT_b", "t_f", "t_b", "CS1a", "CS1b", "W3r", "W3i", "W3n", "T1", "T2",
                 "ICr", "ICi", "zfT_c", "tc_bc", "cond", "xT", "cxT", "smask"}


class LayerK:
    def __init__(self, layer, taps=(), upto=None):
        self.layer = layer
        self.L0 = layer == 0
        self.last = layer == 1
        self.taps = set(taps)
        self.upto = upto
        self.nc = bass.Bass("TRN2", target_bir_lowering=False)
        self.es = ExitStack()
        self.in_shapes = {}
        self.out_shapes = {}
        nc = self.nc
        self.arena_t = self.es.enter_context(nc.sbuf_tensor("arena", [128, ARENA_WORDS], F32))
        self.A = Arena(self.arena_t)
        self.P = Prog(nc)
        for s in self.P.semnames:
            self.P.sems[s] = self.es.enter_context(nc.semaphore(s))
        self.ps = []
        for i in range(8):
            t = self.es.enter_context(nc.psum_tensor(f"ps{i}", [128, 512], F32))
            self.ps.append(Buf(t[:, :], f"ps{i}"))
        self.tapq = 0

    def din(self, name, shape, dt=F32):
        if not hasattr(self, "_in_aps"):
            self._in_aps = {}
        key = name if (name in SHARED_INPUTS or not getattr(self, "fused", False)) else f"L{self.layer}_{name}"
        if key in self._in_aps:
            assert self.in_shapes[key] == tuple(shape), (key, shape)
            self._in_aps[name] = self._in_aps[key]
            return self._in_aps[key]
        t = self.nc.dram_tensor(key, list(shape), dt, kind="ExternalInput").ap()
        self.in_shapes[key] = tuple(shape)
        self._in_aps[key] = t
        self._in_aps[name] = t
        return t

    def dout(self, name, shape, dt=F32):
        t = self.nc.dram_tensor(name, list(shape), dt, kind="ExternalOutput").ap()
        self.out_shapes[name] = tuple(shape)
        return t

    def tap(self, name, ap, res, dt=F32):
        if name not in self.taps:
            return
        shape = list(ap.shape)
        d = self.dout("tap_" + name, shape, dt)
        self.P.dma("sp", d, ap, reads=res)

    def mm(self, out, lhsT, rhs, start, stop, reads, writes, inc=None):
        if inc is None:
            inc = stop
        self.P.op("pe", lambda e: e.matmul(out, lhsT=lhsT, rhs=rhs, start=start, stop=stop),
                  reads, writes, inc)

    def tr(self, out, in_, ident, reads, writes):
        self.P.op("pe", lambda e: e.transpose(out, in_, ident), reads, writes, True)

    def act(self, out, in_, func, reads, writes, scale=1.0, bias=0.0):
        self.P.op("act", lambda e: e.activation(out=out, in_=in_, func=func, bias=bias, scale=scale),
                  reads, writes)

    def tt(self, eng, out, in0, in1, op, reads, writes):
        self.P.op(eng, lambda e: e.tensor_tensor(out=out, in0=in0, in1=in1, op=op), reads, writes)

    def ts(self, eng, out, in0, s1, s2, op0, op1, reads, writes):
        if op1 is None:
            self.P.op(eng, lambda e: e.tensor_scalar(out=out, in0=in0, scalar1=s1, scalar2=None, op0=op0),
                      reads, writes)
        else:
            self.P.op(eng, lambda e: e.tensor_scalar(out=out, in0=in0, scalar1=s1, scalar2=s2, op0=op0, op1=op1),
                      reads, writes)

    def stt(self, out, in0, scalar, in1, op0, op1, reads, writes):
        self.P.op("dve", lambda e: e.scalar_tensor_tensor(out=out, in0=in0, scalar=scalar, in1=in1, op0=op0, op1=op1),
                  reads, writes)

    def cp(self, eng, out, in_, reads, writes):
        if eng == "act":
            self.P.op("act", lambda e: e.copy(out=out, in_=in_), reads, writes)
        else:
            self.P.op(eng, lambda e: e.tensor_copy(out=out, in_=in_), reads, writes)

    def memset(self, eng, ap, val, writes):
        self.P.op(eng, lambda e: e.memset(ap, val), (), writes)

    def recip(self, out, in_, reads, writes):
        self.P.op("dve", lambda e: e.reciprocal(out=out, in_=in_), reads, writes)

    def load(self, q, out, in_, writes, **kw):
        self.P.dma(q, out, in_, writes=writes, **kw)

    def loadw_bf16(self, dst3, src2, kchunks, res):
        for k in range(kchunks):
            self.P.dma("pool", dst3[:, k, :], src2[k * 128:(k + 1) * 128, :], writes=[res])

    def setup_consts(self):
        A = self.A
        self.ones_f = Buf(A.f32(128), "ones")
        self.memset("pool", self.ones_f.ap, 1.0, [self.ones_f.r])
        self.ident_f = Buf(A.f32(128), "identf")
        self.memset("pool", self.ident_f.ap, 0.0, [self.ident_f.r])
        self.P.op("pool", lambda e: e.affine_select(out=self.ident_f.ap, in_=self.ident_f.ap, pattern=[[-1, 128]],
                                                    base=0, channel_multiplier=1, compare_op=ALU.not_equal, fill=1.0),
                  [self.ident_f.r], [self.ident_f.r])
        self.ident_b = Buf(A.bf16(128), "identb")
        self.cp("dve", self.ident_b.ap, self.ident_f.ap, [self.ident_f.r], [self.ident_b.r])

    def rms_bcast(self, src3, C, n, dim, rstd, sq3, src_res, rstd_res, sq_res, psb):
        self.act(sq3, src3, AF.Square, [src_res], [sq_res])
        ss = rstd
        self.P.op("dve", lambda e: e.tensor_reduce(out=ss, in_=sq3.rearrange("p c t -> p t c"), axis=AX.X, op=ALU.add),
                  [sq_res], [rstd_res])
        self.mm(psb.ap[:, 0:n], self.ones_f.ap, ss, True, True, [rstd_res, self.ones_f.r], [psb.r])
        self.act(rstd, psb.ap[:, 0:n], AF.Sqrt, [psb.r], [rstd_res], scale=1.0 / dim, bias=EPS)
        self.recip(rstd, rstd, [rstd_res], [rstd_res])

    def norm_mod(self, xb3, n, gs, sh, col, hT3, tmp3, rstd, xb_res, h_res, tmp_res, rstd_res, psb, hf3=None, hf_res=None):
        self.rms_bcast(xb3, 8, n, D, rstd, tmp3, xb_res, rstd_res, tmp_res, psb)
        for k in range(8):
            self.stt(tmp3[:, k, :], xb3[:, k, :], gs[:, k, col:col + 1], rstd, ALU.mult, ALU.mult,
                     [xb_res, rstd_res, self.small_res], [tmp_res])
        for k in range(8):
            self.act(hT3[:, k, :], tmp3[:, k, :], AF.Identity, [tmp_res, self.small_res], [h_res],
                     bias=sh[:, k, col:col + 1])
            if hf3 is not None:
                self.act(hf3[:, k, :], tmp3[:, k, :], AF.Identity, [tmp_res, self.small_res], [hf_res],
                         bias=sh[:, k, col:col + 1])

    def phase_mod(self):
        A, P = self.A, self.P
        l = self.layer
        self.small_res = Res("small")
        sm = self.small_res
        cond_d = self.din("cond", [128, 8, 2])
        wmod_d = self.din("w_mod", [D, 6 * D])
        bmod_d = self.din("b_mod2", [128, 48, 2])
        n1g_d = self.din("n1g2", [128, 8, 2])
        n2g_d = self.din("n2g2", [128, 8, 2])
        self.mod = A.f32(96).rearrange("p (j c) -> p j c", c=2)
        self.gs1 = A.f32(16).rearrange("p (j c) -> p j c", c=2)
        self.gs2 = A.f32(16).rearrange("p (j c) -> p j c", c=2)
        bmod = A.f32(96).rearrange("p (j c) -> p j c", c=2)
        n1g = A.f32(16).rearrange("p (j c) -> p j c", c=2)
        n2g = A.f32(16).rearrange("p (j c) -> p j c", c=2)
        scond = A.f32(16).rearrange("p (j c) -> p j c", c=2)
        m0 = A.mark()
        sc_res = Res("scond")
        self.load("sp", scond, cond_d, [sc_res])
        self.load("sp", bmod, bmod_d, [sm])
        self.load("sp", n1g, n1g_d, [sm])
        self.load("sp", n2g, n2g_d, [sm])
        self.act(scond, scond, AF.Silu, [sc_res], [sc_res])
        wb = [Buf(A.f32(8 * 512).rearrange("p (k c) -> p k c", k=8), f"wm{i}") for i in range(2)]
        psb = self.ps[0]
        wview = wmod_d.rearrange("(k p) c -> p k c", p=128)
        for blk in range(12):
            w = wb[blk % 2]
            self.load("sp" if blk % 2 == 0 else "act", w.ap, wview[:, :, blk * 512:(blk + 1) * 512], [w.r])
            for jj in range(4):
                j = blk * 4 + jj
                for k in range(8):
                    self.mm(psb.ap[:, 2 * j:2 * j + 2], w.ap[:, k, jj * 128:(jj + 1) * 128], scond[:, k, :],
                            k == 0, k == 7, [w.r, sc_res], [psb.r])
        self.tt("dve", self.mod, psb.ap[:, 0:96].rearrange("p (j c) -> p j c", c=2), bmod, ALU.add, [psb.r, sm], [sm])
        self.ts("dve", self.gs1, self.mod[:, 8:16, :], 1.0, None, ALU.add, None, [sm], [sm])
        self.tt("dve", self.gs1, self.gs1, n1g, ALU.mult, [sm], [sm])
        self.ts("dve", self.gs2, self.mod[:, 32:40, :], 1.0, None, ALU.add, None, [sm], [sm])
        self.tt("dve", self.gs2, self.gs2, n2g, ALU.mult, [sm], [sm])
        self.sh1 = self.mod[:, 0:8, :]
        self.g1 = self.mod[:, 16:24, :]
        self.sh2 = self.mod[:, 24:32, :]
        self.g2 = self.mod[:, 40:48, :]
        self.tap("mod", self.mod, [sm])
        self.tap("gs1", self.gs1, [sm])
        P.barrier()
        A.release(m0)
        self.mark_ffn = A.mark()

    def xblock_src(self, bi):
        if bi < 8:
            return self.xT_v[:, :, bi * TB:(bi + 1) * TB], TB, 0
        return self.cxT_v, CTX, 1

    def phase_kvq(self):
        A, P = self.A, self.P
        wkvq_d = self.din("w_kvq", [D, 7 * 128])
        gkv_d = self.din("g_kv", [128, 2])
        gq_d = self.din("g_q", [128, 3])
        ropeC_d = self.din("ropeC", [128, SEQ])
        ropeS_d = self.din("ropeS", [128, SEQ])
        NK = CTX + SEQ
        self.NK = NK
        nq = OWN + (CTX if self.L0 else 0)
        self.nq = nq
        nq = OWN + (CTX if self.L0 else 0)
        self.nq = nq
        self.oT = Buf(A.bf16(4 * nq).rearrange("p (k t) -> p k t", k=4), "oT")
        self.mark_att = A.mark()
        self.ckvT = Buf(A.bf16(2 * NK).rearrange("p (k t) -> p k t", k=2), "ckvT")
        self.kT = [Buf(A.bf16(NK), f"kT{i}") for i in range(2)]
        self.qnT = Buf(A.bf16(3 * nq).rearrange("p (k t) -> p k t", k=3), "qnT")
        gkv = A.f32(2)
        gq = A.f32(3)
        m0 = A.mark()
        self.load("sp", gkv, gkv_d, [self.small_res])
        self.load("sp", gq, gq_d, [self.small_res])
        w = Buf(A.bf16(8 * 896).rearrange("p (k c) -> p k c", k=8), "wkvq")
        self.loadw_bf16(w.ap, wkvq_d, 8, w.r)
        xb = [Buf(A.f32(8 * TB).rearrange("p (k t) -> p k t", k=8), f"xb{i}") for i in range(1)]
        tmp = Buf(A.f32(8 * TB).rearrange("p (k t) -> p k t", k=8), "tmp")
        hTs = [Buf(A.bf16(8 * TB).rearrange("p (k t) -> p k t", k=8), f"hT{i}") for i in range(2)]
        rstd = Buf(A.f32(TB), "rstd")
        pp = Buf(A.f32(3 * TB).rearrange("p (k t) -> p k t", k=3), "pp")
        sq = Buf(A.f32(3 * TB).rearrange("p (k t) -> p k t", k=3), "sq")
        rs2 = Buf(A.f32(TB), "rs2")
        tabC = Buf(A.f32(TB), "tabC")
        tabS = Buf(A.f32(TB), "tabS")
        t1 = Buf(sq.ap[:, 0, :], "t1")
        t2 = Buf(sq.ap[:, 1, :], "t2")
        t1.res = sq.res
        t2.res = sq.res
        sm = self.small_res
        order = [8] + list(range(8))

        def prep(it_):
            bi_ = order[it_]
            src_, n_, col_ = self.xblock_src(bi_)
            hT_ = hTs[it_ % 2]
            x_ = xb[0]
            self.load("sp", x_.ap[:, :, 0:n_], src_, [x_.r])
            self.norm_mod(x_.ap[:, :, 0:n_], n_, self.gs1, self.sh1, col_, hT_.ap[:, :, 0:n_], tmp.ap[:, :, 0:n_],
                          rstd.ap[:, 0:n_], x_.r, hT_.r, tmp.r, rstd.r, self.ps[0])

        prep(0)
        for it, bi in enumerate(order):
            src, n, col = self.xblock_src(bi)
            hT = hTs[it % 2]
            if it + 1 < len(order):
                prep(it + 1)
            if bi == 0:
                self.tap("hT0", hT.ap, [hT.r], BF16)
            koff = 0 if bi == 8 else CTX + bi * TB
            for g in range(2):
                psb = self.ps[1 + g]
                for k in range(8):
                    self.mm(psb.ap[:, 0:n], w.ap[:, k, g * 128:(g + 1) * 128], hT.ap[:, k, 0:n], k == 0, k == 7,
                            [w.r, hT.r], [psb.r])
                self.cp("act", pp.ap[:, g, 0:n], psb.ap[:, 0:n], [psb.r], [pp.r])
            self.rms_bcast(pp.ap[:, 0:2, 0:n], 2, n, KVL, rs2.ap[:, 0:n], sq.ap[:, 0:2, 0:n], pp.r, rs2.r, sq.r, self.ps[3])
            for g in range(2):
                self.stt(self.ckvT.ap[:, g, koff:koff + n], pp.ap[:, g, 0:n], gkv[:, g:g + 1], rs2.ap[:, 0:n],
                         ALU.mult, ALU.mult, [pp.r, rs2.r, sm], [self.ckvT.r])
            psA, psB = self.ps[4], self.ps[5]
            for k in range(8):
                self.mm(psA.ap[:, 0:n], w.ap[:, k, 256:384], hT.ap[:, k, 0:n], k == 0, k == 7, [w.r, hT.r], [psA.r])
            if bi == 8:
                self.cp("act", self.kT[0].ap[64:96, koff:koff + n], psA.ap[64:96, 0:n], [psA.r], [self.kT[0].r])
            else:
                for k in range(8):
                    self.mm(psB.ap[:, 0:n], w.ap[:, k, 384:512], hT.ap[:, k, 0:n], k == 0, k == 7, [w.r, hT.r], [psB.r])
                self.load("sp", tabC.ap[64:96, 0:n], ropeC_d[64:96, bi * TB:bi * TB + n], [tabC.r])
                self.load("sp", tabS.ap[64:96, 0:n], ropeS_d[64:96, bi * TB:bi * TB + n], [tabS.r])
                self.tt("dve", t1.ap[64:96, 0:n], psA.ap[64:96, 0:n], tabC.ap[64:96, 0:n], ALU.mult, [psA.r, tabC.r], [t1.r])
                self.tt("dve", t2.ap[64:96, 0:n], psB.ap[64:96, 0:n], tabS.ap[64:96, 0:n], ALU.mult, [psB.r, tabS.r], [t2.r])
                self.tt("dve", self.kT[0].ap[64:96, koff:koff + n], t1.ap[64:96, 0:n], t2.ap[64:96, 0:n], ALU.add,
                        [t1.r, t2.r], [self.kT[0].r])
            self.cp("act", self.kT[1].ap[64:96, koff:koff + n], self.kT[0].ap[64:96, koff:koff + n],
                    [self.kT[0].r], [self.kT[1].r])
            isq = (bi < 4) or (bi == 8 and self.L0)
            if isq:
                qoff = bi * TB if bi < 4 else OWN
                for g in range(3):
                    psb = self.ps[6 + (g % 2)]
                    for k in range(8):
                        self.mm(psb.ap[:, 0:n], w.ap[:, k, 512 + g * 128:512 + (g + 1) * 128], hT.ap[:, k, 0:n],
                                k == 0, k == 7, [w.r, hT.r], [psb.r])
                    self.cp("act", pp.ap[:, g, 0:n], psb.ap[:, 0:n], [psb.r], [pp.r])
                self.rms_bcast(pp.ap[:, 0:3, 0:n], 3, n, QL, rs2.ap[:, 0:n], sq.ap[:, 0:3, 0:n], pp.r, rs2.r, sq.r, self.ps[3])
                for g in range(3):
                    self.stt(self.qnT.ap[:, g, qoff:qoff + n], pp.ap[:, g, 0:n], gq[:, g:g + 1], rs2.ap[:, 0:n],
                             ALU.mult, ALU.mult, [pp.r, rs2.r, sm], [self.qnT.r])
        self.tap("ckvT", self.ckvT.ap, [self.ckvT.r], BF16)
        self.tap("kT", self.kT[0].ap[64:96, :], [self.kT[0].r], BF16)
        self.tap("qnT", self.qnT.ap, [self.qnT.r], BF16)
        P.barrier()
        A.release(m0)

    def phase_att(self):
        A, P = self.A, self.P
        NK, nq = self.NK, self.nq
        wuqA_d = self.din("w_uqA", [QL, NH * 96])
        wuqB_d = self.din("w_uqB", [QL, NH * 96])
        wuk_d = self.din("w_uk", [KVL, 512])
        wuv_d = self.din("w_uv", [KVL, 512])
        ropeC_d = self.in_ap("ropeC")
        ropeS_d = self.in_ap("ropeS")
        m0 = A.mark()
        wA = Buf(A.bf16(3 * 768).rearrange("p (k c) -> p k c", k=3), "wA")
        wB = Buf(A.bf16(3 * 768).rearrange("p (k c) -> p k c", k=3), "wB")
        wk = Buf(A.bf16(2 * 512).rearrange("p (k c) -> p k c", k=2), "wk")
        wv = Buf(A.bf16(2 * 512).rearrange("p (k c) -> p k c", k=2), "wv")
        self.loadw_bf16(wA.ap, wuqA_d, 3, wA.r)
        self.loadw_bf16(wB.ap, wuqB_d, 3, wB.r)
        self.loadw_bf16(wk.ap, wuk_d, 2, wk.r)
        self.loadw_bf16(wv.ap, wuv_d, 2, wv.r)
        tabC = Buf(A.f32(TB), "qtabC")
        tabS = Buf(A.f32(TB), "qtabS")
        qh = [Buf(A.bf16(nq), f"qh{i}") for i in range(2)]
        NKC = NK // 128
        vT = [Buf(A.bf16(NKC * 128).rearrange("p (c m) -> p c m", m=128), f"vT{i}") for i in range(2)]
        self.memset("pool", vT[0].ap[:, :, 64:128], 1.0, [vT[0].r])
        self.memset("pool", vT[1].ap[:, :, 0:64], 1.0, [vT[1].r])
        Pt = [Buf(A.bf16(TB), f"Pt{i}") for i in range(4)]
        t1 = Buf(A.f32(TB), "at1")
        t2 = Buf(A.f32(TB), "at2")
        rec = Buf(A.f32(TB), "rec")
        qblocks = [(i * TB, TB, True) for i in range(4)]
        if self.L0:
            qblocks.append((OWN, CTX, False))
        kblocks = [(0, CTX)] + [(CTX + i * TB, TB) for i in range(8)]
        psS = self.ps[0:4]
        psO = self.ps[4:6]
        psQ = [self.ps[6], self.ps[7], self.ps[6]]
        oi = 0
        for h in range(NH):
            par = h % 2
            q = qh[par]
            kT = self.kT[par]
            v = vT[par]
            for bq, (c0, n, lat) in enumerate(qblocks):
                pa = psQ[0]
                for k in range(3):
                    self.mm(pa.ap[0:96, 0:n], wA.ap[:, k, h * 96:(h + 1) * 96], self.qnT.ap[:, k, c0:c0 + n],
                            k == 0, k == 2, [wA.r, self.qnT.r], [pa.r])
                self.cp("act", q.ap[0:64, c0:c0 + n], pa.ap[0:64, 0:n], [pa.r], [q.r])
                if lat:
                    self.load("sp", tabC.ap[64:96, 0:n], ropeC_d[64:96, c0:c0 + n], [tabC.r])
                    self.load("sp", tabS.ap[64:96, 0:n], ropeS_d[64:96, c0:c0 + n], [tabS.r])
                    pb = psQ[1]
                    for k in range(3):
                        self.mm(pb.ap[0:96, 0:n], wB.ap[:, k, h * 96:(h + 1) * 96], self.qnT.ap[:, k, c0:c0 + n],
                                k == 0, k == 2, [wB.r, self.qnT.r], [pb.r])
                    self.tt("dve", t1.ap[64:96, 0:n], pa.ap[64:96, 0:n], tabC.ap[64:96, 0:n], ALU.mult,
                            [pa.r, tabC.r], [t1.r])
                    self.tt("dve", t2.ap[64:96, 0:n], pb.ap[64:96, 0:n], tabS.ap[64:96, 0:n], ALU.mult,
                            [pb.r, tabS.r], [t2.r])
                    self.tt("dve", q.ap[64:96, c0:c0 + n], t1.ap[64:96, 0:n], t2.ap[64:96, 0:n], ALU.add,
                            [t1.r, t2.r], [q.r])
                else:
                    self.cp("act", q.ap[64:96, c0:c0 + n], pa.ap[64:96, 0:n], [pa.r], [q.r])
            for (c0, n) in kblocks:
                pk = psQ[2]
                for k in range(2):
                    self.mm(pk.ap[0:64, 0:n], wk.ap[:, k, h * 64:(h + 1) * 64], self.ckvT.ap[:, k, c0:c0 + n],
                            k == 0, k == 1, [wk.r, self.ckvT.r], [pk.r])
                self.cp("act", kT.ap[0:64, c0:c0 + n], pk.ap[0:64, 0:n], [pk.r], [kT.r])
            voff = 0 if par == 0 else 64
            for g0 in range(0, NKC, 8):
                gn = min(8, NKC - g0)
                pv = psQ[(g0 // 8) % 2]
                for c in range(gn):
                    kc = g0 + c
                    for k in range(2):
                        self.mm(pv.ap[:, c * 64:(c + 1) * 64], self.ckvT.ap[:, k, kc * 128:(kc + 1) * 128],
                                wv.ap[:, k, h * 64:(h + 1) * 64], k == 0, k == 1, [wv.r, self.ckvT.r], [pv.r],
                                inc=(k == 1 and c == gn - 1))
                self.cp("act", v.ap[:, g0:g0 + gn, voff:voff + 64],
                        pv.ap[:, 0:gn * 64].rearrange("p (c m) -> p c m", m=64), [pv.r], [v.r])
            if h == 0:
                self.tap("qh0", q.ap[0:96, :], [q.r], BF16)
                self.tap("kh0", kT.ap[0:96, :], [kT.r], BF16)
                self.tap("vh0", v.ap, [v.r], BF16)
            for (c0, n, lat) in qblocks:
                kcs = list(range(NKC)) if lat else [0, 1]
                po = psO[oi % 2]
                oi += 1

                def S(i):
                    kc = kcs[i]
                    ps = psS[i % 4]
                    self.mm(ps.ap[:, 0:n], kT.ap[0:96, kc * 128:(kc + 1) * 128], q.ap[0:96, c0:c0 + n], True, True,
                            [kT.r, q.r], [ps.r])

                S(0)
                if len(kcs) > 1:
                    S(1)
                if len(kcs) > 2:
                    S(2)
                for i, kc in enumerate(kcs):
                    ps = psS[i % 4]
                    pt = Pt[i % 4]
                    self.act(pt.ap[:, 0:n], ps.ap[:, 0:n], AF.Exp, [ps.r], [pt.r], scale=ATT_SCALE)
                    self.mm(po.ap[:, 0:n], v.ap[:, kc, :], pt.ap[:, 0:n], i == 0, i == len(kcs) - 1,
                            [v.r, pt.r], [po.r])
                    if i + 3 < len(kcs):
                        S(i + 3)
                pair = h // 2
                if par == 0:
                    self.recip(rec.ap[0:64, 0:n], po.ap[64:128, 0:n], [po.r], [rec.r])
                    self.tt("dve", self.oT.ap[0:64, pair, c0:c0 + n], po.ap[0:64, 0:n], rec.ap[0:64, 0:n], ALU.mult,
                            [po.r, rec.r], [self.oT.r])
                else:
                    self.recip(rec.ap[64:128, 0:n], po.ap[0:64, 0:n], [po.r], [rec.r])
                    self.tt("dve", self.oT.ap[64:128, pair, c0:c0 + n], po.ap[64:128, 0:n], rec.ap[64:128, 0:n],
                            ALU.mult, [po.r, rec.r], [self.oT.r])
        self.tap("oT", self.oT.ap, [self.oT.r], BF16)
        P.barrier()
        A.release(m0)

    def in_ap(self, name):
        return self._in_aps[name]

    def finish(self):
        P = self.P
        P.barrier()
        with self.nc.Block() as block:
            P.emit(block)
        self.es.close()
        return self.nc

MAGIC = 12582912.0
TWO_PI = 2.0 * math.pi


class LayerK2(LayerK):
    def _filter_mlp(self, zf_d, ncols, h2T, h2_res, w1, w2, fcol, fb1, fb2, wres):
        A = self.A
        m0 = A.mark()
        zf = Buf(A.f32(512), "zf")
        a1 = Buf(A.f32(512), "a1")
        a2 = Buf(A.f32(512), "a2")
        h1 = Buf(A.f32(512), "h1")
        ps1, ps2 = self.ps[0], self.ps[1]
        for c0 in range(0, ncols, 512):
            n = min(512, ncols - c0)
            self.load("sp", zf.ap[0:17, 0:n], zf_d[:, c0:c0 + n], [zf.r])
            self.mm(ps1.ap[0:64, 0:n], w1[0:17, :], zf.ap[0:17, 0:n], True, True, [wres, zf.r], [ps1.r])
            for (ps, fb, dst, dres) in ((ps1, fb1, h1.ap[0:64, 0:n], h1.r), (ps2, fb2, h2T[0:64, c0:c0 + n], h2_res)):
                if ps is ps2:
                    self.mm(ps2.ap[0:64, 0:n], w2[0:64, :], h1.ap[0:64, 0:n], True, True, [wres, h1.r], [ps2.r])
                self.ts("dve", a1.ap[0:64, 0:n], ps.ap[0:64, 0:n], fcol, fb, ALU.mult, ALU.add, [ps.r, wres], [a1.r])
                self.ts("dve", a2.ap[0:64, 0:n], a1.ap[0:64, 0:n], 1.0 / TWO_PI, MAGIC, ALU.mult, ALU.add, [a1.r], [a2.r])
                self.ts("dve", a2.ap[0:64, 0:n], a2.ap[0:64, 0:n], MAGIC, TWO_PI, ALU.subtract, ALU.mult, [a2.r], [a2.r])
                self.tt("dve", a1.ap[0:64, 0:n], a1.ap[0:64, 0:n], a2.ap[0:64, 0:n], ALU.subtract, [a1.r, a2.r], [a1.r])
                self.act(dst, a1.ap[0:64, 0:n], AF.Sin, [a1.r], [dres])
        A.release(m0)

    def _cmul(self, src, t1, t2, out_r, out_i, conj, m1, m2, src_res, tab_res, out_res):
        i = self._cm_i = getattr(self, "_cm_i", 0) + 1
        m1 = m1[i % len(m1)]
        m2 = m2[i % len(m2)]
        self.tt("dve", m1.ap, src, t1, ALU.mult, src_res + tab_res, [m1.r])
        self.tt("dve", m2.ap, src, t2, ALU.mult, src_res + tab_res, [m2.r])
        if not conj:
            self.tt("pool", out_r, m1.ap[:, 0:128], m2.ap[:, 128:256], ALU.subtract, [m1.r, m2.r], out_res)
            self.tt("pool", out_i, m2.ap[:, 0:128], m1.ap[:, 128:256], ALU.add, [m1.r, m2.r], out_res)
        else:
            self.tt("pool", out_r, m1.ap[:, 0:128], m2.ap[:, 128:256], ALU.add, [m1.r, m2.r], out_res)
            self.tt("pool", out_i, m1.ap[:, 128:256], m2.ap[:, 0:128], ALU.subtract, [m1.r, m2.r], out_res)

    def load_fft_tables(self):
        A = self.A
        self.ft_res = Res("fft_tabs")
        r = self.ft_res
        d = {}
        for nm in ("CS1a", "CS1b"):
            d[nm] = self.din(nm, [128, 256])
        for nm in ("W3r", "W3i", "W3n"):
            d[nm] = self.din(nm, [128, 128])
        for nm in ("T1", "T2"):
            d[nm] = self.din(nm, [128, 256])
        for nm in ("ICr", "ICi"):
            d[nm] = self.din(nm, [128, 64])
        self.CS1a = A.bf16(256); self.CS1b = A.bf16(256)
        self.W3r = A.bf16(128); self.W3i = A.bf16(128); self.W3n = A.bf16(128)
        self.T1 = A.f32(256); self.T2 = A.f32(256)
        self.ICr = A.bf16(64); self.ICi = A.bf16(64)
        for nm in ("CS1a", "CS1b", "W3r", "W3i", "W3n", "ICr", "ICi"):
            self.P.dma("pool", getattr(self, nm), d[nm], writes=[r])
        for nm in ("T1", "T2"):
            self.P.dma("sp", getattr(self, nm), d[nm], writes=[r])

    def _fft_s1(self, lhs_list, cs_list, lhs_res, psA, m1, m2, Bt):
        r = self.ft_res
        nl = len(lhs_list)
        for i, (lh, cs) in enumerate(zip(lhs_list, cs_list)):
            self.mm(psA.ap[:, 0:256], lh, cs, i == 0, i == nl - 1, lhs_res + [r], [psA.r])
        self._cmul(psA.ap[:, 0:256], self.T1, self.T2, Bt.ap[:, 0:128], Bt.ap[:, 128:256], False, m1, m2,
                   [psA.r], [r], [Bt.r])

    def _fft_s3(self, Bt, psZ):
        r = self.ft_res
        self.mm(psZ.ap[:, 0:128], self.W3r, Bt.ap[:, 0:128], True, False, [r, Bt.r], [psZ.r])
        self.mm(psZ.ap[:, 0:128], self.W3n, Bt.ap[:, 128:256], False, True, [r, Bt.r], [psZ.r])
        self.mm(psZ.ap[:, 128:256], self.W3i, Bt.ap[:, 0:128], True, False, [r, Bt.r], [psZ.r])
        self.mm(psZ.ap[:, 128:256], self.W3r, Bt.ap[:, 128:256], False, True, [r, Bt.r], [psZ.r])

    def phase_fg(self):
        A, P = self.A, self.P
        sm = self.small_res
        zff_d = self.din("zfT_f", [17, SEQ])
        zfb_d = self.din("zfT_b", [17, SEQ])
        tf_d = self.din("t_f", [1, SEQ])
        tb_d = self.din("t_b", [1, SEQ])
        w1_d = self.din("hy_w1", [17, 64])
        w2_d = self.din("hy_w2", [64, 64])
        w3_d = self.din("hy_w3", [64, 1024])
        fq_d = self.din("hy_fq", [64, 3])
        dec_d = self.din("hy_decay", [1, 1024])
        bias_d = self.din("hy_bias", [1, 512])
        self.Kh_d = self.nc.dram_tensor(f"Kh_scr{self.layer}", [128, 128, 512], F32, kind="Internal").ap()
        self.kh_res = Res("kh")
        self.x0T = Buf(A.bf16(4 * (OWN + TB)).rearrange("p (c t) -> p c t", c=4), "x0T")
        self.ohyT = Buf(self.x0T.ap[:, :, 1:OWN + 1], "ohyT")
        self.ohyT.res = self.x0T.res
        if self.L0:
            self.x0cT = Buf(A.bf16(4 * (CTX + 2)).rearrange("p (c t) -> p c t", c=4), "x0cT")
            self.ohycT = Buf(self.x0cT.ap[:, :, 1:CTX + 1], "ohycT")
            self.ohycT.res = self.x0cT.res
        self.rL1 = A.f32(4)
        self.mark_hyw = A.mark()
        self.load_fft_tables()
        if self.L0:
            self.zcT = Buf(A.f32(4 * (CTX + 1)).rearrange("p (c t) -> p c t", c=4), "zcT")
        self.fw_res = Res("fw")
        fw = self.fw_res
        self.hw1 = A.f32(64); self.hw2 = A.f32(64); self.hw3 = A.f32(1024); self.hfq = A.f32(3)
        self.hfb = A.f32(2)
        self.negdec = A.f32(1024)
        self.hbias = A.f32(512)
        self.load("sp", self.hw1[0:17, :], w1_d, [fw])
        self.load("sp", self.hw2[0:64, :], w2_d, [fw])
        self.load("sp", self.hw3[0:64, :], w3_d, [fw])
        self.load("sp", self.hfq[0:64, :], fq_d, [fw])
        self.load("sp", self.negdec[64:65, :], dec_d, [fw])
        self.load("sp", self.hbias[0:1, :], bias_d, [fw])
        self.ts("dve", self.hfb[0:64, 0:1], self.hfq[0:64, 1:2], self.hfq[0:64, 0:1], None, ALU.mult, None, [fw], [fw])
        self.ts("dve", self.hfb[0:64, 1:2], self.hfq[0:64, 2:3], self.hfq[0:64, 0:1], None, ALU.mult, None, [fw], [fw])
        self.act(self.negdec[64:65, :], self.negdec[64:65, :], AF.Abs, [fw], [fw])
        self.ts("dve", self.negdec[64:65, :], self.negdec[64:65, :], -1.0, None, ALU.mult, None, [fw], [fw])
        m0 = A.mark()
        h2 = [Buf(A.f32(SEQ), f"h2_{i}") for i in range(2)]
        trow = [Buf(h2[i].ap, f"trow{i}") for i in range(2)]
        self.load("sp", trow[0].ap[64:65, :], tf_d, [trow[0].r])
        self.load("sp", trow[1].ap[64:65, :], tb_d, [trow[1].r])
        fcol = self.hfq[0:64, 0:1]
        for d, zd in enumerate((zff_d, zfb_d)):
            self._filter_mlp(zd, SEQ, h2[d].ap, h2[d].r, self.hw1, self.hw2, fcol, self.hfb[0:64, 0:1],
                             self.hfb[0:64, 1:2], fw)
        kf = [Buf(A.bf16(16384).rearrange("p (g n c) -> p g n c", g=128, n=32), f"kfft{i}") for i in range(2)]
        acc = Buf(A.f32(512), "l1acc")
        self.memset("dve", acc.ap, 0.0, [acc.r])
        Es = [Buf(A.f32(512), f"E{i}") for i in range(2)]
        Ab = [Buf(A.f32(512), f"Ab{i}") for i in range(2)]
        kts = [Buf(A.f32(512), f"kt{i}") for i in range(2)]
        k0 = Buf(A.f32(512), "k0")
        psKs, psEs = [self.ps[2], self.ps[4]], [self.ps[3], self.ps[5]]
        itk = 0
        for d in range(2):
            for n2 in range(32):
                E, kt, ab = Es[itk % 2], kts[itk % 2], Ab[itk % 2]
                psK, psE = psKs[itk % 2], psEs[itk % 2]
                itk += 1
                lh = h2[d].ap.rearrange("p (a b) -> p a b", b=32)[0:64, :, n2]
                self.mm(psK.ap[:, :], lh, self.hw3[0:64, d * 512:(d + 1) * 512], True, True, [h2[d].r, fw], [psK.r])
                self.mm(psE.ap[:, :], trow[d].ap.rearrange("p (a b) -> p a b", b=32)[64:65, :, n2], self.negdec[64:65, d * 512:(d + 1) * 512], True, True,
                        [trow[d].r, fw], [psE.r])
                self.act(E.ap, psE.ap, AF.Exp, [psE.r], [E.r])
                sgn = 1.0 if d == 0 else -1.0
                self.stt(kt.ap, psK.ap, sgn, E.ap, ALU.mult, ALU.mult, [psK.r, E.r], [kt.r])
                if d == 1 and n2 == 0:
                    self.memset("dve", kt.ap[0:1, :], 0.0, [kt.r])
                if d == 0 and n2 == 0:
                    self.cp("dve", k0.ap[0:1, :], kt.ap[0:1, :], [kt.r], [k0.r])
                self.act(ab.ap, kt.ap, AF.Abs, [kt.r], [ab.r])
                self.tt("pool", acc.ap, acc.ap, ab.ap, ALU.add, [ab.r], [acc.r])
                self.cp("act", kf[d].ap[:, :, n2, :], kt.ap.rearrange("p (g c) -> p g c", c=4), [kt.r], [kf[d].r])
        kt = kts[0]
        psL = self.ps[4]
        self.mm(psL.ap[:, :], self.ones_f.ap, acc.ap, True, True, [acc.r, self.ones_f.r], [psL.r])
        self.tt("dve", kt.ap[0:1, :], psL.ap[0:1, :], self.hbias[0:1, :], ALU.mult, [psL.r, fw], [kt.r])
        self.tt("dve", kt.ap[0:1, :], kt.ap[0:1, :], k0.ap[0:1, :], ALU.add, [kt.r, k0.r], [kt.r])
        self.cp("act", kf[0].ap[0:1, :, 0, :], kt.ap[0:1, :].rearrange("p (g c) -> p g c", c=4), [kt.r], [kf[0].r])
        psC = self.ps[5]
        for c in range(4):
            self.mm(psC.ap[:, c:c + 1], acc.ap[:, c * 128:(c + 1) * 128], self.ones_f.ap[:, 0:1], True, True,
                    [acc.r, self.ones_f.r], [psC.r])
        self.recip(self.rL1, psC.ap[:, 0:4], [psC.r], [sm])
        self.tap("rL1", self.rL1, [sm])
        self.tap("kf0", kf[0].ap, [kf[0].r], BF16)
        self.tap("kf1", kf[1].ap, [kf[1].r], BF16)
        m1 = [Buf(A.f32(256), f"m1_{i}") for i in range(3)]; m2 = [Buf(A.f32(256), f"m2_{i}") for i in range(3)]
        Bt = [Buf(A.bf16(256), f"Bt{i}") for i in range(2)]
        Ks = [Buf(A.f32(512), f"Ks{i}") for i in range(2)]
        psAs, psZs = [self.ps[4], self.ps[5]], [self.ps[6], self.ps[7]]
        for it in range(128 + 1):
            g = it
            if g < 128:
                self._fft_s1([kf[0].ap[:, g, :, :].rearrange("p n c -> p (n c)"),
                              kf[1].ap[:, g, :, :].rearrange("p n c -> p (n c)")],
                             [self.CS1a, self.CS1b], [kf[0].r, kf[1].r], psAs[g % 2], m1, m2, Bt[g % 2])
            g = it - 1
            if 0 <= g < 128:
                psZ = psZs[g % 2]
                self._fft_s3(Bt[g % 2], psZ)
                ks = Ks[g % 2]
                self.cp("act", ks.ap[:, 0:512].rearrange("p (a b c) -> p a b c", a=2, b=2)[:, :, 0, :],
                        psZ.ap[:, 0:256].rearrange("p (a c) -> p a c", a=2), [psZ.r], [ks.r])
                self.cp("act", ks.ap[:, 0:512].rearrange("p (a b c) -> p a b c", a=2, b=2)[:, :, 1, :],
                        psZ.ap[:, 0:256].rearrange("p (a c) -> p a c", a=2), [psZ.r], [ks.r])
                self.P.dma("sp", self.Kh_d[g], ks.ap, reads=[ks.r], writes=[self.kh_res])
        P.barrier()
        A.release(m0)

    def phase_h(self):
        A, P = self.A, self.P
        sm = self.small_res
        why_d = self.din("w_hy", [D, 1536])
        sw_d = self.din("hy_sw", [128, 12, 4])
        sw = A.f32(48).rearrange("p (g j) -> p g j", j=4)
        self.load("sp", sw, sw_d, [sm])
        if self.L0:
            swc_d = self.din("hy_swc", [128, 12, 4])
            swc = A.f32(48).rearrange("p (g j) -> p g j", j=4)
            self.load("sp", swc, swc_d, [sm])
        self.zT = Buf(A.bf16(4 * (SEQ + 1)).rearrange("p (c t) -> p c t", c=4), "zT")
        m0 = A.mark()
        w = Buf(A.bf16(8 * 1536).rearrange("p (k c) -> p k c", k=8), "why")
        self.loadw_bf16(w.ap, why_d, 8, w.r)
        xb = [Buf(A.f32(8 * TB).rearrange("p (k t) -> p k t", k=8), f"xb{i}") for i in range(1)]
        tmp = Buf(A.f32(8 * TB).rearrange("p (k t) -> p k t", k=8), "tmp")
        hTs = [Buf(A.bf16(8 * TB).rearrange("p (k t) -> p k t", k=8), f"hT{i}") for i in range(2)]
        rstd = Buf(A.f32(TB), "rstd")
        R = [Buf(A.bf16(TB + 2), f"R{g}") for g in range(12)]
        ua = [Buf(A.f32(TB), f"ua{i}") for i in range(2)]
        ub = [Buf(A.f32(TB), f"ub{i}") for i in range(2)]

        cur = {"sw": sw}

        def conv(g, n, out_ap, out_res, eng_tmp):
            r = R[g]
            sw = cur["sw"]
            self.ts("dve", eng_tmp.ap[:, 0:n], r.ap[:, 0:n], sw[:, g, 0:1], sw[:, g, 3:4], ALU.mult, ALU.add,
                    [r.r, sm], [eng_tmp.r])
            self.stt(eng_tmp.ap[:, 0:n], r.ap[:, 1:n + 1], sw[:, g, 1:2], eng_tmp.ap[:, 0:n], ALU.mult, ALU.add,
                     [r.r, sm], [eng_tmp.r])
            self.stt(out_ap, r.ap[:, 2:n + 2], sw[:, g, 2:3], eng_tmp.ap[:, 0:n], ALU.mult, ALU.add,
                     [r.r, sm, eng_tmp.r], out_res)

        def reset_R():
            for g in range(12):
                self.memset("pool", R[g].ap[:, 0:2], 0.0, [R[g].r])

        order = ([8] if self.L0 else []) + list(range(8))
        reset_R()

        def prep(it_):
            bi_ = order[it_]
            src_, n_, col_ = self.xblock_src(bi_)
            hT_ = hTs[it_ % 2]
            x_ = xb[0]
            self.load("sp", x_.ap[:, :, 0:n_], src_, [x_.r])
            self.norm_mod(x_.ap[:, :, 0:n_], n_, self.gs1, self.sh1, col_, hT_.ap[:, :, 0:n_], tmp.ap[:, :, 0:n_],
                          rstd.ap[:, 0:n_], x_.r, hT_.r, tmp.r, rstd.r, self.ps[0])

        prep(0)
        for it, bi in enumerate(order):
            src, n, col = self.xblock_src(bi)
            cur["sw"] = swc if bi == 8 else sw
            hT = hTs[it % 2]
            if it + 1 < len(order):
                prep(it + 1)
            need_x0 = (bi == 8) or (bi <= 4)
            groups = list(range(12)) if need_x0 else list(range(8))
            for g in groups:
                psb = self.ps[1 + (g % 4)]
                for k in range(8):
                    self.mm(psb.ap[:, 0:n], w.ap[:, k, g * 128:(g + 1) * 128], hT.ap[:, k, 0:n], k == 0, k == 7,
                            [w.r, hT.r], [psb.r])
                self.cp("act", R[g].ap[:, 2:n + 2], psb.ap[:, 0:n], [psb.r], [R[g].r])
            base = 0 if bi == 8 else bi * TB
            zdst = self.zcT if bi == 8 else self.zT
            x0dst = self.x0cT if bi == 8 else self.x0T
            T4 = [ua[0], ub[0], ua[1], ub[1]]

            def conv_batch(gl, outs):
                sw_ = cur["sw"]
                for i, g in enumerate(gl):
                    t_ = T4[i]
                    self.ts("dve", t_.ap[:, 0:n], R[g].ap[:, 0:n], sw_[:, g, 0:1], sw_[:, g, 3:4], ALU.mult, ALU.add,
                            [R[g].r, sm], [t_.r])
                for i, g in enumerate(gl):
                    t_ = T4[i]
                    self.stt(t_.ap[:, 0:n], R[g].ap[:, 1:n + 1], sw_[:, g, 1:2], t_.ap[:, 0:n], ALU.mult, ALU.add,
                             [R[g].r, sm], [t_.r])
                for i, g in enumerate(gl):
                    t_ = T4[i]
                    oap, ores = outs[i]
                    self.stt(oap, R[g].ap[:, 2:n + 2], sw_[:, g, 2:3], t_.ap[:, 0:n], ALU.mult, ALU.add,
                             [R[g].r, sm, t_.r], ores)

            for c2 in range(0, 4, 2):
                gl = [c2, 4 + c2, c2 + 1, 4 + c2 + 1]
                conv_batch(gl, [(T4[i].ap[:, 0:n], [T4[i].r]) for i in range(4)])
                self.tt("dve", zdst.ap[:, c2, base:base + n], T4[0].ap[:, 0:n], T4[1].ap[:, 0:n], ALU.mult,
                        [T4[0].r, T4[1].r], [zdst.r])
                self.tt("dve", zdst.ap[:, c2 + 1, base:base + n], T4[2].ap[:, 0:n], T4[3].ap[:, 0:n], ALU.mult,
                        [T4[2].r, T4[3].r], [zdst.r])
            if need_x0:
                conv_batch([8, 9, 10, 11], [(x0dst.ap[:, c, base:base + n], [x0dst.r]) for c in range(4)])
            for g in groups:
                self.cp("pool", R[g].ap[:, 0:2], R[g].ap[:, n:n + 2], [R[g].r], [R[g].r])
            last_of_seq = (bi == 8) or (bi == 7)
            if last_of_seq:
                fl = base + n
                fgroups = list(range(12)) if bi == 8 else list(range(8))
                for g in fgroups:
                    self.memset("pool", R[g].ap[:, 2:3], 0.0, [R[g].r])
                for c in range(4):
                    a, b_ = ua[c % 2], ub[c % 2]
                    conv(c, 1, a.ap[:, 0:1], [a.r], a)
                    conv(4 + c, 1, b_.ap[:, 0:1], [b_.r], b_)
                    self.tt("dve", zdst.ap[:, c, fl:fl + 1], a.ap[:, 0:1], b_.ap[:, 0:1], ALU.mult, [a.r, b_.r], [zdst.r])
                    if bi == 8:
                        conv(8 + c, 1, x0dst.ap[:, c, fl:fl + 1], [x0dst.r], a)
                reset_R()
        self.tap("zT", self.zT.ap, [self.zT.r], BF16)
        self.tap("x0T", self.x0T.ap, [self.x0T.r], BF16)
        if self.L0:
            self.tap("zcT", self.zcT.ap, [self.zcT.r])
            self.tap("x0cT", self.x0cT.ap, [self.x0cT.r], BF16)
        P.barrier()
        A.release(m0)

    def phase_f(self):
        A, P = self.A, self.P
        sm = self.small_res
        r = self.ft_res
        m0 = A.mark()
        ztm = Buf(A.bf16(16384).rearrange("p (g n c) -> p g n c", g=128, n=32), "ztm")
        zv = self.zT.ap[:, :, 1:SEQ + 1].rearrange("p c (a b) -> p c a b", b=32)
        psT = [self.ps[0], self.ps[1]]
        ti = 0
        for cc in range(4):
            for n20 in range(0, 32, 4):
                pt = psT[ti % 2]
                ti += 1
                ptb = pt.ap.bitcast(BF16)
                for j in range(4):
                    self.tr(ptb[:, j * 128:(j + 1) * 128], zv[:, cc, :, n20 + j], self.ident_b.ap,
                            [self.zT.r, self.ident_b.r], [pt.r])
                self.cp("act" if ti % 2 else "dve",
                        ztm.ap[:, cc * 32:(cc + 1) * 32, n20:n20 + 4, :].rearrange("p g n c -> p n g c"),
                        ptb[:, 0:512].rearrange("p (n g c) -> p n g c", n=4, c=4), [pt.r], [ztm.r])
        self.tap("ztm", ztm.ap, [ztm.r], BF16)
        m1 = [Buf(A.f32(256), f"m1_{i}") for i in range(3)]; m2 = [Buf(A.f32(256), f"m2_{i}") for i in range(3)]
        Bt = [Buf(A.bf16(256), f"Bt{i}") for i in range(2)]
        Ks = [Buf(A.f32(512), f"Ks{i}") for i in range(2)]
        Yt = [Buf(A.bf16(256), f"Yt{i}") for i in range(2)]
        Gp = [Buf(A.bf16(256), f"Gp{i}") for i in range(2)]
        GT = [Buf(A.bf16(1024).rearrange("p (r c) -> p r c", r=2), f"GT{i}") for i in range(2)]
        yall = Buf(A.f32(32 * 128), "yall")
        yv = yall.ap.rearrange("p (n g c) -> p n g c", n=32, g=32)
        y3 = yall.ap.rearrange("p (n m) -> p n m", n=32)
        psAs, psZs, psGs = [self.ps[0], self.ps[1]], [self.ps[2], self.ps[3]], [self.ps[4], self.ps[5]]
        psM, psYT = self.ps[6], self.ps[7]
        psGT_res, psY_res = Res("psGT"), Res("psY")
        pgb = psM.ap.bitcast(BF16)
        psY = psM.ap[0:64, 256:512]
        GT2 = [Buf(A.bf16(512).rearrange("p (r c) -> p r c", r=2), f"GTp{i}") for i in range(2)]

        def stage_a(g):
            ks = Ks[g % 2]
            self.P.dma("sp", ks.ap, self.Kh_d[g], reads=[self.kh_res], writes=[ks.r])
            self._fft_s1([ztm.ap[:, g, :, :].rearrange("p n c -> p (n c)")], [self.CS1a], [ztm.r],
                         psAs[g % 2], m1, m2, Bt[g % 2])

        def stage_b(g):
            ks = Ks[g % 2]
            psZ = psZs[g % 2]
            self._fft_s3(Bt[g % 2], psZ)
            yt = Yt[g % 2]
            self._cmul(psZ.ap[:, 0:256], ks.ap[:, 0:256], ks.ap[:, 256:512], yt.ap[:, 0:128], yt.ap[:, 128:256],
                       False, m1, m2, [psZ.r], [ks.r], [yt.r])

        def stage_c(g):
            yt = Yt[g % 2]
            psG = psGs[g % 2]
            self.mm(psG.ap[:, 0:128], self.W3r, yt.ap[:, 0:128], True, False, [r, yt.r], [psG.r])
            self.mm(psG.ap[:, 0:128], self.W3i, yt.ap[:, 128:256], False, True, [r, yt.r], [psG.r])
            self.mm(psG.ap[:, 128:256], self.W3n, yt.ap[:, 0:128], True, False, [r, yt.r], [psG.r])
            self.mm(psG.ap[:, 128:256], self.W3r, yt.ap[:, 128:256], False, True, [r, yt.r], [psG.r])
            gp = Gp[g % 2]
            self._cmul(psG.ap[:, 0:256], self.T1, self.T2, gp.ap[:, 0:128], gp.ap[:, 128:256], True, m1, m2,
                       [psG.r], [r], [gp.r])

        def stage_d(g):
            gp = Gp[g % 2]
            gt = GT2[(g // 2) % 2]
            gl = g % 2
            self.tr(pgb[:, 0:128], gp.ap[:, 0:128], self.ident_b.ap, [gp.r, self.ident_b.r], [psGT_res])
            self.tr(pgb[:, 128:256], gp.ap[:, 128:256], self.ident_b.ap, [gp.r, self.ident_b.r], [psGT_res])
            self.cp("act", gt.ap[:, :, gl * 128:(gl + 1) * 128], pgb[:, 0:256].rearrange("p (r c) -> p r c", r=2),
                    [psGT_res], [gt.r])
            if gl == 1:
                self.mm(psY, self.ICr, gt.ap[:, 0, :], True, False, [r, gt.r], [psY_res])
                self.mm(psY, self.ICi, gt.ap[:, 1, :], False, True, [r, gt.r], [psY_res])
                gq = (g // 2) % 16
                self.cp("act", yv[0:64, :, gq * 2:(gq + 1) * 2, :].rearrange("p n g c -> p g n c"),
                        psY.rearrange("p (g n c) -> p g n c", g=2, n=32), [psY_res], [yall.r])
            if g % 32 == 31:
                cc = g // 32
                if cc == 0:
                    self.tap("yall0", yall.ap[0:64, :], [yall.r])
                x0v = self.x0T.ap[:, cc, 1:OWN + 1].rearrange("p (a b) -> p a b", b=32)
                ov = self.ohyT.ap[:, cc, :].rearrange("p (a b) -> p a b", b=32)
                for n20 in range(0, 32, 8):
                    for j in range(8):
                        n2 = n20 + j
                        self.tr(psYT.ap[:, j * 64:(j + 1) * 64], y3[0:64, n2, :], self.ident_f.ap[0:64, 0:64],
                                [yall.r, self.ident_f.r], [psYT.r])
                    self.stt(ov[:, :, n20:n20 + 8].rearrange("p a b -> p b a"),
                             psYT.ap[:, 0:512].rearrange("p (b a) -> p b a", b=8), self.rL1[:, cc:cc + 1],
                             x0v[:, :, n20:n20 + 8].rearrange("p a b -> p b a"), ALU.mult, ALU.mult,
                             [psYT.r, self.x0T.r, sm], [self.ohyT.r])

        for it in range(128 + 3):
            if it < 128:
                stage_a(it)
            if 0 <= it - 1 < 128:
                stage_b(it - 1)
            if 0 <= it - 2 < 128:
                stage_c(it - 2)
            if 0 <= it - 3 < 128:
                stage_d(it - 3)
        self.tap("ohyT", self.ohyT.ap, [self.ohyT.r], BF16)
        P.barrier()
        A.release(m0)

    def phase_ctxhy(self):
        A, P = self.A, self.P
        sm = self.small_res
        fw = self.fw_res
        zfc_d = self.din("zfT_c", [17, CTX])
        tc_d = self.din("tc_bc", [128, CTX])
        w3c_d = self.din("hyc_w3", [64, 1024])
        dec_d = self.din("hyc_dec", [128, 8])
        bias_d = self.din("hyc_bias", [128, 4])
        m0 = A.mark()
        w3c = A.f32(1024)
        nd = A.f32(8)
        bc = A.f32(4)
        tcb = A.f32(CTX)
        cw = Res("ctxw")
        self.load("sp", w3c[0:64, :], w3c_d, [cw])
        self.load("sp", nd, dec_d, [cw])
        self.load("sp", bc, bias_d, [cw])
        self.load("sp", tcb, tc_d, [cw])
        self.act(nd, nd, AF.Abs, [cw], [cw])
        self.ts("dve", nd, nd, -1.0, None, ALU.mult, None, [cw], [cw])
        h2c = Buf(A.f32(CTX), "h2c")
        self._filter_mlp(zfc_d, CTX, h2c.ap, h2c.r, self.hw1, self.hw2, self.hfq[0:64, 0:1], self.hfb[0:64, 0:1],
                         self.hfb[0:64, 1:2], fw)
        kc = [Buf(A.f32(4 * CTX).rearrange("p (c t) -> p c t", c=4), f"kc{d}") for d in range(2)]
        E = Buf(A.f32(CTX), "Ec")
        l1 = Buf(A.f32(8), "l1c")
        for d in range(2):
            for c in range(4):
                ps = self.ps[(d * 4 + c) % 2]
                self.mm(ps.ap[:, 0:CTX], w3c[0:64, d * 512 + c * 128:d * 512 + (c + 1) * 128], h2c.ap[0:64, :], True, True,
                        [cw, h2c.r], [ps.r])
                j = d * 4 + c
                self.act(E.ap, tcb, AF.Exp, [cw], [E.r], scale=nd[:, j:j + 1])
                self.tt("dve", kc[d].ap[:, c, :], ps.ap[:, 0:CTX], E.ap, ALU.mult, [ps.r, E.r], [kc[d].r])
                src = kc[d].ap[:, c, :] if d == 0 else kc[d].ap[:, c, 1:CTX]
                self.P.op("dve", lambda e, o=l1.ap[:, j:j + 1], s=src: e.tensor_reduce(
                    out=o, in_=s, axis=AX.X, op=ALU.add, apply_absolute_value=True), [kc[d].r], [l1.r])
        self.tt("dve", l1.ap[:, 0:4], l1.ap[:, 0:4], l1.ap[:, 4:8], ALU.add, [l1.r], [l1.r])
        self.recip(l1.ap[:, 0:4], l1.ap[:, 0:4], [l1.r], [l1.r])
        for d in range(2):
            for c in range(4):
                self.ts("dve", kc[d].ap[:, c, :], kc[d].ap[:, c, :], l1.ap[:, c:c + 1], None, ALU.mult, None,
                        [l1.r, kc[d].r], [kc[d].r])
        for c in range(4):
            self.tt("dve", kc[0].ap[:, c, 0:1], kc[0].ap[:, c, 0:1], bc[:, c:c + 1], ALU.add, [kc[0].r, cw], [kc[0].r])
        self.tap("kc0", kc[0].ap, [kc[0].r])
        z = self.zcT
        zpad = Buf(A.bf16(4 * 3 * CTX).rearrange("p (c t) -> p c t", c=4), "zpad")
        self.memset("pool", zpad.ap, 0.0, [zpad.r])
        self.cp("act", zpad.ap[:, :, CTX:2 * CTX], z.ap[:, :, 1:CTX + 1], [z.r], [zpad.r])
        NDG = 16
        Dg = [Buf(A.bf16(128), f"Dg{i}") for i in range(NDG)]
        accs = self.ps[0:4]
        di = 0
        nl = 2 * CTX - 1
        for e_ in range(nl):
            d = e_ - (CTX - 1)
            kk = kc[0] if d >= 0 else kc[1]
            for c in range(4):
                dg = Dg[di % NDG]
                eng = "dve"
                di += 1
                self.ts(eng, dg.ap, self.ident_b.ap, kk.ap[:, c, abs(d):abs(d) + 1], None, ALU.mult, None,
                        [kk.r, self.ident_b.r], [dg.r])
                self.mm(accs[c].ap[:, 0:CTX], dg.ap, zpad.ap[:, c, CTX - d:2 * CTX - d], e_ == 0, e_ == nl - 1,
                        [dg.r, zpad.r], [accs[c].r], inc=True)
        for c in range(4):
            self.tt("dve", self.ohycT.ap[:, c, :], accs[c].ap[:, 0:CTX], self.x0cT.ap[:, c, 1:CTX + 1], ALU.mult,
                    [accs[c].r], [self.x0cT.r])
        self.tap("ohycT", self.ohycT.ap, [self.x0cT.r], BF16)
        P.barrier()
        A.release(m0)

    def phase_m(self):
        A, P = self.A, self.P
        sm = self.small_res
        wg_d = self.din("w_gate", [D, 2048])
        wba_d = self.din("w_br_attn", [512, D])
        wbh_d = self.din("w_br_hy", [512, D])
        wo_d = self.din("w_out", [D, D])
        NT = OWN + (CTX if self.L0 else 0)
        self.NT = NT
        self.xmid_d = self.nc.dram_tensor(f"xmid_scr{self.layer}", [D, NT], F32, kind="Internal").ap()
        self.xmid_v = self.xmid_d.rearrange("(k p) c -> p k c", p=128)
        self.xmid_res = Res("xmid")
        m0 = A.mark()
        wg = Buf(A.bf16(8 * 2048).rearrange("p (k c) -> p k c", k=8), "wg")
        wba = Buf(A.bf16(4 * D).rearrange("p (k c) -> p k c", k=4), "wba")
        wbh = Buf(A.bf16(4 * D).rearrange("p (k c) -> p k c", k=4), "wbh")
        wo = Buf(A.bf16(8 * D).rearrange("p (k c) -> p k c", k=8), "wo")
        self.loadw_bf16(wg.ap, wg_d, 8, wg.r)
        self.loadw_bf16(wba.ap, wba_d, 4, wba.r)
        self.loadw_bf16(wbh.ap, wbh_d, 4, wbh.r)
        self.loadw_bf16(wo.ap, wo_d, 8, wo.r)
        xb = Buf(A.f32(8 * TB).rearrange("p (k t) -> p k t", k=8), "xb")
        tmp = Buf(A.f32(8 * TB).rearrange("p (k t) -> p k t", k=8), "tmp")
        hT = Buf(A.bf16(8 * TB).rearrange("p (k t) -> p k t", k=8), "hT")
        rstd = Buf(A.f32(TB), "rstd")
        mg = Buf(A.bf16(8 * TB).rearrange("p (k t) -> p k t", k=8), "mg")
        ga = [Buf(A.f32(TB), f"ga{i}") for i in range(2)]
        gh = [Buf(A.f32(TB), f"gh{i}") for i in range(2)]
        blocks = [0, 1, 2, 3] + ([8] if self.L0 else [])
        for bi in blocks:
            src, n, col = self.xblock_src(bi)
            c0 = bi * TB if bi < 8 else OWN
            self.load("sp", xb.ap[:, :, 0:n], src, [xb.r])
            self.norm_mod(xb.ap[:, :, 0:n], n, self.gs1, self.sh1, col, hT.ap[:, :, 0:n], tmp.ap[:, :, 0:n],
                          rstd.ap[:, 0:n], xb.r, hT.r, tmp.r, rstd.r, self.ps[0])
            oa = self.oT.ap[:, :, c0:c0 + n]
            if bi < 8:
                oh = self.ohyT.ap[:, :, c0:c0 + n]
                oh_res = self.ohyT.r
            else:
                oh = self.ohycT.ap[:, :, 0:n]
                oh_res = self.ohycT.r
            for c in range(8):
                pga, pgh, pba, pbh = self.ps[(c % 2) * 4:(c % 2) * 4 + 4]
                for k in range(8):
                    self.mm(pga.ap[:, 0:n], wg.ap[:, k, c * 128:(c + 1) * 128], hT.ap[:, k, 0:n], k == 0, k == 7,
                            [wg.r, hT.r], [pga.r])
                for k in range(8):
                    self.mm(pgh.ap[:, 0:n], wg.ap[:, k, 1024 + c * 128:1024 + (c + 1) * 128], hT.ap[:, k, 0:n],
                            k == 0, k == 7, [wg.r, hT.r], [pgh.r])
                for k in range(4):
                    self.mm(pba.ap[:, 0:n], wba.ap[:, k, c * 128:(c + 1) * 128], oa[:, k, :], k == 0, k == 3,
                            [wba.r, self.oT.r], [pba.r])
                for k in range(4):
                    self.mm(pbh.ap[:, 0:n], wbh.ap[:, k, c * 128:(c + 1) * 128], oh[:, k, :], k == 0, k == 3,
                            [wbh.r, oh_res], [pbh.r])
                a_, h_ = ga[c % 2], gh[c % 2]
                self.act(a_.ap[:, 0:n], pga.ap[:, 0:n], AF.Sigmoid, [pga.r], [a_.r])
                self.act(h_.ap[:, 0:n], pgh.ap[:, 0:n], AF.Sigmoid, [pgh.r], [h_.r])
                self.tt("dve", a_.ap[:, 0:n], a_.ap[:, 0:n], pba.ap[:, 0:n], ALU.mult, [a_.r, pba.r], [a_.r])
                self.tt("dve", h_.ap[:, 0:n], h_.ap[:, 0:n], pbh.ap[:, 0:n], ALU.mult, [h_.r, pbh.r], [h_.r])
                self.tt("dve", mg.ap[:, c, 0:n], a_.ap[:, 0:n], h_.ap[:, 0:n], ALU.add, [a_.r, h_.r], [mg.r])
            if bi == 0:
                self.tap("mg0", mg.ap, [mg.r], BF16)
            for c in range(8):
                po = self.ps[c % 2]
                for k in range(8):
                    self.mm(po.ap[:, 0:n], wo.ap[:, k, c * 128:(c + 1) * 128], mg.ap[:, k, 0:n], k == 0, k == 7,
                            [wo.r, mg.r], [po.r])
                self.stt(xb.ap[:, c, 0:n], po.ap[:, 0:n], self.g1[:, c, col:col + 1], xb.ap[:, c, 0:n],
                         ALU.mult, ALU.add, [po.r, sm], [xb.r])
            self.P.dma("sp", self.xmid_v[:, :, c0:c0 + n], xb.ap[:, :, 0:n], reads=[xb.r], writes=[self.xmid_res])
        P.barrier()
        A.release(m0)

    def phase_ffn(self):
        A, P = self.A, self.P
        sm = self.small_res
        moe = not self.L0
        NT = self.NT
        nexp = NE if moe else 1
        if moe:
            wgd = self.din("moe_w_gate", [NE, D, DFF])
            wud = self.din("moe_w_up", [NE, D, DFF])
            wdd = self.din("moe_w_down", [NE, DFF, D])
            rt_d = self.din("moe_router", [128, 8, NE])
            fg_d = self.din("final_g", [128, 8])
        else:
            wgd = self.din("ffn_w_gate", [1, D, DFF])
            wud = self.din("ffn_w_up", [1, D, DFF])
            wdd = self.din("ffn_w_down", [1, DFF, D])
        fused0 = self.L0 and getattr(self, "fused", False)
        if not fused0:
            xout_d = self.dout("xout", [D, OWN])
            xout_v = xout_d.rearrange("(k p) c -> p k c", p=128)
        if self.L0 and not fused0:
            cout_d = self.dout("cout", [D, CTX])
            cout_v = cout_d.rearrange("(k p) c -> p k c", p=128)
        xT = Buf(A.f32(8 * NT).rearrange("p (k t) -> p k t", k=8), "xT", nres=5)
        h2T = Buf(A.bf16(8 * NT).rearrange("p (k t) -> p k t", k=8), "h2T", nres=5)
        blocks = [(i * TB, TB, 0) for i in range(4)] + ([(OWN, CTX, 1)] if self.L0 else [])
        if moe:
            cb = Buf(A.bf16(NE * OWN).rearrange("p (e t) -> p e t", e=NE), "cb", nres=4)
            rt = A.f32(8 * NE).rearrange("p (k e) -> p k e", k=8)
            fgc = A.f32(8)
            self.load("sp", rt, rt_d, [sm])
            self.load("sp", fgc, fg_d, [sm])
        m0 = A.mark()
        tmp = Buf(A.f32(8 * TB).rearrange("p (k t) -> p k t", k=8), "tmp")
        rstd = Buf(A.f32(TB), "rstd")
        if moe:
            hf = tmp
            lg = Buf(A.f32(8), "lg"); m8 = Buf(A.f32(8), "m8"); wv = Buf(A.f32(8), "wv"); nv1 = Buf(A.f32(2), "nv1")
            dg = Buf(A.f32(128), "dg")
        for bidx, (c0, n, col) in enumerate(blocks):
            self.load("sp", xT.ap[:, :, c0:c0 + n], self.xmid_v[:, :, c0:c0 + n], [xT.res[bidx]])
            self.norm_mod(xT.ap[:, :, c0:c0 + n], n, self.gs2, self.sh2, col, h2T.ap[:, :, c0:c0 + n], tmp.ap[:, :, 0:n],
                          rstd.ap[:, 0:n], xT.res[bidx], h2T.res[bidx], tmp.r, rstd.r, self.ps[0],
                          hf3=(hf.ap[:, :, 0:n] if moe else None), hf_res=(hf.r if moe else None))
            if bidx == 0:
                self.tap("h2T0", h2T.ap[:, :, 0:TB], [h2T.res[0]], BF16)
            if moe:
                for t in range(n // 128):
                    pl = self.ps[1 + (t % 2)]
                    for k in range(8):
                        self.mm(pl.ap[:, 0:NE], hf.ap[:, k, t * 128:(t + 1) * 128], rt[:, k, :], k == 0, k == 7,
                                [hf.r, sm], [pl.r])
                    self.cp("act", lg.ap, pl.ap[:, 0:NE], [pl.r], [lg.r])
                    self.P.op("dve", lambda e, o=m8.ap, i=lg.ap: e.max(out=o, in_=i), [lg.r], [m8.r])
                    self.ts("dve", wv.ap, lg.ap, m8.ap[:, 1:2], None, ALU.is_ge, None, [lg.r, m8.r], [wv.r])
                    self.ts("dve", nv1.ap[:, 0:1], m8.ap[:, 0:1], -1.0, None, ALU.mult, None, [m8.r], [nv1.r])
                    self.act(lg.ap, lg.ap, AF.Exp, [lg.r, nv1.r], [lg.r], bias=nv1.ap[:, 0:1])
                    self.tt("dve", wv.ap, wv.ap, lg.ap, ALU.mult, [wv.r, lg.r], [wv.r])
                    self.P.op("dve", lambda e, o=nv1.ap[:, 1:2], i=wv.ap: e.tensor_reduce(out=o, in_=i, axis=AX.X, op=ALU.add),
                              [wv.r], [nv1.r])
                    self.recip(nv1.ap[:, 1:2], nv1.ap[:, 1:2], [nv1.r], [nv1.r])
                    self.ts("dve", wv.ap, wv.ap, nv1.ap[:, 1:2], None, ALU.mult, None, [wv.r, nv1.r], [wv.r])
                    if bidx == 0 and t == 0:
                        self.tap("comb0", wv.ap, [wv.r])
                    for e_ in range(NE):
                        self.ts("dve", dg.ap, self.ident_f.ap, wv.ap[:, e_:e_ + 1], None, ALU.mult, None,
                                [wv.r, self.ident_f.r], [dg.r])
                        pc = self.ps[3 + (e_ % 4)]
                        self.mm(pc.ap[:, 0:128], self.ones_f.ap, dg.ap, True, True, [dg.r, self.ones_f.r], [pc.r])
                        self.cp("act", cb.ap[:, e_, c0 + t * 128:c0 + (t + 1) * 128], pc.ap[:, 0:128], [pc.r],
                                [cb.res[bidx]])
        P.barrier()
        A.release(m0)
        NG = DFF // 256
        W = []
        for i in range(2):
            W.append((Buf(A.bf16(8 * 256).rearrange("p (k c) -> p k c", k=8), f"wg{i}"),
                      Buf(A.bf16(8 * 256).rearrange("p (k c) -> p k c", k=8), f"wu{i}"),
                      Buf(A.bf16(2 * D).rearrange("p (k c) -> p k c", k=2), f"wd{i}")))
        sg = [Buf(A.bf16(TB), f"sg{i}") for i in range(2)]
        hid = [Buf(A.bf16(2 * TB).rearrange("p (j t) -> p j t", j=2), f"hid{i}") for i in range(2)]
        gi = 0
        for e_ in range(nexp):
            for g in range(NG):
                wg_, wu_, wd_ = W[gi % 2]
                f0 = g * 256
                for k in range(8):
                    self.P.dma("pool", wg_.ap[:, k, :], wgd[e_, k * 128:(k + 1) * 128, f0:f0 + 256], writes=[wg_.r])
                    self.P.dma("pool", wu_.ap[:, k, :], wud[e_, k * 128:(k + 1) * 128, f0:f0 + 256], writes=[wu_.r])
                for j in range(2):
                    self.P.dma("pool", wd_.ap[:, j, :], wdd[e_, f0 + j * 128:f0 + (j + 1) * 128, :], writes=[wd_.r])
                for bidx, (c0, n, col) in enumerate(blocks):
                    hd = hid[bidx % 2]
                    for j in range(2):
                        pg, pu = self.ps[2 * j], self.ps[2 * j + 1]
                        for k in range(8):
                            self.mm(pg.ap[:, 0:n], wg_.ap[:, k, j * 128:(j + 1) * 128], h2T.ap[:, k, c0:c0 + n],
                                    k == 0, k == 7, [wg_.r, h2T.res[bidx]], [pg.r])
                        for k in range(8):
                            self.mm(pu.ap[:, 0:n], wu_.ap[:, k, j * 128:(j + 1) * 128], h2T.ap[:, k, c0:c0 + n],
                                    k == 0, k == 7, [wu_.r, h2T.res[bidx]], [pu.r])
                        s_ = sg[j]
                        self.act(s_.ap[:, 0:n], pg.ap[:, 0:n], AF.Silu, [pg.r], [s_.r])
                        if moe:
                            self.tt("dve", s_.ap[:, 0:n], s_.ap[:, 0:n], cb.ap[:, e_, c0:c0 + n], ALU.mult,
                                    [s_.r, cb.res[bidx]], [s_.r])
                        self.tt("dve", hd.ap[:, j, 0:n], s_.ap[:, 0:n], pu.ap[:, 0:n], ALU.mult, [s_.r, pu.r], [hd.r])
                    for dc in range(8):
                        py = self.ps[4 + (dc % 4)]
                        for j in range(2):
                            self.mm(py.ap[:, 0:n], wd_.ap[:, j, dc * 128:(dc + 1) * 128], hd.ap[:, j, 0:n],
                                    j == 0, j == 1, [wd_.r, hd.r], [py.r])
                        self.stt(xT.ap[:, dc, c0:c0 + n], py.ap[:, 0:n], self.g2[:, dc, col:col + 1],
                                 xT.ap[:, dc, c0:c0 + n], ALU.mult, ALU.add, [py.r, sm], [xT.res[bidx]])
                gi += 1
        self.out_res = Res("out")
        if self.L0 and getattr(self, "fused", False):
            P.barrier()
            A.release(m0)
            sc = [Buf(A.f32(8 * TB).rearrange("p (k t) -> p k t", k=8), f"sc{i}") for i in range(2)]
            bv = self.bounce_d.rearrange("(s k p) c -> s p k c", s=2, p=128)
            for bidx, (c0, n, col) in enumerate(blocks):
                if col == 0:
                    self.P.dma("sp", self.x1T_v[:, :, c0:c0 + n], xT.ap[:, :, c0:c0 + n], reads=[xT.res[bidx]],
                               writes=[self.out_res])
                    for sl in range(2):
                        self.ts("dve", sc[sl].ap[:, :, 0:n], xT.ap[:, :, c0:c0 + n], self.smask[:, sl:sl + 1], None,
                                ALU.mult, None, [xT.res[bidx], sm], [sc[sl].r])
                        self.P.dma("sp", bv[sl][:, :, c0:c0 + n], sc[sl].ap[:, :, 0:n], reads=[sc[sl].r],
                                   writes=[self.out_res])
                else:
                    self.P.dma("sp", self.c1T_v, xT.ap[:, :, c0:c0 + n], reads=[xT.res[bidx]], writes=[self.out_res])
        elif self.L0:
            for bidx, (c0, n, col) in enumerate(blocks):
                if col == 0:
                    self.P.dma("sp", xout_v[:, :, c0:c0 + n], xT.ap[:, :, c0:c0 + n], reads=[xT.res[bidx]], writes=[self.out_res])
                else:
                    self.P.dma("sp", cout_v, xT.ap[:, :, c0:c0 + n], reads=[xT.res[bidx]], writes=[self.out_res])
        else:
            P.barrier()
            A.release(m0)
            tmp = Buf(A.f32(8 * TB).rearrange("p (k t) -> p k t", k=8), "ftmp")
            rstd = Buf(A.f32(TB), "frstd")
            for bidx, (c0, n, col) in enumerate(blocks):
                self.rms_bcast(xT.ap[:, :, c0:c0 + n], 8, n, D, rstd.ap[:, 0:n], tmp.ap[:, :, 0:n], xT.res[bidx], rstd.r,
                               tmp.r, self.ps[0])
                for k in range(8):
                    self.stt(tmp.ap[:, k, 0:n], xT.ap[:, k, c0:c0 + n], fgc[:, k:k + 1], rstd.ap[:, 0:n], ALU.mult, ALU.mult,
                             [xT.res[bidx], rstd.r, sm], [tmp.r])
                self.P.dma("sp", xout_v[:, :, c0:c0 + n], tmp.ap[:, :, 0:n], reads=[tmp.r], writes=[self.out_res])
        P.barrier()


def build_layer(layer, taps=()):
    K = LayerK2(layer, taps=taps)
    K.setup_consts()
    K.phase_mod()
    K.xT_d = K.din("xT", [D, SEQ])
    K.cxT_d = K.din("cxT", [D, CTX])
    K.xT_v = K.xT_d.rearrange("(k p) c -> p k c", p=128)
    K.cxT_v = K.cxT_d.rearrange("(k p) c -> p k c", p=128)
    K.phase_fg()
    K.phase_h()
    K.phase_f()
    if K.L0:
        K.phase_ctxhy()
    K.A.release(K.mark_hyw)
    K.phase_kvq()
    K.phase_att()
    K.A.release(K.mark_att)
    K.phase_m()
    K.A.release(K.mark_ffn)
    K.phase_ffn()
    nc = K.finish()
    return K, nc


class FusedK(LayerK2):
    def __init__(self, taps=()):
        super().__init__(0, taps=taps)
        self.fused = True

    def set_layer(self, l):
        self.layer = l
        self.L0 = l == 0
        self.last = l == 1

    def setup_exchange(self):
        nc = self.nc
        A = self.A
        self.bounce_d = nc.dram_tensor("xbounce", [2 * D, OWN], F32, kind="Internal").ap()
        self.gath_d = nc.dram_tensor("xgath", [2 * D, OWN], F32, kind="Internal").ap()
        self.x1T_d = nc.dram_tensor("x1T_scr", [D, SEQ], F32, kind="Internal").ap()
        self.c1T_d = nc.dram_tensor("c1T_scr", [D, CTX], F32, kind="Internal").ap()
        self.x1T_v = self.x1T_d.rearrange("(k p) c -> p k c", p=128)
        self.c1T_v = self.c1T_d.rearrange("(k p) c -> p k c", p=128)
        sm_d = self.din("smask", [128, 2])
        self.smask = A.f32(2)
        self.load("sp", self.smask, sm_d, [self.small_res_c])
        self.J = Buf(A.f32(128), "J")
        self.memset("pool", self.J.ap, 0.0, [self.J.r])
        self.P.op("pool", lambda e: e.affine_select(out=self.J.ap, in_=self.J.ap, pattern=[[1, 128]], base=-127,
                                                    channel_multiplier=1, compare_op=ALU.not_equal, fill=1.0),
                  [self.J.r], [self.J.r])

    def exchange_start(self):
        P = self.P
        self.gr = []
        nchunk = 8
        rr = (2 * D) // nchunk
        order = [0, 4, 1, 5, 2, 6, 3, 7]
        self.gr = {}
        for i in order:
            g = Res(f"gath{i}")
            self.gr[i] = g
            P.cc(lambda e, i=i: e.collective_compute(
                "AllReduce", ALU.add, replica_groups=[[0, 1], [2, 3], [4, 5], [6, 7]],
                ins=[self.bounce_d[i * rr:(i + 1) * rr, :].opt()], outs=[self.gath_d[i * rr:(i + 1) * rr, :].opt()]),
                reads=[self.out_res], writes=[g])

    def exchange(self):
        A, P = self.A, self.P
        m0 = A.mark()
        a = [Buf(A.f32(TB), f"xa{i}") for i in range(2)]
        b = [Buf(A.f32(TB), f"xb{i}") for i in range(2)]
        Tt = [Buf(A.f32(TB), f"xT{i}") for i in range(2)]
        o = [Buf(A.f32(TB), f"xo{i}") for i in range(2)]
        sres = self.small_res_c
        it = 0
        for k in range(8):
            for jb in range(4):
                i2 = it % 2
                it += 1
                self.P.dma("sp", a[i2].ap, self.gath_d[k * 128:(k + 1) * 128, jb * TB:(jb + 1) * TB],
                           reads=[self.gr[k // 2]], writes=[a[i2].r])
                self.P.dma("act", b[i2].ap, self.gath_d[D + k * 128:D + (k + 1) * 128, jb * TB:(jb + 1) * TB],
                           reads=[self.gr[4 + k // 2]], writes=[b[i2].r])
                self.ts("dve", a[i2].ap, a[i2].ap, self.smask[:, 1:2], None, ALU.mult, None, [sres], [a[i2].r])
                self.stt(a[i2].ap, b[i2].ap, self.smask[:, 0:1], a[i2].ap, ALU.mult, ALU.add, [b[i2].r, sres], [a[i2].r])
                pt, po = self.ps[2 * i2], self.ps[2 * i2 + 1]
                for q in range(4):
                    self.tr(pt.ap[:, q * 128:(q + 1) * 128], a[i2].ap[:, q * 128:(q + 1) * 128], self.ident_f.ap,
                            [a[i2].r, self.ident_f.r], [pt.r])
                self.cp("act", Tt[i2].ap, pt.ap, [pt.r], [Tt[i2].r])
                for q in range(4):
                    self.mm(po.ap[:, (3 - q) * 128:(4 - q) * 128], Tt[i2].ap[:, q * 128:(q + 1) * 128], self.J.ap,
                            True, True, [Tt[i2].r, self.J.r], [po.r], inc=(q == 3))
                self.cp("dve", o[i2].ap, po.ap, [po.r], [o[i2].r])
                self.P.dma("sp", self.x1T_d[k * 128:(k + 1) * 128, OWN + (3 - jb) * TB:OWN + (4 - jb) * TB], o[i2].ap,
                           reads=[o[i2].r], writes=[Res("x1w")])
        P.barrier(with_cc=True)
        A.release(m0)


def build_fused(taps=()):
    K = FusedK(taps=taps)
    K.setup_consts()
    K.small_res_c = Res("smallc")
    K.setup_exchange()
    mark_base = K.A.mark()
    for l in range(2):
        K.set_layer(l)
        K.A.release(mark_base)
        K.phase_mod()
        if l == 0:
            K.xT_d = K.din("xT", [D, SEQ])
            K.cxT_d = K.din("cxT", [D, CTX])
            K.xT_v = K.xT_d.rearrange("(k p) c -> p k c", p=128)
            K.cxT_v = K.cxT_d.rearrange("(k p) c -> p k c", p=128)
        else:
            K.xT_v = K.x1T_v
            K.cxT_v = K.c1T_v
        K.phase_fg()
        if l == 1:
            K.exchange()
        K.phase_h()
        K.phase_f()
        if K.L0:
            K.phase_ctxhy()
        K.A.release(K.mark_hyw)
        K.phase_kvq()
        K.phase_att()
        K.A.release(K.mark_att)
        K.phase_m()
        K.A.release(K.mark_ffn)
        K.phase_ffn()
        if l == 0:
            K.exchange_start()
            K.P.barrier(with_cc=True)
    nc = K.finish()
    return K, nc

GRID_W = 64
KV_START = 384
HY_START = 384 + 256 + 32
GATE_START = HY_START + 1536


def chunked(vec, nchunk):
    return np.ascontiguousarray(vec.reshape(nchunk, 128).T)


def rope_tables(hf):
    n_freq = 8
    inv_freq = (10000.0 ** (-np.arange(n_freq, dtype=np.float32) / n_freq)).astype(np.float32)
    t = np.arange(SEQ)
    pos = t if hf == 0 else (SEQ - 1 - t)
    r = (pos // GRID_W).astype(np.float32)
    c = (pos % GRID_W).astype(np.float32)
    ang = np.concatenate([r[:, None] * inv_freq, c[:, None] * inv_freq], axis=-1).astype(np.float32)
    cos = np.cos(ang).astype(np.float32).T
    sin = np.sin(ang).astype(np.float32).T
    C = np.zeros((128, SEQ), np.float32)
    S = np.zeros((128, SEQ), np.float32)
    C[64:80] = cos; C[80:96] = cos
    S[64:80] = -sin; S[80:96] = sin
    return C, S


def layer_inputs(inp, l, b, hf, xT_b, cxT_b, names):
    out = {}
    f = np.float32
    w_in = inp["w_in"][l]

    def need(n):
        return n in names

    if need("xT"):
        out["xT"] = np.ascontiguousarray(xT_b if hf == 0 else xT_b[:, ::-1])
    if need("cxT"):
        out["cxT"] = np.ascontiguousarray(cxT_b)
    if need("cond"):
        out["cond"] = np.ascontiguousarray(np.stack([chunked(inp["c"][b], 8), chunked(inp["c_ctx"], 8)], axis=-1))
    if need("w_mod"):
        out["w_mod"] = np.ascontiguousarray(inp["w_mod"][l])
    if need("b_mod2"):
        bm = chunked(inp["b_mod"][l], 48)
        out["b_mod2"] = np.ascontiguousarray(np.stack([bm, bm], axis=-1))
    for nm, key in (("n1g2", "norm1_g"), ("n2g2", "norm2_g")):
        if need(nm):
            g = chunked(inp[key][l], 8)
            out[nm] = np.ascontiguousarray(np.stack([g, g], axis=-1))
    if need("w_kvq"):
        w = np.zeros((D, 7 * 128), f)
        w[:, 0:256] = w_in[:, KV_START:KV_START + 256]
        ra = w_in[:, KV_START + 256:KV_START + 272]
        rb = w_in[:, KV_START + 272:KV_START + 288]
        w[:, 256 + 64:256 + 80] = ra; w[:, 256 + 80:256 + 96] = rb
        w[:, 384 + 64:384 + 80] = rb; w[:, 384 + 80:384 + 96] = ra
        w[:, 512:896] = w_in[:, 0:384]
        out["w_kvq"] = w
    if need("g_kv"):
        out["g_kv"] = chunked(inp["kv_norm_g"][l], 2)
    if need("g_q"):
        out["g_q"] = chunked(inp["q_norm_g"][l], 3)
    if need("ropeC") or need("ropeS"):
        C, S = rope_tables(hf)
        out["ropeC"] = C; out["ropeS"] = S
    if need("w_uqA"):
        wq = inp["w_uq"][l].reshape(QL, NH, 96)
        A = wq.copy()
        B = np.zeros_like(wq)
        B[:, :, 64:80] = wq[:, :, 80:96]
        B[:, :, 80:96] = wq[:, :, 64:80]
        out["w_uqA"] = np.ascontiguousarray(A.reshape(QL, NH * 96))
        out["w_uqB"] = np.ascontiguousarray(B.reshape(QL, NH * 96))
    if need("w_uk"):
        out["w_uk"] = np.ascontiguousarray(inp["w_uk"][l])
    if need("w_uv"):
        out["w_uv"] = np.ascontiguousarray(inp["w_uv"][l])
    return out


def zfeat(n):
    bands = 8
    t = np.linspace(0.0, 1.0, n, dtype=np.float32)[:, None]
    phase = (np.float32(2.0 * math.pi / n) * np.arange(n, dtype=np.float32)[:, None]
             * np.linspace(1e-4, bands - 1, bands, dtype=np.float32)).astype(np.float32)
    z = np.concatenate([t, np.cos(phase), -np.sin(phase)], -1).astype(np.float32)
    return z, t[:, 0]


_FFT = {}


def fft_tables():
    if _FFT:
        return _FFT
    N = 8192
    n1 = np.arange(128)[:, None].astype(np.float64)
    f1 = (np.arange(128)[None, :] + 0.5)
    th = 2 * np.pi * n1 * f1 / 256
    _FFT["CS1a"] = np.concatenate([np.cos(th), -np.sin(th)], 1).astype(np.float32)
    th = 2 * np.pi * (n1 + 128) * f1 / 256
    _FFT["CS1b"] = np.concatenate([np.cos(th), -np.sin(th)], 1).astype(np.float32)
    p = np.arange(128)
    n2 = (p // 4)[:, None].astype(np.float64)
    th = 2 * np.pi * n2 * f1 / N
    Tr, Ti = np.cos(th), -np.sin(th)
    _FFT["T1"] = np.concatenate([Tr, Tr], 1).astype(np.float32)
    _FFT["T2"] = np.concatenate([Ti, Ti], 1).astype(np.float32)
    a = (p // 4); c = p % 4
    same = (c[:, None] == c[None, :]).astype(np.float64)
    th = 2 * np.pi * a[:, None] * a[None, :] / 32
    _FFT["W3r"] = (same * np.cos(th)).astype(np.float32)
    _FFT["W3i"] = (same * -np.sin(th)).astype(np.float32)
    _FFT["W3n"] = (same * np.sin(th)).astype(np.float32)
    n1p = np.arange(64)[None, :].astype(np.float64)
    f1c = (np.arange(128)[:, None] + 0.5)
    th = 2 * np.pi * n1p * f1c / 256
    _FFT["ICr"] = (2.0 / N * np.cos(th)).astype(np.float32)
    _FFT["ICi"] = (-2.0 / N * np.sin(th)).astype(np.float32)
    return _FFT


def hyena_inputs(inp, l, hf, names):
    out = {}
    f = np.float32
    w_in = inp["w_in"][l]
    if "zfT_f" in names:
        z, t = zfeat(SEQ)
        out["zfT_f"] = np.ascontiguousarray(z.T)
        idx = (SEQ - np.arange(SEQ)) % SEQ
        out["zfT_b"] = np.ascontiguousarray(z[idx].T)
        out["t_f"] = np.ascontiguousarray(t[None, :])
        out["t_b"] = np.ascontiguousarray(t[idx][None, :])
    if "hy_w1" in names:
        out["hy_w1"] = np.ascontiguousarray(inp["hy_w1"][l])
        out["hy_w2"] = np.ascontiguousarray(inp["hy_w2"][l])
        w3 = inp["hy_w3"][l].reshape(64, 2, HYW)
        dec = inp["hy_decay"][l]
        if hf == 1:
            w3 = w3[:, ::-1, :]
            dec = dec[::-1]
        out["hy_w3"] = np.ascontiguousarray(w3.reshape(64, 1024))
        out["hy_decay"] = np.ascontiguousarray(dec.reshape(1, 1024))
        out["hy_fq"] = np.ascontiguousarray(np.stack([inp["hy_freq"][l], inp["hy_b1"][l], inp["hy_b2"][l]], -1))
        out["hy_bias"] = np.ascontiguousarray(inp["hy_bias"][l][None, :])
    if "w_hy" in names:
        out["w_hy"] = np.ascontiguousarray(w_in[:, HY_START:GATE_START])
        sw = inp["hy_short_w"][l]
        if hf == 1:
            sw = sw[::-1]
        sb = inp["hy_short_b"][l]
        arr = np.concatenate([sw, sb[None]], 0)
        out["hy_sw"] = np.ascontiguousarray(arr.reshape(4, 12, 128).transpose(2, 1, 0))
    T = fft_tables()
    for k in T:
        if k in names:
            out[k] = T[k]
    return out


def rest_inputs(inp, l, hf, names):
    out = {}
    w_in = inp["w_in"][l]
    if "zfT_c" in names:
        z, t = zfeat(CTX)
        out["zfT_c"] = np.ascontiguousarray(z.T)
        out["tc_bc"] = np.ascontiguousarray(np.broadcast_to(t[None, :], (128, CTX)))
        out["hyc_w3"] = np.ascontiguousarray(inp["hy_w3"][l])
        dec = inp["hy_decay"][l]
        out["hyc_dec"] = np.ascontiguousarray(dec.reshape(2, 4, 128).transpose(2, 0, 1).reshape(128, 8))
        out["hyc_bias"] = chunked(inp["hy_bias"][l], 4)
    if "hy_swc" in names:
        sw = inp["hy_short_w"][l]
        sb = inp["hy_short_b"][l]
        arr = np.concatenate([sw, sb[None]], 0)
        out["hy_swc"] = np.ascontiguousarray(arr.reshape(4, 12, 128).transpose(2, 1, 0))
    if "w_gate" in names:
        out["w_gate"] = np.ascontiguousarray(w_in[:, GATE_START:])
        out["w_br_attn"] = np.ascontiguousarray(inp["w_br_attn"][l])
        out["w_br_hy"] = np.ascontiguousarray(inp["w_br_hy"][l])
        out["w_out"] = np.ascontiguousarray(inp["w_out"][l])
    i = l // 2
    if "ffn_w_gate" in names:
        out["ffn_w_gate"] = inp["ffn_w_gate"][i:i + 1]
        out["ffn_w_up"] = inp["ffn_w_up"][i:i + 1]
        out["ffn_w_down"] = inp["ffn_w_down"][i:i + 1]
    if "moe_w_gate" in names:
        out["moe_w_gate"] = inp["moe_w_gate"][i]
        out["moe_w_up"] = inp["moe_w_up"][i]
        out["moe_w_down"] = inp["moe_w_down"][i]
        out["moe_router"] = np.ascontiguousarray(inp["moe_router"][i].reshape(8, 128, NE).transpose(1, 0, 2))
        out["final_g"] = chunked(inp["final_g"], 8)
    return out


def all_inputs(inp, l, b, hf, xT_b, cxT_b, names):
    im = layer_inputs(inp, l, b, hf, xT_b, cxT_b, names)
    im.update(hyena_inputs(inp, l, hf, names))
    im.update(rest_inputs(inp, l, hf, names))
    return im


_FCACHE = {}


def _get_fused():
    if "k" not in _FCACHE:
        _FCACHE["k"] = build_fused()
    return _FCACHE["k"]


def kernel(**inputs):
    inp = {k: np.asarray(v, dtype=np.float32) for k, v in inputs.items()}
    B = inp["x"].shape[0]
    K, nc = _get_fused()
    all_names = set(K.in_shapes)
    shared = {n for n in all_names if not (n.startswith("L0_") or n.startswith("L1_"))}
    lnames = {l: {n[3:] for n in all_names if n.startswith(f"L{l}_")} for l in range(2)}
    xT = [np.ascontiguousarray(inp["x"][b].T) for b in range(B)]
    cxT = [np.ascontiguousarray(inp["ctx"][b].T) for b in range(B)]
    in_maps = []
    for core in range(8):
        b, hf = core // 2, core % 2
        m = {}
        im = all_inputs(inp, 0, b, hf, xT[b], cxT[b], shared | lnames[0])
        for n in shared:
            if n == "smask":
                sm = np.zeros((128, 2), np.float32)
                sm[:, hf] = 1.0
                m[n] = sm
            else:
                m[n] = im[n]
        for n in lnames[0]:
            m["L0_" + n] = im[n]
        im1 = all_inputs(inp, 1, b, hf, xT[b], cxT[b], lnames[1])
        for n in lnames[1]:
            m["L1_" + n] = im1[n]
        for k, s in K.in_shapes.items():
            assert m[k].shape == s, (k, m[k].shape, s)
            m[k] = np.ascontiguousarray(m[k], dtype=np.float32)
        in_maps.append(m)
    res = run_bass_kernel_spmd(nc, in_maps, core_ids=list(range(8)))
    r1 = res.results
    out = np.empty((B, SEQ, D), np.float32)
    for b in range(B):
        lo = np.asarray(r1[2 * b]["xout"])
        hi = np.asarray(r1[2 * b + 1]["xout"])[:, ::-1]
        out[b] = np.concatenate([lo, hi], axis=1).T
    return out
```

```python
from concourse.bass_utils import run_bass_kernel_spmd
import math
import numpy as np
from contextlib import ExitStack
import concourse.bass as bass
import concourse.mybir as mybir

F32 = mybir.dt.float32
BF16 = mybir.dt.bfloat16
AF = mybir.ActivationFunctionType
ALU = mybir.AluOpType
AX = mybir.AxisListType

import os
ENGS = ("pe", "act", "dve", "pool", "sp")
SKIP_SAME = set(os.environ.get("MK_SKIP_SAME", "").split(",")) - {""}
NDMA = 8

D = 1024
SEQ = 4096
CTX = 256
OWN = 2048
NH = 8
QL = 384
KVL = 256
ROPE = 32
HYW = 512
DFF = 2816
NE = 8
EPS = 1e-6
ATT_SCALE = (64 + 32) ** -0.5
TB = 512
ARENA_WORDS = 44500


class Res:
    __slots__ = ("name", "w", "r")

    def __init__(self, name="r"):
        self.name = name
        self.w = None
        self.r = {}


class Prog:
    def __init__(self, nc):
        self.nc = nc
        self.q = {k: [] for k in ENGS}
        self.cnt = {}
        self.known = {k: {} for k in ENGS}
        self.semnames = list(ENGS) + ["ccsem"]
        for q in ("sp", "pool", "act"):
            for i in range(NDMA):
                self.semnames.append(f"d_{q}_{i}")
        for s in self.semnames:
            self.cnt[s] = 0
        self.dma_rr = {"sp": 0, "pool": 0, "act": 0}
        self.sems = {}
        self.n_ops = 0

    def _need(self, eng, deps):
        best = {}
        for (s, t) in deps:
            if t > best.get(s, 0):
                best[s] = t
        for s, t in best.items():
            if eng == "pe" and s == "pe":
                continue
            if s == eng and eng in SKIP_SAME:
                continue
            if self.known[eng].get(s, 0) >= t:
                continue
            self.known[eng][s] = t
            self.q[eng].append(("wait", s, t))

    def _collect(self, reads, writes):
        deps = []
        assert not isinstance(reads, Res) and not isinstance(writes, Res)
        for r in reads:
            if r.w is not None:
                deps.append(r.w)
        for w in writes:
            if w.w is not None:
                deps.append(w.w)
            for s, t in w.r.items():
                deps.append((s, t))
        return deps

    def _mark(self, semkey, ticket, reads, writes):
        for r in reads:
            if r.r.get(semkey, 0) < ticket:
                r.r[semkey] = ticket
        for w in writes:
            w.w = (semkey, ticket)
            w.r = {}

    def op(self, eng, fn, reads=(), writes=(), inc=True):
        self.n_ops += 1
        self._need(eng, self._collect(reads, writes))
        ticket = self.cnt[eng] + 1
        if inc:
            self.cnt[eng] = ticket
        self.q[eng].append(("op", fn, inc))
        self._mark(eng, ticket, reads, writes)

    def dma(self, q, out, in_, reads=(), writes=(), **kw):
        self.n_ops += 1
        i = self.dma_rr[q]
        self.dma_rr[q] = (i + 1) % NDMA
        s = f"d_{q}_{i}"
        deps = self._collect(reads, writes)
        if self.cnt[s] > 0:
            deps.append((s, self.cnt[s]))
        self._need(q, deps)
        ticket = self.cnt[s] + 16
        self.cnt[s] = ticket
        self.q[q].append(("dma", out, in_, s, kw))
        self._mark(s, ticket, reads, writes)

    def cc(self, fn, reads=(), writes=()):
        q = "pool"
        s = "ccsem"
        deps = self._collect(reads, writes)
        if self.cnt[s] > 0:
            deps.append((s, self.cnt[s]))
        self._need(q, deps)
        ticket = self.cnt[s] + 1
        self.cnt[s] = ticket
        self.q[q].append(("cc", fn, s))
        self._mark(s, ticket, reads, writes)

    def barrier(self, with_cc=False):
        for e in ENGS:
            deps = [(s, c) for s, c in self.cnt.items() if c > 0 and (s != "ccsem" or with_cc)]
            self._need(e, deps)

    def emit(self, block):
        sems = self.sems

        def run(eng_name, engine):
            for item in self.q[eng_name]:
                if item[0] == "wait":
                    engine.wait_ge(sems[item[1]], item[2])
                elif item[0] == "op":
                    ins = item[1](engine)
                    if item[2]:
                        ins.then_inc(sems[eng_name], 1)
                elif item[0] == "cc":
                    item[1](engine).then_inc(sems[item[2]])
                else:
                    _, out, in_, s, kw = item
                    engine.dma_start(out=out, in_=in_, **kw).then_inc(sems[s], 16)

        @block.sync
        def _(e):
            run("sp", e)

        @block.scalar
        def _(e):
            run("act", e)

        @block.vector
        def _(e):
            run("dve", e)

        @block.gpsimd
        def _(e):
            run("pool", e)

        @block.tensor
        def _(e):
            run("pe", e)


class Arena:
    def __init__(self, t):
        self.t = t
        self.top = 0
        self.hi = 0

    def mark(self):
        return self.top

    def release(self, m):
        self.top = m

    def f32(self, words, shape=None):
        a = self.t[:, self.top:self.top + words]
        self.top += words
        self.hi = max(self.hi, self.top)
        assert self.top <= ARENA_WORDS, f"arena overflow {self.top}"
        return a

    def bf16(self, elems):
        words = (elems + 1) // 2
        a = self.t[:, self.top:self.top + words].bitcast(BF16)
        self.top += words
        self.hi = max(self.hi, self.top)
        assert self.top <= ARENA_WORDS, f"arena overflow {self.top}"
        return a


class Buf:
    def __init__(self, ap, name="b", nres=1):
        self.ap = ap
        self.res = [Res(f"{name}{i}") for i in range(nres)]

    @property
    def r(self):
        return self.res[0]


def _bind(f, *a, **k):
    return lambda e: f(e, *a, **k)


SHARED_INPUTS = {"ropeC", "ropeS", "zfT_f", "zfT_b", "t_f", "t_b", "CS1a", "CS1b", "W3r", "W3i", "W3n", "T1", "T2",
                 "ICr", "ICi", "zfT_c", "tc_bc", "cond", "xT", "cxT", "smask"}


class LayerK:
    def __init__(self, layer, taps=(), upto=None):
        self.layer = layer
        self.L0 = layer == 0
        self.last = layer == 1
        self.taps = set(taps)
        self.upto = upto
        self.nc = bass.Bass("TRN2", target_bir_lowering=False)
        self.es = ExitStack()
        self.in_shapes = {}
        self.out_shapes = {}
        nc = self.nc
        self.arena_t = self.es.enter_context(nc.sbuf_tensor("arena", [128, ARENA_WORDS], F32))
        self.A = Arena(self.arena_t)
        self.P = Prog(nc)
        for s in self.P.semnames:
            self.P.sems[s] = self.es.enter_context(nc.semaphore(s))
        self.ps = []
        for i in range(8):
            t = self.es.enter_context(nc.psum_tensor(f"ps{i}", [128, 512], F32))
            self.ps.append(Buf(t[:, :], f"ps{i}"))
        self.tapq = 0

    def din(self, name, shape, dt=F32):
        if not hasattr(self, "_in_aps"):
            self._in_aps = {}
        key = name if (name in SHARED_INPUTS or not getattr(self, "fused", False)) else f"L{self.layer}_{name}"
        if key in self._in_aps:
            assert self.in_shapes[key] == tuple(shape), (key, shape)
            self._in_aps[name] = self._in_aps[key]
            return self._in_aps[key]
        t = self.nc.dram_tensor(key, list(shape), dt, kind="ExternalInput").ap()
        self.in_shapes[key] = tuple(shape)
        self._in_aps[key] = t
        self._in_aps[name] = t
        return t

    def dout(self, name, shape, dt=F32):
        t = self.nc.dram_tensor(name, list(shape), dt, kind="ExternalOutput").ap()
        self.out_shapes[name] = tuple(shape)
        return t

    def tap(self, name, ap, res, dt=F32):
        if name not in self.taps:
            return
        shape = list(ap.shape)
        d = self.dout("tap_" + name, shape, dt)
        self.P.dma("sp", d, ap, reads=res)

    def mm(self, out, lhsT, rhs, start, stop, reads, writes, inc=None):
        if inc is None:
            inc = stop
        self.P.op("pe", lambda e: e.matmul(out, lhsT=lhsT, rhs=rhs, start=start, stop=stop),
                  reads, writes, inc)

    def tr(self, out, in_, ident, reads, writes):
        self.P.op("pe", lambda e: e.transpose(out, in_, ident), reads, writes, True)

    def act(self, out, in_, func, reads, writes, scale=1.0, bias=0.0):
        self.P.op("act", lambda e: e.activation(out=out, in_=in_, func=func, bias=bias, scale=scale),
                  reads, writes)

    def tt(self, eng, out, in0, in1, op, reads, writes):
        self.P.op(eng, lambda e: e.tensor_tensor(out=out, in0=in0, in1=in1, op=op), reads, writes)

    def ts(self, eng, out, in0, s1, s2, op0, op1, reads, writes):
        if op1 is None:
            self.P.op(eng, lambda e: e.tensor_scalar(out=out, in0=in0, scalar1=s1, scalar2=None, op0=op0),
                      reads, writes)
        else:
            self.P.op(eng, lambda e: e.tensor_scalar(out=out, in0=in0, scalar1=s1, scalar2=s2, op0=op0, op1=op1),
                      reads, writes)

    def stt(self, out, in0, scalar, in1, op0, op1, reads, writes):
        self.P.op("dve", lambda e: e.scalar_tensor_tensor(out=out, in0=in0, scalar=scalar, in1=in1, op0=op0, op1=op1),
                  reads, writes)

    def cp(self, eng, out, in_, reads, writes):
        if eng == "act":
            self.P.op("act", lambda e: e.copy(out=out, in_=in_), reads, writes)
        else:
            self.P.op(eng, lambda e: e.tensor_copy(out=out, in_=in_), reads, writes)

    def memset(self, eng, ap, val, writes):
        self.P.op(eng, lambda e: e.memset(ap, val), (), writes)

    def recip(self, out, in_, reads, writes):
        self.P.op("dve", lambda e: e.reciprocal(out=out, in_=in_), reads, writes)

    def load(self, q, out, in_, writes, **kw):
        self.P.dma(q, out, in_, writes=writes, **kw)

    def loadw_bf16(self, dst3, src2, kchunks, res):
        for k in range(kchunks):
            self.P.dma("pool", dst3[:, k, :], src2[k * 128:(k + 1) * 128, :], writes=[res])

    def setup_consts(self):
        A = self.A
        self.ones_f = Buf(A.f32(128), "ones")
        self.memset("pool", self.ones_f.ap, 1.0, [self.ones_f.r])
        self.ident_f = Buf(A.f32(128), "identf")
        self.memset("pool", self.ident_f.ap, 0.0, [self.ident_f.r])
        self.P.op("pool", lambda e: e.affine_select(out=self.ident_f.ap, in_=self.ident_f.ap, pattern=[[-1, 128]],
                                                    base=0, channel_multiplier=1, compare_op=ALU.not_equal, fill=1.0),
                  [self.ident_f.r], [self.ident_f.r])
        self.ident_b = Buf(A.bf16(128), "identb")
        self.cp("dve", self.ident_b.ap, self.ident_f.ap, [self.ident_f.r], [self.ident_b.r])

    def rms_bcast(self, src3, C, n, dim, rstd, sq3, src_res, rstd_res, sq_res, psb):
        self.act(sq3, src3, AF.Square, [src_res], [sq_res])
        ss = rstd
        self.P.op("dve", lambda e: e.tensor_reduce(out=ss, in_=sq3.rearrange("p c t -> p t c"), axis=AX.X, op=ALU.add),
                  [sq_res], [rstd_res])
        self.mm(psb.ap[:, 0:n], self.ones_f.ap, ss, True, True, [rstd_res, self.ones_f.r], [psb.r])
        self.act(rstd, psb.ap[:, 0:n], AF.Sqrt, [psb.r], [rstd_res], scale=1.0 / dim, bias=EPS)
        self.recip(rstd, rstd, [rstd_res], [rstd_res])

    def norm_mod(self, xb3, n, gs, sh, col, hT3, tmp3, rstd, xb_res, h_res, tmp_res, rstd_res, psb, hf3=None, hf_res=None):
        self.rms_bcast(xb3, 8, n, D, rstd, tmp3, xb_res, rstd_res, tmp_res, psb)
        for k in range(8):
            self.stt(tmp3[:, k, :], xb3[:, k, :], gs[:, k, col:col + 1], rstd, ALU.mult, ALU.mult,
                     [xb_res, rstd_res, self.small_res], [tmp_res])
        for k in range(8):
            self.act(hT3[:, k, :], tmp3[:, k, :], AF.Identity, [tmp_res, self.small_res], [h_res],
                     bias=sh[:, k, col:col + 1])
            if hf3 is not None:
                self.act(hf3[:, k, :], tmp3[:, k, :], AF.Identity, [tmp_res, self.small_res], [hf_res],
                         bias=sh[:, k, col:col + 1])

    def phase_mod(self):
        A, P = self.A, self.P
        l = self.layer
        self.small_res = Res("small")
        sm = self.small_res
        cond_d = self.din("cond", [128, 8, 2])
        wmod_d = self.din("w_mod", [D, 6 * D])
        bmod_d = self.din("b_mod2", [128, 48, 2])
        n1g_d = self.din("n1g2", [128, 8, 2])
        n2g_d = self.din("n2g2", [128, 8, 2])
        self.mod = A.f32(96).rearrange("p (j c) -> p j c", c=2)
        self.gs1 = A.f32(16).rearrange("p (j c) -> p j c", c=2)
        self.gs2 = A.f32(16).rearrange("p (j c) -> p j c", c=2)
        bmod = A.f32(96).rearrange("p (j c) -> p j c", c=2)
        n1g = A.f32(16).rearrange("p (j c) -> p j c", c=2)
        n2g = A.f32(16).rearrange("p (j c) -> p j c", c=2)
        scond = A.f32(16).rearrange("p (j c) -> p j c", c=2)
        m0 = A.mark()
        sc_res = Res("scond")
        self.load("sp", scond, cond_d, [sc_res])
        self.load("sp", bmod, bmod_d, [sm])
        self.load("sp", n1g, n1g_d, [sm])
        self.load("sp", n2g, n2g_d, [sm])
        self.act(scond, scond, AF.Silu, [sc_res], [sc_res])
        wb = [Buf(A.f32(8 * 512).rearrange("p (k c) -> p k c", k=8), f"wm{i}") for i in range(2)]
        psb = self.ps[0]
        wview = wmod_d.rearrange("(k p) c -> p k c", p=128)
        for blk in range(12):
            w = wb[blk % 2]
            self.load("sp" if blk % 2 == 0 else "act", w.ap, wview[:, :, blk * 512:(blk + 1) * 512], [w.r])
            for jj in range(4):
                j = blk * 4 + jj
                for k in range(8):
                    self.mm(psb.ap[:, 2 * j:2 * j + 2], w.ap[:, k, jj * 128:(jj + 1) * 128], scond[:, k, :],
                            k == 0, k == 7, [w.r, sc_res], [psb.r])
        self.tt("dve", self.mod, psb.ap[:, 0:96].rearrange("p (j c) -> p j c", c=2), bmod, ALU.add, [psb.r, sm], [sm])
        self.ts("dve", self.gs1, self.mod[:, 8:16, :], 1.0, None, ALU.add, None, [sm], [sm])
        self.tt("dve", self.gs1, self.gs1, n1g, ALU.mult, [sm], [sm])
        self.ts("dve", self.gs2, self.mod[:, 32:40, :], 1.0, None, ALU.add, None, [sm], [sm])
        self.tt("dve", self.gs2, self.gs2, n2g, ALU.mult, [sm], [sm])
        self.sh1 = self.mod[:, 0:8, :]
        self.g1 = self.mod[:, 16:24, :]
        self.sh2 = self.mod[:, 24:32, :]
        self.g2 = self.mod[:, 40:48, :]
        self.tap("mod", self.mod, [sm])
        self.tap("gs1", self.gs1, [sm])
        P.barrier()
        A.release(m0)
        self.mark_ffn = A.mark()

    def xblock_src(self, bi):
        if bi < 8:
            return self.xT_v[:, :, bi * TB:(bi + 1) * TB], TB, 0
        return self.cxT_v, CTX, 1

    def phase_kvq(self):
        A, P = self.A, self.P
        wkvq_d = self.din("w_kvq", [D, 7 * 128])
        gkv_d = self.din("g_kv", [128, 2])
        gq_d = self.din("g_q", [128, 3])
        ropeC_d = self.din("ropeC", [128, SEQ])
        ropeS_d = self.din("ropeS", [128, SEQ])
        NK = CTX + SEQ
        self.NK = NK
        nq = OWN + (CTX if self.L0 else 0)
        self.nq = nq
        nq = OWN + (CTX if self.L0 else 0)
        self.nq = nq
        self.oT = Buf(A.bf16(4 * nq).rearrange("p (k t) -> p k t", k=4), "oT")
        self.mark_att = A.mark()
        self.ckvT = Buf(A.bf16(2 * NK).rearrange("p (k t) -> p k t", k=2), "ckvT")
        self.kT = [Buf(A.bf16(NK), f"kT{i}") for i in range(2)]
        self.qnT = Buf(A.bf16(3 * nq).rearrange("p (k t) -> p k t", k=3), "qnT")
        gkv = A.f32(2)
        gq = A.f32(3)
        m0 = A.mark()
        self.load("sp", gkv, gkv_d, [self.small_res])
        self.load("sp", gq, gq_d, [self.small_res])
        w = Buf(A.bf16(8 * 896).rearrange("p (k c) -> p k c", k=8), "wkvq")
        self.loadw_bf16(w.ap, wkvq_d, 8, w.r)
        xb = [Buf(A.f32(8 * TB).rearrange("p (k t) -> p k t", k=8), f"xb{i}") for i in range(1)]
        tmp = Buf(A.f32(8 * TB).rearrange("p (k t) -> p k t", k=8), "tmp")
        hTs = [Buf(A.bf16(8 * TB).rearrange("p (k t) -> p k t", k=8), f"hT{i}") for i in range(2)]
        rstd = Buf(A.f32(TB), "rstd")
        pp = Buf(A.f32(3 * TB).rearrange("p (k t) -> p k t", k=3), "pp")
        sq = Buf(A.f32(3 * TB).rearrange("p (k t) -> p k t", k=3), "sq")
        rs2 = Buf(A.f32(TB), "rs2")
        tabC = Buf(A.f32(TB), "tabC")
        tabS = Buf(A.f32(TB), "tabS")
        t1 = Buf(sq.ap[:, 0, :], "t1")
        t2 = Buf(sq.ap[:, 1, :], "t2")
        t1.res = sq.res
        t2.res = sq.res
        sm = self.small_res
        order = [8] + list(range(8))

        def prep(it_):
            bi_ = order[it_]
            src_, n_, col_ = self.xblock_src(bi_)
            hT_ = hTs[it_ % 2]
            x_ = xb[0]
            self.load("sp", x_.ap[:, :, 0:n_], src_, [x_.r])
            self.norm_mod(x_.ap[:, :, 0:n_], n_, self.gs1, self.sh1, col_, hT_.ap[:, :, 0:n_], tmp.ap[:, :, 0:n_],
                          rstd.ap[:, 0:n_], x_.r, hT_.r, tmp.r, rstd.r, self.ps[0])

        prep(0)
        for it, bi in enumerate(order):
            src, n, col = self.xblock_src(bi)
            hT = hTs[it % 2]
            if it + 1 < len(order):
                prep(it + 1)
            if bi == 0:
                self.tap("hT0", hT.ap, [hT.r], BF16)
            koff = 0 if bi == 8 else CTX + bi * TB
            for g in range(2):
                psb = self.ps[1 + g]
                for k in range(8):
                    self.mm(psb.ap[:, 0:n], w.ap[:, k, g * 128:(g + 1) * 128], hT.ap[:, k, 0:n], k == 0, k == 7,
                            [w.r, hT.r], [psb.r])
                self.cp("act", pp.ap[:, g, 0:n], psb.ap[:, 0:n], [psb.r], [pp.r])
            self.rms_bcast(pp.ap[:, 0:2, 0:n], 2, n, KVL, rs2.ap[:, 0:n], sq.ap[:, 0:2, 0:n], pp.r, rs2.r, sq.r, self.ps[3])
            for g in range(2):
                self.stt(self.ckvT.ap[:, g, koff:koff + n], pp.ap[:, g, 0:n], gkv[:, g:g + 1], rs2.ap[:, 0:n],
                         ALU.mult, ALU.mult, [pp.r, rs2.r, sm], [self.ckvT.r])
            psA, psB = self.ps[4], self.ps[5]
            for k in range(8):
                self.mm(psA.ap[:, 0:n], w.ap[:, k, 256:384], hT.ap[:, k, 0:n], k == 0, k == 7, [w.r, hT.r], [psA.r])
            if bi == 8:
                self.cp("act", self.kT[0].ap[64:96, koff:koff + n], psA.ap[64:96, 0:n], [psA.r], [self.kT[0].r])
            else:
                for k in range(8):
                    self.mm(psB.ap[:, 0:n], w.ap[:, k, 384:512], hT.ap[:, k, 0:n], k == 0, k == 7, [w.r, hT.r], [psB.r])
                self.load("sp", tabC.ap[64:96, 0:n], ropeC_d[64:96, bi * TB:bi * TB + n], [tabC.r])
                self.load("sp", tabS.ap[64:96, 0:n], ropeS_d[64:96, bi * TB:bi * TB + n], [tabS.r])
                self.tt("dve", t1.ap[64:96, 0:n], psA.ap[64:96, 0:n], tabC.ap[64:96, 0:n], ALU.mult, [psA.r, tabC.r], [t1.r])
                self.tt("dve", t2.ap[64:96, 0:n], psB.ap[64:96, 0:n], tabS.ap[64:96, 0:n], ALU.mult, [psB.r, tabS.r], [t2.r])
                self.tt("dve", self.kT[0].ap[64:96, koff:koff + n], t1.ap[64:96, 0:n], t2.ap[64:96, 0:n], ALU.add,
                        [t1.r, t2.r], [self.kT[0].r])
            self.cp("act", self.kT[1].ap[64:96, koff:koff + n], self.kT[0].ap[64:96, koff:koff + n],
                    [self.kT[0].r], [self.kT[1].r])
            isq = (bi < 4) or (bi == 8 and self.L0)
            if isq:
                qoff = bi * TB if bi < 4 else OWN
                for g in range(3):
                    psb = self.ps[6 + (g % 2)]
                    for k in range(8):
                        self.mm(psb.ap[:, 0:n], w.ap[:, k, 512 + g * 128:512 + (g + 1) * 128], hT.ap[:, k, 0:n],
                                k == 0, k == 7, [w.r, hT.r], [psb.r])
                    self.cp("act", pp.ap[:, g, 0:n], psb.ap[:, 0:n], [psb.r], [pp.r])
                self.rms_bcast(pp.ap[:, 0:3, 0:n], 3, n, QL, rs2.ap[:, 0:n], sq.ap[:, 0:3, 0:n], pp.r, rs2.r, sq.r, self.ps[3])
                for g in range(3):
                    self.stt(self.qnT.ap[:, g, qoff:qoff + n], pp.ap[:, g, 0:n], gq[:, g:g + 1], rs2.ap[:, 0:n],
                             ALU.mult, ALU.mult, [pp.r, rs2.r, sm], [self.qnT.r])
        self.tap("ckvT", self.ckvT.ap, [self.ckvT.r], BF16)
        self.tap("kT", self.kT[0].ap[64:96, :], [self.kT[0].r], BF16)
        self.tap("qnT", self.qnT.ap, [self.qnT.r], BF16)
        P.barrier()
        A.release(m0)

    def phase_att(self):
        A, P = self.A, self.P
        NK, nq = self.NK, self.nq
        wuqA_d = self.din("w_uqA", [QL, NH * 96])
        wuqB_d = self.din("w_uqB", [QL, NH * 96])
        wuk_d = self.din("w_uk", [KVL, 512])
        wuv_d = self.din("w_uv", [KVL, 512])
        ropeC_d = self.in_ap("ropeC")
        ropeS_d = self.in_ap("ropeS")
        m0 = A.mark()
        wA = Buf(A.bf16(3 * 768).rearrange("p (k c) -> p k c", k=3), "wA")
        wB = Buf(A.bf16(3 * 768).rearrange("p (k c) -> p k c", k=3), "wB")
        wk = Buf(A.bf16(2 * 512).rearrange("p (k c) -> p k c", k=2), "wk")
        wv = Buf(A.bf16(2 * 512).rearrange("p (k c) -> p k c", k=2), "wv")
        self.loadw_bf16(wA.ap, wuqA_d, 3, wA.r)
        self.loadw_bf16(wB.ap, wuqB_d, 3, wB.r)
        self.loadw_bf16(wk.ap, wuk_d, 2, wk.r)
        self.loadw_bf16(wv.ap, wuv_d, 2, wv.r)
        tabC = Buf(A.f32(TB), "qtabC")
        tabS = Buf(A.f32(TB), "qtabS")
        qh = [Buf(A.bf16(nq), f"qh{i}") for i in range(2)]
        NKC = NK // 128
        vT = [Buf(A.bf16(NKC * 128).rearrange("p (c m) -> p c m", m=128), f"vT{i}") for i in range(2)]
        self.memset("pool", vT[0].ap[:, :, 64:128], 1.0, [vT[0].r])
        self.memset("pool", vT[1].ap[:, :, 0:64], 1.0, [vT[1].r])
        Pt = [Buf(A.bf16(TB), f"Pt{i}") for i in range(4)]
        t1 = Buf(A.f32(TB), "at1")
        t2 = Buf(A.f32(TB), "at2")
        rec = Buf(A.f32(TB), "rec")
        qblocks = [(i * TB, TB, True) for i in range(4)]
        if self.L0:
            qblocks.append((OWN, CTX, False))
        kblocks = [(0, CTX)] + [(CTX + i * TB, TB) for i in range(8)]
        psS = self.ps[0:4]
        psO = self.ps[4:6]
        psQ = [self.ps[6], self.ps[7], self.ps[6]]
        oi = 0
        for h in range(NH):
            par = h % 2
            q = qh[par]
            kT = self.kT[par]
            v = vT[par]
            for bq, (c0, n, lat) in enumerate(qblocks):
                pa = psQ[0]
                for k in range(3):
                    self.mm(pa.ap[0:96, 0:n], wA.ap[:, k, h * 96:(h + 1) * 96], self.qnT.ap[:, k, c0:c0 + n],
                            k == 0, k == 2, [wA.r, self.qnT.r], [pa.r])
                self.cp("act", q.ap[0:64, c0:c0 + n], pa.ap[0:64, 0:n], [pa.r], [q.r])
                if lat:
                    self.load("sp", tabC.ap[64:96, 0:n], ropeC_d[64:96, c0:c0 + n], [tabC.r])
                    self.load("sp", tabS.ap[64:96, 0:n], ropeS_d[64:96, c0:c0 + n], [tabS.r])
                    pb = psQ[1]
                    for k in range(3):
                        self.mm(pb.ap[0:96, 0:n], wB.ap[:, k, h * 96:(h + 1) * 96], self.qnT.ap[:, k, c0:c0 + n],
                                k == 0, k == 2, [wB.r, self.qnT.r], [pb.r])
                    self.tt("dve", t1.ap[64:96, 0:n], pa.ap[64:96, 0:n], tabC.ap[64:96, 0:n], ALU.mult,
                            [pa.r, tabC.r], [t1.r])
                    self.tt("dve", t2.ap[64:96, 0:n], pb.ap[64:96, 0:n], tabS.ap[64:96, 0:n], ALU.mult,
                            [pb.r, tabS.r], [t2.r])
                    self.tt("dve", q.ap[64:96, c0:c0 + n], t1.ap[64:96, 0:n], t2.ap[64:96, 0:n], ALU.add,
                            [t1.r, t2.r], [q.r])
                else:
                    self.cp("act", q.ap[64:96, c0:c0 + n], pa.ap[64:96, 0:n], [pa.r], [q.r])
            for (c0, n) in kblocks:
                pk = psQ[2]
                for k in range(2):
                    self.mm(pk.ap[0:64, 0:n], wk.ap[:, k, h * 64:(h + 1) * 64], self.ckvT.ap[:, k, c0:c0 + n],
                            k == 0, k == 1, [wk.r, self.ckvT.r], [pk.r])
                self.cp("act", kT.ap[0:64, c0:c0 + n], pk.ap[0:64, 0:n], [pk.r], [kT.r])
            voff = 0 if par == 0 else 64
            for g0 in range(0, NKC, 8):
                gn = min(8, NKC - g0)
                pv = psQ[(g0 // 8) % 2]
                for c in range(gn):
                    kc = g0 + c
                    for k in range(2):
                        self.mm(pv.ap[:, c * 64:(c + 1) * 64], self.ckvT.ap[:, k, kc * 128:(kc + 1) * 128],
                                wv.ap[:, k, h * 64:(h + 1) * 64], k == 0, k == 1, [wv.r, self.ckvT.r], [pv.r],
                                inc=(k == 1 and c == gn - 1))
                self.cp("act", v.ap[:, g0:g0 + gn, voff:voff + 64],
                        pv.ap[:, 0:gn * 64].rearrange("p (c m) -> p c m", m=64), [pv.r], [v.r])
            if h == 0:
                self.tap("qh0", q.ap[0:96, :], [q.r], BF16)
                self.tap("kh0", kT.ap[0:96, :], [kT.r], BF16)
                self.tap("vh0", v.ap, [v.r], BF16)
            for (c0, n, lat) in qblocks:
                kcs = list(range(NKC)) if lat else [0, 1]
                po = psO[oi % 2]
                oi += 1

                def S(i):
                    kc = kcs[i]
                    ps = psS[i % 4]
                    self.mm(ps.ap[:, 0:n], kT.ap[0:96, kc * 128:(kc + 1) * 128], q.ap[0:96, c0:c0 + n], True, True,
                            [kT.r, q.r], [ps.r])

                S(0)
                if len(kcs) > 1:
                    S(1)
                if len(kcs) > 2:
                    S(2)
                for i, kc in enumerate(kcs):
                    ps = psS[i % 4]
                    pt = Pt[i % 4]
                    self.act(pt.ap[:, 0:n], ps.ap[:, 0:n], AF.Exp, [ps.r], [pt.r], scale=ATT_SCALE)
                    self.mm(po.ap[:, 0:n], v.ap[:, kc, :], pt.ap[:, 0:n], i == 0, i == len(kcs) - 1,
                            [v.r, pt.r], [po.r])
                    if i + 3 < len(kcs):
                        S(i + 3)
                pair = h // 2
                if par == 0:
                    self.recip(rec.ap[0:64, 0:n], po.ap[64:128, 0:n], [po.r], [rec.r])
                    self.tt("dve", self.oT.ap[0:64, pair, c0:c0 + n], po.ap[0:64, 0:n], rec.ap[0:64, 0:n], ALU.mult,
                            [po.r, rec.r], [self.oT.r])
                else:
                    self.recip(rec.ap[64:128, 0:n], po.ap[0:64, 0:n], [po.r], [rec.r])
                    self.tt("dve", self.oT.ap[64:128, pair, c0:c0 + n], po.ap[64:128, 0:n], rec.ap[64:128, 0:n],
                            ALU.mult, [po.r, rec.r], [self.oT.r])
        self.tap("oT", self.oT.ap, [self.oT.r], BF16)
        P.barrier()
        A.release(m0)

    def in_ap(self, name):
        return self._in_aps[name]

    def finish(self):
        P = self.P
        P.barrier()
        with self.nc.Block() as block:
            P.emit(block)
        self.es.close()
        return self.nc

MAGIC = 12582912.0
TWO_PI = 2.0 * math.pi


class LayerK2(LayerK):
    def _filter_mlp(self, zf_d, ncols, h2T, h2_res, w1, w2, fcol, fb1, fb2, wres):
        A = self.A
        m0 = A.mark()
        zf = Buf(A.f32(512), "zf")
        a1 = Buf(A.f32(512), "a1")
        a2 = Buf(A.f32(512), "a2")
        h1 = Buf(A.f32(512), "h1")
        ps1, ps2 = self.ps[0], self.ps[1]
        for c0 in range(0, ncols, 512):
            n = min(512, ncols - c0)
            self.load("sp", zf.ap[0:17, 0:n], zf_d[:, c0:c0 + n], [zf.r])
            self.mm(ps1.ap[0:64, 0:n], w1[0:17, :], zf.ap[0:17, 0:n], True, True, [wres, zf.r], [ps1.r])
            for (ps, fb, dst, dres) in ((ps1, fb1, h1.ap[0:64, 0:n], h1.r), (ps2, fb2, h2T[0:64, c0:c0 + n], h2_res)):
                if ps is ps2:
                    self.mm(ps2.ap[0:64, 0:n], w2[0:64, :], h1.ap[0:64, 0:n], True, True, [wres, h1.r], [ps2.r])
                self.ts("dve", a1.ap[0:64, 0:n], ps.ap[0:64, 0:n], fcol, fb, ALU.mult, ALU.add, [ps.r, wres], [a1.r])
                self.ts("dve", a2.ap[0:64, 0:n], a1.ap[0:64, 0:n], 1.0 / TWO_PI, MAGIC, ALU.mult, ALU.add, [a1.r], [a2.r])
                self.ts("dve", a2.ap[0:64, 0:n], a2.ap[0:64, 0:n], MAGIC, TWO_PI, ALU.subtract, ALU.mult, [a2.r], [a2.r])
                self.tt("dve", a1.ap[0:64, 0:n], a1.ap[0:64, 0:n], a2.ap[0:64, 0:n], ALU.subtract, [a1.r, a2.r], [a1.r])
                self.act(dst, a1.ap[0:64, 0:n], AF.Sin, [a1.r], [dres])
        A.release(m0)

    def _cmul(self, src, t1, t2, out_r, out_i, conj, m1, m2, src_res, tab_res, out_res):
        i = self._cm_i = getattr(self, "_cm_i", 0) + 1
        m1 = m1[i % len(m1)]
        m2 = m2[i % len(m2)]
        self.tt("dve", m1.ap, src, t1, ALU.mult, src_res + tab_res, [m1.r])
        self.tt("dve", m2.ap, src, t2, ALU.mult, src_res + tab_res, [m2.r])
        if not conj:
            self.tt("pool", out_r, m1.ap[:, 0:128], m2.ap[:, 128:256], ALU.subtract, [m1.r, m2.r], out_res)
            self.tt("pool", out_i, m2.ap[:, 0:128], m1.ap[:, 128:256], ALU.add, [m1.r, m2.r], out_res)
        else:
            self.tt("pool", out_r, m1.ap[:, 0:128], m2.ap[:, 128:256], ALU.add, [m1.r, m2.r], out_res)
            self.tt("pool", out_i, m1.ap[:, 128:256], m2.ap[:, 0:128], ALU.subtract, [m1.r, m2.r], out_res)

    def load_fft_tables(self):
        A = self.A
        self.ft_res = Res("fft_tabs")
        r = self.ft_res
        d = {}
        for nm in ("CS1a", "CS1b"):
            d[nm] = self.din(nm, [128, 256])
        for nm in ("W3r", "W3i", "W3n"):
            d[nm] = self.din(nm, [128, 128])
        for nm in ("T1", "T2"):
            d[nm] = self.din(nm, [128, 256])
        for nm in ("ICr", "ICi"):
            d[nm] = self.din(nm, [128, 64])
        self.CS1a = A.bf16(256); self.CS1b = A.bf16(256)
        self.W3r = A.bf16(128); self.W3i = A.bf16(128); self.W3n = A.bf16(128)
        self.T1 = A.f32(256); self.T2 = A.f32(256)
        self.ICr = A.bf16(64); self.ICi = A.bf16(64)
        for nm in ("CS1a", "CS1b", "W3r", "W3i", "W3n", "ICr", "ICi"):
            self.P.dma("pool", getattr(self, nm), d[nm], writes=[r])
        for nm in ("T1", "T2"):
            self.P.dma("sp", getattr(self, nm), d[nm], writes=[r])

    def _fft_s1(self, lhs_list, cs_list, lhs_res, psA, m1, m2, Bt):
        r = self.ft_res
        nl = len(lhs_list)
        for i, (lh, cs) in enumerate(zip(lhs_list, cs_list)):
            self.mm(psA.ap[:, 0:256], lh, cs, i == 0, i == nl - 1, lhs_res + [r], [psA.r])
        self._cmul(psA.ap[:, 0:256], self.T1, self.T2, Bt.ap[:, 0:128], Bt.ap[:, 128:256], False, m1, m2,
                   [psA.r], [r], [Bt.r])

    def _fft_s3(self, Bt, psZ):
        r = self.ft_res
        self.mm(psZ.ap[:, 0:128], self.W3r, Bt.ap[:, 0:128], True, False, [r, Bt.r], [psZ.r])
        self.mm(psZ.ap[:, 0:128], self.W3n, Bt.ap[:, 128:256], False, True, [r, Bt.r], [psZ.r])
        self.mm(psZ.ap[:, 128:256], self.W3i, Bt.ap[:, 0:128], True, False, [r, Bt.r], [psZ.r])
        self.mm(psZ.ap[:, 128:256], self.W3r, Bt.ap[:, 128:256], False, True, [r, Bt.r], [psZ.r])

    def phase_fg(self):
        A, P = self.A, self.P
        sm = self.small_res
        zff_d = self.din("zfT_f", [17, SEQ])
        zfb_d = self.din("zfT_b", [17, SEQ])
        tf_d = self.din("t_f", [1, SEQ])
        tb_d = self.din("t_b", [1, SEQ])
        w1_d = self.din("hy_w1", [17, 64])
        w2_d = self.din("hy_w2", [64, 64])
        w3_d = self.din("hy_w3", [64, 1024])
        fq_d = self.din("hy_fq", [64, 3])
        dec_d = self.din("hy_decay", [1, 1024])
        bias_d = self.din("hy_bias", [1, 512])
        self.Kh_d = self.nc.dram_tensor(f"Kh_scr{self.layer}", [128, 128, 512], F32, kind="Internal").ap()
        self.kh_res = Res("kh")
        self.x0T = Buf(A.bf16(4 * (OWN + TB)).rearrange("p (c t) -> p c t", c=4), "x0T")
        self.ohyT = Buf(self.x0T.ap[:, :, 1:OWN + 1], "ohyT")
        self.ohyT.res = self.x0T.res
        if self.L0:
            self.x0cT = Buf(A.bf16(4 * (CTX + 2)).rearrange("p (c t) -> p c t", c=4), "x0cT")
            self.ohycT = Buf(self.x0cT.ap[:, :, 1:CTX + 1], "ohycT")
            self.ohycT.res = self.x0cT.res
        self.rL1 = A.f32(4)
        self.mark_hyw = A.mark()
        self.load_fft_tables()
        if self.L0:
            self.zcT = Buf(A.f32(4 * (CTX + 1)).rearrange("p (c t) -> p c t", c=4), "zcT")
        self.fw_res = Res("fw")
        fw = self.fw_res
        self.hw1 = A.f32(64); self.hw2 = A.f32(64); self.hw3 = A.f32(1024); self.hfq = A.f32(3)
        self.hfb = A.f32(2)
        self.negdec = A.f32(1024)
        self.hbias = A.f32(512)
        self.load("sp", self.hw1[0:17, :], w1_d, [fw])
        self.load("sp", self.hw2[0:64, :], w2_d, [fw])
        self.load("sp", self.hw3[0:64, :], w3_d, [fw])
        self.load("sp", self.hfq[0:64, :], fq_d, [fw])
        self.load("sp", self.negdec[64:65, :], dec_d, [fw])
        self.load("sp", self.hbias[0:1, :], bias_d, [fw])
        self.ts("dve", self.hfb[0:64, 0:1], self.hfq[0:64, 1:2], self.hfq[0:64, 0:1], None, ALU.mult, None, [fw], [fw])
        self.ts("dve", self.hfb[0:64, 1:2], self.hfq[0:64, 2:3], self.hfq[0:64, 0:1], None, ALU.mult, None, [fw], [fw])
        self.act(self.negdec[64:65, :], self.negdec[64:65, :], AF.Abs, [fw], [fw])
        self.ts("dve", self.negdec[64:65, :], self.negdec[64:65, :], -1.0, None, ALU.mult, None, [fw], [fw])
        m0 = A.mark()
        h2 = [Buf(A.f32(SEQ), f"h2_{i}") for i in range(2)]
        trow = [Buf(h2[i].ap, f"trow{i}") for i in range(2)]
        self.load("sp", trow[0].ap[64:65, :], tf_d, [trow[0].r])
        self.load("sp", trow[1].ap[64:65, :], tb_d, [trow[1].r])
        fcol = self.hfq[0:64, 0:1]
        for d, zd in enumerate((zff_d, zfb_d)):
            self._filter_mlp(zd, SEQ, h2[d].ap, h2[d].r, self.hw1, self.hw2, fcol, self.hfb[0:64, 0:1],
                             self.hfb[0:64, 1:2], fw)
        kf = [Buf(A.bf16(16384).rearrange("p (g n c) -> p g n c", g=128, n=32), f"kfft{i}") for i in range(2)]
        acc = Buf(A.f32(512), "l1acc")
        self.memset("dve", acc.ap, 0.0, [acc.r])
        Es = [Buf(A.f32(512), f"E{i}") for i in range(2)]
        Ab = [Buf(A.f32(512), f"Ab{i}") for i in range(2)]
        kts = [Buf(A.f32(512), f"kt{i}") for i in range(2)]
        k0 = Buf(A.f32(512), "k0")
        psKs, psEs = [self.ps[2], self.ps[4]], [self.ps[3], self.ps[5]]
        itk = 0
        for d in range(2):
            for n2 in range(32):
                E, kt, ab = Es[itk % 2], kts[itk % 2], Ab[itk % 2]
                psK, psE = psKs[itk % 2], psEs[itk % 2]
                itk += 1
                lh = h2[d].ap.rearrange("p (a b) -> p a b", b=32)[0:64, :, n2]
                self.mm(psK.ap[:, :], lh, self.hw3[0:64, d * 512:(d + 1) * 512], True, True, [h2[d].r, fw], [psK.r])
                self.mm(psE.ap[:, :], trow[d].ap.rearrange("p (a b) -> p a b", b=32)[64:65, :, n2], self.negdec[64:65, d * 512:(d + 1) * 512], True, True,
                        [trow[d].r, fw], [psE.r])
                self.act(E.ap, psE.ap, AF.Exp, [psE.r], [E.r])
                sgn = 1.0 if d == 0 else -1.0
                self.stt(kt.ap, psK.ap, sgn, E.ap, ALU.mult, ALU.mult, [psK.r, E.r], [kt.r])
                if d == 1 and n2 == 0:
                    self.memset("dve", kt.ap[0:1, :], 0.0, [kt.r])
                if d == 0 and n2 == 0:
                    self.cp("dve", k0.ap[0:1, :], kt.ap[0:1, :], [kt.r], [k0.r])
                self.act(ab.ap, kt.ap, AF.Abs, [kt.r], [ab.r])
                self.tt("pool", acc.ap, acc.ap, ab.ap, ALU.add, [ab.r], [acc.r])
                self.cp("act", kf[d].ap[:, :, n2, :], kt.ap.rearrange("p (g c) -> p g c", c=4), [kt.r], [kf[d].r])
        kt = kts[0]
        psL = self.ps[4]
        self.mm(psL.ap[:, :], self.ones_f.ap, acc.ap, True, True, [acc.r, self.ones_f.r], [psL.r])
        self.tt("dve", kt.ap[0:1, :], psL.ap[0:1, :], self.hbias[0:1, :], ALU.mult, [psL.r, fw], [kt.r])
        self.tt("dve", kt.ap[0:1, :], kt.ap[0:1, :], k0.ap[0:1, :], ALU.add, [kt.r, k0.r], [kt.r])
        self.cp("act", kf[0].ap[0:1, :, 0, :], kt.ap[0:1, :].rearrange("p (g c) -> p g c", c=4), [kt.r], [kf[0].r])
        psC = self.ps[5]
        for c in range(4):
            self.mm(psC.ap[:, c:c + 1], acc.ap[:, c * 128:(c + 1) * 128], self.ones_f.ap[:, 0:1], True, True,
                    [acc.r, self.ones_f.r], [psC.r])
        self.recip(self.rL1, psC.ap[:, 0:4], [psC.r], [sm])
        self.tap("rL1", self.rL1, [sm])
        self.tap("kf0", kf[0].ap, [kf[0].r], BF16)
        self.tap("kf1", kf[1].ap, [kf[1].r], BF16)
        m1 = [Buf(A.f32(256), f"m1_{i}") for i in range(3)]; m2 = [Buf(A.f32(256), f"m2_{i}") for i in range(3)]
        Bt = [Buf(A.bf16(256), f"Bt{i}") for i in range(2)]
        Ks = [Buf(A.f32(512), f"Ks{i}") for i in range(2)]
        psAs, psZs = [self.ps[4], self.ps[5]], [self.ps[6], self.ps[7]]
        for it in range(128 + 1):
            g = it
            if g < 128:
                self._fft_s1([kf[0].ap[:, g, :, :].rearrange("p n c -> p (n c)"),
                              kf[1].ap[:, g, :, :].rearrange("p n c -> p (n c)")],
                             [self.CS1a, self.CS1b], [kf[0].r, kf[1].r], psAs[g % 2], m1, m2, Bt[g % 2])
            g = it - 1
            if 0 <= g < 128:
                psZ = psZs[g % 2]
                self._fft_s3(Bt[g % 2], psZ)
                ks = Ks[g % 2]
                self.cp("act", ks.ap[:, 0:512].rearrange("p (a b c) -> p a b c", a=2, b=2)[:, :, 0, :],
                        psZ.ap[:, 0:256].rearrange("p (a c) -> p a c", a=2), [psZ.r], [ks.r])
                self.cp("act", ks.ap[:, 0:512].rearrange("p (a b c) -> p a b c", a=2, b=2)[:, :, 1, :],
                        psZ.ap[:, 0:256].rearrange("p (a c) -> p a c", a=2), [psZ.r], [ks.r])
                self.P.dma("sp", self.Kh_d[g], ks.ap, reads=[ks.r], writes=[self.kh_res])
        P.barrier()
        A.release(m0)

    def phase_h(self):
        A, P = self.A, self.P
        sm = self.small_res
        why_d = self.din("w_hy", [D, 1536])
        sw_d = self.din("hy_sw", [128, 12, 4])
        sw = A.f32(48).rearrange("p (g j) -> p g j", j=4)
        self.load("sp", sw, sw_d, [sm])
        if self.L0:
            swc_d = self.din("hy_swc", [128, 12, 4])
            swc = A.f32(48).rearrange("p (g j) -> p g j", j=4)
            self.load("sp", swc, swc_d, [sm])
        self.zT = Buf(A.bf16(4 * (SEQ + 1)).rearrange("p (c t) -> p c t", c=4), "zT")
        m0 = A.mark()
        w = Buf(A.bf16(8 * 1536).rearrange("p (k c) -> p k c", k=8), "why")
        self.loadw_bf16(w.ap, why_d, 8, w.r)
        xb = [Buf(A.f32(8 * TB).rearrange("p (k t) -> p k t", k=8), f"xb{i}") for i in range(1)]
        tmp = Buf(A.f32(8 * TB).rearrange("p (k t) -> p k t", k=8), "tmp")
        hTs = [Buf(A.bf16(8 * TB).rearrange("p (k t) -> p k t", k=8), f"hT{i}") for i in range(2)]
        rstd = Buf(A.f32(TB), "rstd")
        R = [Buf(A.bf16(TB + 2), f"R{g}") for g in range(12)]
        ua = [Buf(A.f32(TB), f"ua{i}") for i in range(2)]
        ub = [Buf(A.f32(TB), f"ub{i}") for i in range(2)]

        cur = {"sw": sw}

        def conv(g, n, out_ap, out_res, eng_tmp):
            r = R[g]
            sw = cur["sw"]
            self.ts("dve", eng_tmp.ap[:, 0:n], r.ap[:, 0:n], sw[:, g, 0:1], sw[:, g, 3:4], ALU.mult, ALU.add,
                    [r.r, sm], [eng_tmp.r])
            self.stt(eng_tmp.ap[:, 0:n], r.ap[:, 1:n + 1], sw[:, g, 1:2], eng_tmp.ap[:, 0:n], ALU.mult, ALU.add,
                     [r.r, sm], [eng_tmp.r])
            self.stt(out_ap, r.ap[:, 2:n + 2], sw[:, g, 2:3], eng_tmp.ap[:, 0:n], ALU.mult, ALU.add,
                     [r.r, sm, eng_tmp.r], out_res)

        def reset_R():
            for g in range(12):
                self.memset("pool", R[g].ap[:, 0:2], 0.0, [R[g].r])

        order = ([8] if self.L0 else []) + list(range(8))
        reset_R()

        def prep(it_):
            bi_ = order[it_]
            src_, n_, col_ = self.xblock_src(bi_)
            hT_ = hTs[it_ % 2]
            x_ = xb[0]
            self.load("sp", x_.ap[:, :, 0:n_], src_, [x_.r])
            self.norm_mod(x_.ap[:, :, 0:n_], n_, self.gs1, self.sh1, col_, hT_.ap[:, :, 0:n_], tmp.ap[:, :, 0:n_],
                          rstd.ap[:, 0:n_], x_.r, hT_.r, tmp.r, rstd.r, self.ps[0])

        prep(0)
        for it, bi in enumerate(order):
            src, n, col = self.xblock_src(bi)
            cur["sw"] = swc if bi == 8 else sw
            hT = hTs[it % 2]
            if it + 1 < len(order):
                prep(it + 1)
            need_x0 = (bi == 8) or (bi <= 4)
            groups = list(range(12)) if need_x0 else list(range(8))
            for g in groups:
                psb = self.ps[1 + (g % 4)]
                for k in range(8):
                    self.mm(psb.ap[:, 0:n], w.ap[:, k, g * 128:(g + 1) * 128], hT.ap[:, k, 0:n], k == 0, k == 7,
                            [w.r, hT.r], [psb.r])
                self.cp("act", R[g].ap[:, 2:n + 2], psb.ap[:, 0:n], [psb.r], [R[g].r])
            base = 0 if bi == 8 else bi * TB
            zdst = self.zcT if bi == 8 else self.zT
            x0dst = self.x0cT if bi == 8 else self.x0T
            T4 = [ua[0], ub[0], ua[1], ub[1]]

            def conv_batch(gl, outs):
                sw_ = cur["sw"]
                for i, g in enumerate(gl):
                    t_ = T4[i]
                    self.ts("dve", t_.ap[:, 0:n], R[g].ap[:, 0:n], sw_[:, g, 0:1], sw_[:, g, 3:4], ALU.mult, ALU.add,
                            [R[g].r, sm], [t_.r])
                for i, g in enumerate(gl):
                    t_ = T4[i]
                    self.stt(t_.ap[:, 0:n], R[g].ap[:, 1:n + 1], sw_[:, g, 1:2], t_.ap[:, 0:n], ALU.mult, ALU.add,
                             [R[g].r, sm], [t_.r])
                for i, g in enumerate(gl):
                    t_ = T4[i]
                    oap, ores = outs[i]
                    self.stt(oap, R[g].ap[:, 2:n + 2], sw_[:, g, 2:3], t_.ap[:, 0:n], ALU.mult, ALU.add,
                             [R[g].r, sm, t_.r], ores)

            for c2 in range(0, 4, 2):
                gl = [c2, 4 + c2, c2 + 1, 4 + c2 + 1]
                conv_batch(gl, [(T4[i].ap[:, 0:n], [T4[i].r]) for i in range(4)])
                self.tt("dve", zdst.ap[:, c2, base:base + n], T4[0].ap[:, 0:n], T4[1].ap[:, 0:n], ALU.mult,
                        [T4[0].r, T4[1].r], [zdst.r])
                self.tt("dve", zdst.ap[:, c2 + 1, base:base + n], T4[2].ap[:, 0:n], T4[3].ap[:, 0:n], ALU.mult,
                        [T4[2].r, T4[3].r], [zdst.r])
            if need_x0:
                conv_batch([8, 9, 10, 11], [(x0dst.ap[:, c, base:base + n], [x0dst.r]) for c in range(4)])
            for g in groups:
                self.cp("pool", R[g].ap[:, 0:2], R[g].ap[:, n:n + 2], [R[g].r], [R[g].r])
            last_of_seq = (bi == 8) or (bi == 7)
            if last_of_seq:
                fl = base + n
                fgroups = list(range(12)) if bi == 8 else list(range(8))
                for g in fgroups:
                    self.memset("pool", R[g].ap[:, 2:3], 0.0, [R[g].r])
                for c in range(4):
                    a, b_ = ua[c % 2], ub[c % 2]
                    conv(c, 1, a.ap[:, 0:1], [a.r], a)
                    conv(4 + c, 1, b_.ap[:, 0:1], [b_.r], b_)
                    self.tt("dve", zdst.ap[:, c, fl:fl + 1], a.ap[:, 0:1], b_.ap[:, 0:1], ALU.mult, [a.r, b_.r], [zdst.r])
                    if bi == 8:
                        conv(8 + c, 1, x0dst.ap[:, c, fl:fl + 1], [x0dst.r], a)
                reset_R()
        self.tap("zT", self.zT.ap, [self.zT.r], BF16)
        self.tap("x0T", self.x0T.ap, [self.x0T.r], BF16)
        if self.L0:
            self.tap("zcT", self.zcT.ap, [self.zcT.r])
            self.tap("x0cT", self.x0cT.ap, [self.x0cT.r], BF16)
        P.barrier()
        A.release(m0)

    def phase_f(self):
        A, P = self.A, self.P
        sm = self.small_res
        r = self.ft_res
        m0 = A.mark()
        ztm = Buf(A.bf16(16384).rearrange("p (g n c) -> p g n c", g=128, n=32), "ztm")
        zv = self.zT.ap[:, :, 1:SEQ + 1].rearrange("p c (a b) -> p c a b", b=32)
        psT = [self.ps[0], self.ps[1]]
        ti = 0
        for cc in range(4):
            for n20 in range(0, 32, 4):
                pt = psT[ti % 2]
                ti += 1
                ptb = pt.ap.bitcast(BF16)
                for j in range(4):
                    self.tr(ptb[:, j * 128:(j + 1) * 128], zv[:, cc, :, n20 + j], self.ident_b.ap,
                            [self.zT.r, self.ident_b.r], [pt.r])
                self.cp("act" if ti % 2 else "dve",
                        ztm.ap[:, cc * 32:(cc + 1) * 32, n20:n20 + 4, :].rearrange("p g n c -> p n g c"),
                        ptb[:, 0:512].rearrange("p (n g c) -> p n g c", n=4, c=4), [pt.r], [ztm.r])
        self.tap("ztm", ztm.ap, [ztm.r], BF16)
        m1 = [Buf(A.f32(256), f"m1_{i}") for i in range(3)]; m2 = [Buf(A.f32(256), f"m2_{i}") for i in range(3)]
        Bt = [Buf(A.bf16(256), f"Bt{i}") for i in range(2)]
        Ks = [Buf(A.f32(512), f"Ks{i}") for i in range(2)]
        Yt = [Buf(A.bf16(256), f"Yt{i}") for i in range(2)]
        Gp = [Buf(A.bf16(256), f"Gp{i}") for i in range(2)]
        GT = [Buf(A.bf16(1024).rearrange("p (r c) -> p r c", r=2), f"GT{i}") for i in range(2)]
        yall = Buf(A.f32(32 * 128), "yall")
        yv = yall.ap.rearrange("p (n g c) -> p n g c", n=32, g=32)
        y3 = yall.ap.rearrange("p (n m) -> p n m", n=32)
        psAs, psZs, psGs = [self.ps[0], self.ps[1]], [self.ps[2], self.ps[3]], [self.ps[4], self.ps[5]]
        psM, psYT = self.ps[6], self.ps[7]
        psGT_res, psY_res = Res("psGT"), Res("psY")
        pgb = psM.ap.bitcast(BF16)
        psY = psM.ap[0:64, 256:512]
        GT2 = [Buf(A.bf16(512).rearrange("p (r c) -> p r c", r=2), f"GTp{i}") for i in range(2)]

        def stage_a(g):
            ks = Ks[g % 2]
            self.P.dma("sp", ks.ap, self.Kh_d[g], reads=[self.kh_res], writes=[ks.r])
            self._fft_s1([ztm.ap[:, g, :, :].rearrange("p n c -> p (n c)")], [self.CS1a], [ztm.r],
                         psAs[g % 2], m1, m2, Bt[g % 2])

        def stage_b(g):
            ks = Ks[g % 2]
            psZ = psZs[g % 2]
            self._fft_s3(Bt[g % 2], psZ)
            yt = Yt[g % 2]
            self._cmul(psZ.ap[:, 0:256], ks.ap[:, 0:256], ks.ap[:, 256:512], yt.ap[:, 0:128], yt.ap[:, 128:256],
                       False, m1, m2, [psZ.r], [ks.r], [yt.r])

        def stage_c(g):
            yt = Yt[g % 2]
            psG = psGs[g % 2]
            self.mm(psG.ap[:, 0:128], self.W3r, yt.ap[:, 0:128], True, False, [r, yt.r], [psG.r])
            self.mm(psG.ap[:, 0:128], self.W3i, yt.ap[:, 128:256], False, True, [r, yt.r], [psG.r])
            self.mm(psG.ap[:, 128:256], self.W3n, yt.ap[:, 0:128], True, False, [r, yt.r], [psG.r])
            self.mm(psG.ap[:, 128:256], self.W3r, yt.ap[:, 128:256], False, True, [r, yt.r], [psG.r])
            gp = Gp[g % 2]
            self._cmul(psG.ap[:, 0:256], self.T1, self.T2, gp.ap[:, 0:128], gp.ap[:, 128:256], True, m1, m2,
                       [psG.r], [r], [gp.r])

        def stage_d(g):
            gp = Gp[g % 2]
            gt = GT2[(g // 2) % 2]
            gl = g % 2
            self.tr(pgb[:, 0:128], gp.ap[:, 0:128], self.ident_b.ap, [gp.r, self.ident_b.r], [psGT_res])
            self.tr(pgb[:, 128:256], gp.ap[:, 128:256], self.ident_b.ap, [gp.r, self.ident_b.r], [psGT_res])
            self.cp("act", gt.ap[:, :, gl * 128:(gl + 1) * 128], pgb[:, 0:256].rearrange("p (r c) -> p r c", r=2),
                    [psGT_res], [gt.r])
            if gl == 1:
                self.mm(psY, self.ICr, gt.ap[:, 0, :], True, False, [r, gt.r], [psY_res])
                self.mm(psY, self.ICi, gt.ap[:, 1, :], False, True, [r, gt.r], [psY_res])
                gq = (g // 2) % 16
                self.cp("act", yv[0:64, :, gq * 2:(gq + 1) * 2, :].rearrange("p n g c -> p g n c"),
                        psY.rearrange("p (g n c) -> p g n c", g=2, n=32), [psY_res], [yall.r])
            if g % 32 == 31:
                cc = g // 32
                if cc == 0:
                    self.tap("yall0", yall.ap[0:64, :], [yall.r])
                x0v = self.x0T.ap[:, cc, 1:OWN + 1].rearrange("p (a b) -> p a b", b=32)
                ov = self.ohyT.ap[:, cc, :].rearrange("p (a b) -> p a b", b=32)
                for n20 in range(0, 32, 8):
                    for j in range(8):
                        n2 = n20 + j
                        self.tr(psYT.ap[:, j * 64:(j + 1) * 64], y3[0:64, n2, :], self.ident_f.ap[0:64, 0:64],
                                [yall.r, self.ident_f.r], [psYT.r])
                    self.stt(ov[:, :, n20:n20 + 8].rearrange("p a b -> p b a"),
                             psYT.ap[:, 0:512].rearrange("p (b a) -> p b a", b=8), self.rL1[:, cc:cc + 1],
                             x0v[:, :, n20:n20 + 8].rearrange("p a b -> p b a"), ALU.mult, ALU.mult,
                             [psYT.r, self.x0T.r, sm], [self.ohyT.r])

        for it in range(128 + 3):
            if it < 128:
                stage_a(it)
            if 0 <= it - 1 < 128:
                stage_b(it - 1)
            if 0 <= it - 2 < 128:
                stage_c(it - 2)
            if 0 <= it - 3 < 128:
                stage_d(it - 3)
        self.tap("ohyT", self.ohyT.ap, [self.ohyT.r], BF16)
        P.barrier()
        A.release(m0)

    def phase_ctxhy(self):
        A, P = self.A, self.P
        sm = self.small_res
        fw = self.fw_res
        zfc_d = self.din("zfT_c", [17, CTX])
        tc_d = self.din("tc_bc", [128, CTX])
        w3c_d = self.din("hyc_w3", [64, 1024])
        dec_d = self.din("hyc_dec", [128, 8])
        bias_d = self.din("hyc_bias", [128, 4])
        m0 = A.mark()
        w3c = A.f32(1024)
        nd = A.f32(8)
        bc = A.f32(4)
        tcb = A.f32(CTX)
        cw = Res("ctxw")
        self.load("sp", w3c[0:64, :], w3c_d, [cw])
        self.load("sp", nd, dec_d, [cw])
        self.load("sp", bc, bias_d, [cw])
        self.load("sp", tcb, tc_d, [cw])
        self.act(nd, nd, AF.Abs, [cw], [cw])
        self.ts("dve", nd, nd, -1.0, None, ALU.mult, None, [cw], [cw])
        h2c = Buf(A.f32(CTX), "h2c")
        self._filter_mlp(zfc_d, CTX, h2c.ap, h2c.r, self.hw1, self.hw2, self.hfq[0:64, 0:1], self.hfb[0:64, 0:1],
                         self.hfb[0:64, 1:2], fw)
        kc = [Buf(A.f32(4 * CTX).rearrange("p (c t) -> p c t", c=4), f"kc{d}") for d in range(2)]
        E = Buf(A.f32(CTX), "Ec")
        l1 = Buf(A.f32(8), "l1c")
        for d in range(2):
            for c in range(4):
                ps = self.ps[(d * 4 + c) % 2]
                self.mm(ps.ap[:, 0:CTX], w3c[0:64, d * 512 + c * 128:d * 512 + (c + 1) * 128], h2c.ap[0:64, :], True, True,
                        [cw, h2c.r], [ps.r])
                j = d * 4 + c
                self.act(E.ap, tcb, AF.Exp, [cw], [E.r], scale=nd[:, j:j + 1])
                self.tt("dve", kc[d].ap[:, c, :], ps.ap[:, 0:CTX], E.ap, ALU.mult, [ps.r, E.r], [kc[d].r])
                src = kc[d].ap[:, c, :] if d == 0 else kc[d].ap[:, c, 1:CTX]
                self.P.op("dve", lambda e, o=l1.ap[:, j:j + 1], s=src: e.tensor_reduce(
                    out=o, in_=s, axis=AX.X, op=ALU.add, apply_absolute_value=True), [kc[d].r], [l1.r])
        self.tt("dve", l1.ap[:, 0:4], l1.ap[:, 0:4], l1.ap[:, 4:8], ALU.add, [l1.r], [l1.r])
        self.recip(l1.ap[:, 0:4], l1.ap[:, 0:4], [l1.r], [l1.r])
        for d in range(2):
            for c in range(4):
                self.ts("dve", kc[d].ap[:, c, :], kc[d].ap[:, c, :], l1.ap[:, c:c + 1], None, ALU.mult, None,
                        [l1.r, kc[d].r], [kc[d].r])
        for c in range(4):
            self.tt("dve", kc[0].ap[:, c, 0:1], kc[0].ap[:, c, 0:1], bc[:, c:c + 1], ALU.add, [kc[0].r, cw], [kc[0].r])
        self.tap("kc0", kc[0].ap, [kc[0].r])
        z = self.zcT
        zpad = Buf(A.bf16(4 * 3 * CTX).rearrange("p (c t) -> p c t", c=4), "zpad")
        self.memset("pool", zpad.ap, 0.0, [zpad.r])
        self.cp("act", zpad.ap[:, :, CTX:2 * CTX], z.ap[:, :, 1:CTX + 1], [z.r], [zpad.r])
        NDG = 16
        Dg = [Buf(A.bf16(128), f"Dg{i}") for i in range(NDG)]
        accs = self.ps[0:4]
        di = 0
        nl = 2 * CTX - 1
        for e_ in range(nl):
            d = e_ - (CTX - 1)
            kk = kc[0] if d >= 0 else kc[1]
            for c in range(4):
                dg = Dg[di % NDG]
                eng = "dve"
                di += 1
                self.ts(eng, dg.ap, self.ident_b.ap, kk.ap[:, c, abs(d):abs(d) + 1], None, ALU.mult, None,
                        [kk.r, self.ident_b.r], [dg.r])
                self.mm(accs[c].ap[:, 0:CTX], dg.ap, zpad.ap[:, c, CTX - d:2 * CTX - d], e_ == 0, e_ == nl - 1,
                        [dg.r, zpad.r], [accs[c].r], inc=True)
        for c in range(4):
            self.tt("dve", self.ohycT.ap[:, c, :], accs[c].ap[:, 0:CTX], self.x0cT.ap[:, c, 1:CTX + 1], ALU.mult,
                    [accs[c].r], [self.x0cT.r])
        self.tap("ohycT", self.ohycT.ap, [self.x0cT.r], BF16)
        P.barrier()
        A.release(m0)

    def phase_m(self):
        A, P = self.A, self.P
        sm = self.small_res
        wg_d = self.din("w_gate", [D, 2048])
        wba_d = self.din("w_br_attn", [512, D])
        wbh_d = self.din("w_br_hy", [512, D])
        wo_d = self.din("w_out", [D, D])
        NT = OWN + (CTX if self.L0 else 0)
        self.NT = NT
        self.xmid_d = self.nc.dram_tensor(f"xmid_scr{self.layer}", [D, NT], F32, kind="Internal").ap()
        self.xmid_v = self.xmid_d.rearrange("(k p) c -> p k c", p=128)
        self.xmid_res = Res("xmid")
        m0 = A.mark()
        wg = Buf(A.bf16(8 * 2048).rearrange("p (k c) -> p k c", k=8), "wg")
        wba = Buf(A.bf16(4 * D).rearrange("p (k c) -> p k c", k=4), "wba")
        wbh = Buf(A.bf16(4 * D).rearrange("p (k c) -> p k c", k=4), "wbh")
        wo = Buf(A.bf16(8 * D).rearrange("p (k c) -> p k c", k=8), "wo")
        self.loadw_bf16(wg.ap, wg_d, 8, wg.r)
        self.loadw_bf16(wba.ap, wba_d, 4, wba.r)
        self.loadw_bf16(wbh.ap, wbh_d, 4, wbh.r)
        self.loadw_bf16(wo.ap, wo_d, 8, wo.r)
        xb = Buf(A.f32(8 * TB).rearrange("p (k t) -> p k t", k=8), "xb")
        tmp = Buf(A.f32(8 * TB).rearrange("p (k t) -> p k t", k=8), "tmp")
        hT = Buf(A.bf16(8 * TB).rearrange("p (k t) -> p k t", k=8), "hT")
        rstd = Buf(A.f32(TB), "rstd")
        mg = Buf(A.bf16(8 * TB).rearrange("p (k t) -> p k t", k=8), "mg")
        ga = [Buf(A.f32(TB), f"ga{i}") for i in range(2)]
        gh = [Buf(A.f32(TB), f"gh{i}") for i in range(2)]
        blocks = [0, 1, 2, 3] + ([8] if self.L0 else [])
        for bi in blocks:
            src, n, col = self.xblock_src(bi)
            c0 = bi * TB if bi < 8 else OWN
            self.load("sp", xb.ap[:, :, 0:n], src, [xb.r])
            self.norm_mod(xb.ap[:, :, 0:n], n, self.gs1, self.sh1, col, hT.ap[:, :, 0:n], tmp.ap[:, :, 0:n],
                          rstd.ap[:, 0:n], xb.r, hT.r, tmp.r, rstd.r, self.ps[0])
            oa = self.oT.ap[:, :, c0:c0 + n]
            if bi < 8:
                oh = self.ohyT.ap[:, :, c0:c0 + n]
                oh_res = self.ohyT.r
            else:
                oh = self.ohycT.ap[:, :, 0:n]
                oh_res = self.ohycT.r
            for c in range(8):
                pga, pgh, pba, pbh = self.ps[(c % 2) * 4:(c % 2) * 4 + 4]
                for k in range(8):
                    self.mm(pga.ap[:, 0:n], wg.ap[:, k, c * 128:(c + 1) * 128], hT.ap[:, k, 0:n], k == 0, k == 7,
                            [wg.r, hT.r], [pga.r])
                for k in range(8):
                    self.mm(pgh.ap[:, 0:n], wg.ap[:, k, 1024 + c * 128:1024 + (c + 1) * 128], hT.ap[:, k, 0:n],
                            k == 0, k == 7, [wg.r, hT.r], [pgh.r])
                for k in range(4):
                    self.mm(pba.ap[:, 0:n], wba.ap[:, k, c * 128:(c + 1) * 128], oa[:, k, :], k == 0, k == 3,
                            [wba.r, self.oT.r], [pba.r])
                for k in range(4):
                    self.mm(pbh.ap[:, 0:n], wbh.ap[:, k, c * 128:(c + 1) * 128], oh[:, k, :], k == 0, k == 3,
                            [wbh.r, oh_res], [pbh.r])
                a_, h_ = ga[c % 2], gh[c % 2]
                self.act(a_.ap[:, 0:n], pga.ap[:, 0:n], AF.Sigmoid, [pga.r], [a_.r])
                self.act(h_.ap[:, 0:n], pgh.ap[:, 0:n], AF.Sigmoid, [pgh.r], [h_.r])
                self.tt("dve", a_.ap[:, 0:n], a_.ap[:, 0:n], pba.ap[:, 0:n], ALU.mult, [a_.r, pba.r], [a_.r])
                self.tt("dve", h_.ap[:, 0:n], h_.ap[:, 0:n], pbh.ap[:, 0:n], ALU.mult, [h_.r, pbh.r], [h_.r])
                self.tt("dve", mg.ap[:, c, 0:n], a_.ap[:, 0:n], h_.ap[:, 0:n], ALU.add, [a_.r, h_.r], [mg.r])
            if bi == 0:
                self.tap("mg0", mg.ap, [mg.r], BF16)
            for c in range(8):
                po = self.ps[c % 2]
                for k in range(8):
                    self.mm(po.ap[:, 0:n], wo.ap[:, k, c * 128:(c + 1) * 128], mg.ap[:, k, 0:n], k == 0, k == 7,
                            [wo.r, mg.r], [po.r])
                self.stt(xb.ap[:, c, 0:n], po.ap[:, 0:n], self.g1[:, c, col:col + 1], xb.ap[:, c, 0:n],
                         ALU.mult, ALU.add, [po.r, sm], [xb.r])
            self.P.dma("sp", self.xmid_v[:, :, c0:c0 + n], xb.ap[:, :, 0:n], reads=[xb.r], writes=[self.xmid_res])
        P.barrier()
        A.release(m0)

    def phase_ffn(self):
        A, P = self.A, self.P
        sm = self.small_res
        moe = not self.L0
        NT = self.NT
        nexp = NE if moe else 1
        if moe:
            wgd = self.din("moe_w_gate", [NE, D, DFF])
            wud = self.din("moe_w_up", [NE, D, DFF])
            wdd = self.din("moe_w_down", [NE, DFF, D])
            rt_d = self.din("moe_router", [128, 8, NE])
            fg_d = self.din("final_g", [128, 8])
        else:
            wgd = self.din("ffn_w_gate", [1, D, DFF])
            wud = self.din("ffn_w_up", [1, D, DFF])
            wdd = self.din("ffn_w_down", [1, DFF, D])
        fused0 = self.L0 and getattr(self, "fused", False)
        if not fused0:
            xout_d = self.dout("xout", [D, OWN])
            xout_v = xout_d.rearrange("(k p) c -> p k c", p=128)
        if self.L0 and not fused0:
            cout_d = self.dout("cout", [D, CTX])
            cout_v = cout_d.rearrange("(k p) c -> p k c", p=128)
        xT = Buf(A.f32(8 * NT).rearrange("p (k t) -> p k t", k=8), "xT", nres=5)
        h2T = Buf(A.bf16(8 * NT).rearrange("p (k t) -> p k t", k=8), "h2T", nres=5)
        blocks = [(i * TB, TB, 0) for i in range(4)] + ([(OWN, CTX, 1)] if self.L0 else [])
        if moe:
            cb = Buf(A.bf16(NE * OWN).rearrange("p (e t) -> p e t", e=NE), "cb", nres=4)
            rt = A.f32(8 * NE).rearrange("p (k e) -> p k e", k=8)
            fgc = A.f32(8)
            self.load("sp", rt, rt_d, [sm])
            self.load("sp", fgc, fg_d, [sm])
        m0 = A.mark()
        tmp = Buf(A.f32(8 * TB).rearrange("p (k t) -> p k t", k=8), "tmp")
        rstd = Buf(A.f32(TB), "rstd")
        if moe:
            hf = tmp
            lg = Buf(A.f32(8), "lg"); m8 = Buf(A.f32(8), "m8"); wv = Buf(A.f32(8), "wv"); nv1 = Buf(A.f32(2), "nv1")
            dg = Buf(A.f32(128), "dg")
        for bidx, (c0, n, col) in enumerate(blocks):
            self.load("sp", xT.ap[:, :, c0:c0 + n], self.xmid_v[:, :, c0:c0 + n], [xT.res[bidx]])
            self.norm_mod(xT.ap[:, :, c0:c0 + n], n, self.gs2, self.sh2, col, h2T.ap[:, :, c0:c0 + n], tmp.ap[:, :, 0:n],
                          rstd.ap[:, 0:n], xT.res[bidx], h2T.res[bidx], tmp.r, rstd.r, self.ps[0],
                          hf3=(hf.ap[:, :, 0:n] if moe else None), hf_res=(hf.r if moe else None))
            if bidx == 0:
                self.tap("h2T0", h2T.ap[:, :, 0:TB], [h2T.res[0]], BF16)
            if moe:
                for t in range(n // 128):
                    pl = self.ps[1 + (t % 2)]
                    for k in range(8):
                        self.mm(pl.ap[:, 0:NE], hf.ap[:, k, t * 128:(t + 1) * 128], rt[:, k, :], k == 0, k == 7,
                                [hf.r, sm], [pl.r])
                    self.cp("act", lg.ap, pl.ap[:, 0:NE], [pl.r], [lg.r])
                    self.P.op("dve", lambda e, o=m8.ap, i=lg.ap: e.max(out=o, in_=i), [lg.r], [m8.r])
                    self.ts("dve", wv.ap, lg.ap, m8.ap[:, 1:2], None, ALU.is_ge, None, [lg.r, m8.r], [wv.r])
                    self.ts("dve", nv1.ap[:, 0:1], m8.ap[:, 0:1], -1.0, None, ALU.mult, None, [m8.r], [nv1.r])
                    self.act(lg.ap, lg.ap, AF.Exp, [lg.r, nv1.r], [lg.r], bias=nv1.ap[:, 0:1])
                    self.tt("dve", wv.ap, wv.ap, lg.ap, ALU.mult, [wv.r, lg.r], [wv.r])
                    self.P.op("dve", lambda e, o=nv1.ap[:, 1:2], i=wv.ap: e.tensor_reduce(out=o, in_=i, axis=AX.X, op=ALU.add),
                              [wv.r], [nv1.r])
                    self.recip(nv1.ap[:, 1:2], nv1.ap[:, 1:2], [nv1.r], [nv1.r])
                    self.ts("dve", wv.ap, wv.ap, nv1.ap[:, 1:2], None, ALU.mult, None, [wv.r, nv1.r], [wv.r])
                    if bidx == 0 and t == 0:
                        self.tap("comb0", wv.ap, [wv.r])
                    for e_ in range(NE):
                        self.ts("dve", dg.ap, self.ident_f.ap, wv.ap[:, e_:e_ + 1], None, ALU.mult, None,
                                [wv.r, self.ident_f.r], [dg.r])
                        pc = self.ps[3 + (e_ % 4)]
                        self.mm(pc.ap[:, 0:128], self.ones_f.ap, dg.ap, True, True, [dg.r, self.ones_f.r], [pc.r])
                        self.cp("act", cb.ap[:, e_, c0 + t * 128:c0 + (t + 1) * 128], pc.ap[:, 0:128], [pc.r],
                                [cb.res[bidx]])
        P.barrier()
        A.release(m0)
        NG = DFF // 256
        W = []
        for i in range(2):
            W.append((Buf(A.bf16(8 * 256).rearrange("p (k c) -> p k c", k=8), f"wg{i}"),
                      Buf(A.bf16(8 * 256).rearrange("p (k c) -> p k c", k=8), f"wu{i}"),
                      Buf(A.bf16(2 * D).rearrange("p (k c) -> p k c", k=2), f"wd{i}")))
        sg = [Buf(A.bf16(TB), f"sg{i}") for i in range(2)]
        hid = [Buf(A.bf16(2 * TB).rearrange("p (j t) -> p j t", j=2), f"hid{i}") for i in range(2)]
        gi = 0
        for e_ in range(nexp):
            for g in range(NG):
                wg_, wu_, wd_ = W[gi % 2]
                f0 = g * 256
                for k in range(8):
                    self.P.dma("pool", wg_.ap[:, k, :], wgd[e_, k * 128:(k + 1) * 128, f0:f0 + 256], writes=[wg_.r])
                    self.P.dma("pool", wu_.ap[:, k, :], wud[e_, k * 128:(k + 1) * 128, f0:f0 + 256], writes=[wu_.r])
                for j in range(2):
                    self.P.dma("pool", wd_.ap[:, j, :], wdd[e_, f0 + j * 128:f0 + (j + 1) * 128, :], writes=[wd_.r])
                for bidx, (c0, n, col) in enumerate(blocks):
                    hd = hid[bidx % 2]
                    for j in range(2):
                        pg, pu = self.ps[2 * j], self.ps[2 * j + 1]
                        for k in range(8):
                            self.mm(pg.ap[:, 0:n], wg_.ap[:, k, j * 128:(j + 1) * 128], h2T.ap[:, k, c0:c0 + n],
                                    k == 0, k == 7, [wg_.r, h2T.res[bidx]], [pg.r])
                        for k in range(8):
                            self.mm(pu.ap[:, 0:n], wu_.ap[:, k, j * 128:(j + 1) * 128], h2T.ap[:, k, c0:c0 + n],
                                    k == 0, k == 7, [wu_.r, h2T.res[bidx]], [pu.r])
                        s_ = sg[j]
                        self.act(s_.ap[:, 0:n], pg.ap[:, 0:n], AF.Silu, [pg.r], [s_.r])
                        if moe:
                            self.tt("dve", s_.ap[:, 0:n], s_.ap[:, 0:n], cb.ap[:, e_, c0:c0 + n], ALU.mult,
                                    [s_.r, cb.res[bidx]], [s_.r])
                        self.tt("dve", hd.ap[:, j, 0:n], s_.ap[:, 0:n], pu.ap[:, 0:n], ALU.mult, [s_.r, pu.r], [hd.r])
                    for dc in range(8):
                        py = self.ps[4 + (dc % 4)]
                        for j in range(2):
                            self.mm(py.ap[:, 0:n], wd_.ap[:, j, dc * 128:(dc + 1) * 128], hd.ap[:, j, 0:n],
                                    j == 0, j == 1, [wd_.r, hd.r], [py.r])
                        self.stt(xT.ap[:, dc, c0:c0 + n], py.ap[:, 0:n], self.g2[:, dc, col:col + 1],
                                 xT.ap[:, dc, c0:c0 + n], ALU.mult, ALU.add, [py.r, sm], [xT.res[bidx]])
                gi += 1
        self.out_res = Res("out")
        self.out_res2 = Res("out2")
        if self.L0 and getattr(self, "fused", False):
            P.barrier()
            A.release(m0)
            bv = self.bounce_d.rearrange("(k p) c -> p k c", p=128)
            for bidx, (c0, n, col) in enumerate(blocks):
                if col == 0:
                    self.P.dma("sp", self.x1T_v[:, :, c0:c0 + n], xT.ap[:, :, c0:c0 + n], reads=[xT.res[bidx]],
                               writes=[self.out_res])
                    self.P.dma("act", bv[:, :, c0:c0 + n], xT.ap[:, :, c0:c0 + n], reads=[xT.res[bidx]],
                               writes=[self.out_res2])
                else:
                    self.P.dma("sp", self.c1T_v, xT.ap[:, :, c0:c0 + n], reads=[xT.res[bidx]], writes=[self.out_res])
        elif self.L0:
            for bidx, (c0, n, col) in enumerate(blocks):
                if col == 0:
                    self.P.dma("sp", xout_v[:, :, c0:c0 + n], xT.ap[:, :, c0:c0 + n], reads=[xT.res[bidx]], writes=[self.out_res])
                else:
                    self.P.dma("sp", cout_v, xT.ap[:, :, c0:c0 + n], reads=[xT.res[bidx]], writes=[self.out_res])
        else:
            P.barrier()
            A.release(m0)
            tmp = Buf(A.f32(8 * TB).rearrange("p (k t) -> p k t", k=8), "ftmp")
            rstd = Buf(A.f32(TB), "frstd")
            for bidx, (c0, n, col) in enumerate(blocks):
                self.rms_bcast(xT.ap[:, :, c0:c0 + n], 8, n, D, rstd.ap[:, 0:n], tmp.ap[:, :, 0:n], xT.res[bidx], rstd.r,
                               tmp.r, self.ps[0])
                for k in range(8):
                    self.stt(tmp.ap[:, k, 0:n], xT.ap[:, k, c0:c0 + n], fgc[:, k:k + 1], rstd.ap[:, 0:n], ALU.mult, ALU.mult,
                             [xT.res[bidx], rstd.r, sm], [tmp.r])
                self.P.dma("sp", xout_v[:, :, c0:c0 + n], tmp.ap[:, :, 0:n], reads=[tmp.r], writes=[self.out_res])
        P.barrier()


def build_layer(layer, taps=()):
    K = LayerK2(layer, taps=taps)
    K.setup_consts()
    K.phase_mod()
    K.xT_d = K.din("xT", [D, SEQ])
    K.cxT_d = K.din("cxT", [D, CTX])
    K.xT_v = K.xT_d.rearrange("(k p) c -> p k c", p=128)
    K.cxT_v = K.cxT_d.rearrange("(k p) c -> p k c", p=128)
    K.phase_fg()
    K.phase_h()
    K.phase_f()
    if K.L0:
        K.phase_ctxhy()
    K.A.release(K.mark_hyw)
    K.phase_kvq()
    K.phase_att()
    K.A.release(K.mark_att)
    K.phase_m()
    K.A.release(K.mark_ffn)
    K.phase_ffn()
    nc = K.finish()
    return K, nc


class FusedK(LayerK2):
    def __init__(self, taps=()):
        super().__init__(0, taps=taps)
        self.fused = True

    def set_layer(self, l):
        self.layer = l
        self.L0 = l == 0
        self.last = l == 1

    def setup_exchange(self):
        nc = self.nc
        A = self.A
        self.bounce_d = nc.dram_tensor("xbounce", [D, OWN], F32, kind="Internal").ap()
        self.gath_d = nc.dram_tensor("xgath", [D, OWN], F32, kind="Internal").ap()
        self.x1T_d = nc.dram_tensor("x1T_scr", [D, SEQ], F32, kind="Internal").ap()
        self.c1T_d = nc.dram_tensor("c1T_scr", [D, CTX], F32, kind="Internal").ap()
        self.x1T_v = self.x1T_d.rearrange("(k p) c -> p k c", p=128)
        self.c1T_v = self.c1T_d.rearrange("(k p) c -> p k c", p=128)
        sm_d = self.din("smask", [128, 2])
        self.smask = A.f32(2)
        self.load("sp", self.smask, sm_d, [self.small_res_c])
        self.J = Buf(A.f32(128), "J")
        self.memset("pool", self.J.ap, 0.0, [self.J.r])
        self.P.op("pool", lambda e: e.affine_select(out=self.J.ap, in_=self.J.ap, pattern=[[1, 128]], base=-127,
                                                    channel_multiplier=1, compare_op=ALU.not_equal, fill=1.0),
                  [self.J.r], [self.J.r])

    def exchange_start(self):
        P = self.P
        self.gr = []
        nchunk = 4
        rr = D // nchunk
        self.gr = {}
        for i in range(nchunk):
            g = Res(f"gath{i}")
            self.gr[i] = g
            P.cc(lambda e, i=i: e.collective_compute(
                "AllReduce", ALU.add, replica_groups=[[0, 1], [2, 3], [4, 5], [6, 7]],
                ins=[self.bounce_d[i * rr:(i + 1) * rr, :].opt()], outs=[self.gath_d[i * rr:(i + 1) * rr, :].opt()]),
                reads=[self.out_res2], writes=[g])

    def exchange(self):
        A, P = self.A, self.P
        m0 = A.mark()
        NB_ = 4
        a = [Buf(A.f32(TB), f"xa{i}") for i in range(NB_)]
        b = [Buf(A.f32(TB), f"xb{i}") for i in range(NB_)]
        Tt = [Buf(A.f32(TB), f"xT{i}") for i in range(NB_)]
        o = [Buf(A.f32(TB), f"xo{i}") for i in range(NB_)]
        it = 0
        for k in range(8):
            for jb in range(4):
                i2 = it % NB_
                it += 1
                self.P.dma("sp", a[i2].ap, self.gath_d[k * 128:(k + 1) * 128, jb * TB:(jb + 1) * TB],
                           reads=[self.gr[k // 2]], writes=[a[i2].r])
                self.P.dma("act", b[i2].ap, self.x1T_d[k * 128:(k + 1) * 128, jb * TB:(jb + 1) * TB], writes=[b[i2].r])
                self.tt("dve", a[i2].ap, a[i2].ap, b[i2].ap, ALU.subtract, [b[i2].r], [a[i2].r])
                pt, po = self.ps[2 * i2], self.ps[2 * i2 + 1]
                for q in range(4):
                    self.tr(pt.ap[:, q * 128:(q + 1) * 128], a[i2].ap[:, q * 128:(q + 1) * 128], self.ident_f.ap,
                            [a[i2].r, self.ident_f.r], [pt.r])
                self.cp("act", Tt[i2].ap, pt.ap, [pt.r], [Tt[i2].r])
                for q in range(4):
                    self.mm(po.ap[:, (3 - q) * 128:(4 - q) * 128], Tt[i2].ap[:, q * 128:(q + 1) * 128], self.J.ap,
                            True, True, [Tt[i2].r, self.J.r], [po.r], inc=(q == 3))
                self.cp("dve", o[i2].ap, po.ap, [po.r], [o[i2].r])
                self.P.dma("sp", self.x1T_d[k * 128:(k + 1) * 128, OWN + (3 - jb) * TB:OWN + (4 - jb) * TB], o[i2].ap,
                           reads=[o[i2].r], writes=[Res("x1w")])
        P.barrier(with_cc=True)
        A.release(m0)


def build_fused(taps=()):
    K = FusedK(taps=taps)
    K.setup_consts()
    K.small_res_c = Res("smallc")
    K.setup_exchange()
    mark_base = K.A.mark()
    for l in range(2):
        K.set_layer(l)
        K.A.release(mark_base)
        K.phase_mod()
        if l == 0:
            K.xT_d = K.din("xT", [D, SEQ])
            K.cxT_d = K.din("cxT", [D, CTX])
            K.xT_v = K.xT_d.rearrange("(k p) c -> p k c", p=128)
            K.cxT_v = K.cxT_d.rearrange("(k p) c -> p k c", p=128)
        else:
            K.xT_v = K.x1T_v
            K.cxT_v = K.c1T_v
        K.phase_fg()
        if l == 1:
            K.exchange()
        K.phase_h()
        K.phase_f()
        if K.L0:
            K.phase_ctxhy()
        K.A.release(K.mark_hyw)
        K.phase_kvq()
        K.phase_att()
        K.A.release(K.mark_att)
        K.phase_m()
        K.A.release(K.mark_ffn)
        K.phase_ffn()
        if l == 0:
            K.exchange_start()
            K.P.barrier(with_cc=True)
    nc = K.finish()
    return K, nc

GRID_W = 64
KV_START = 384
HY_START = 384 + 256 + 32
GATE_START = HY_START + 1536


def chunked(vec, nchunk):
    return np.ascontiguousarray(vec.reshape(nchunk, 128).T)


def rope_tables(hf):
    n_freq = 8
    inv_freq = (10000.0 ** (-np.arange(n_freq, dtype=np.float32) / n_freq)).astype(np.float32)
    t = np.arange(SEQ)
    pos = t if hf == 0 else (SEQ - 1 - t)
    r = (pos // GRID_W).astype(np.float32)
    c = (pos % GRID_W).astype(np.float32)
    ang = np.concatenate([r[:, None] * inv_freq, c[:, None] * inv_freq], axis=-1).astype(np.float32)
    cos = np.cos(ang).astype(np.float32).T
    sin = np.sin(ang).astype(np.float32).T
    C = np.zeros((128, SEQ), np.float32)
    S = np.zeros((128, SEQ), np.float32)
    C[64:80] = cos; C[80:96] = cos
    S[64:80] = -sin; S[80:96] = sin
    return C, S


def layer_inputs(inp, l, b, hf, xT_b, cxT_b, names):
    out = {}
    f = np.float32
    w_in = inp["w_in"][l]

    def need(n):
        return n in names

    if need("xT"):
        out["xT"] = np.ascontiguousarray(xT_b if hf == 0 else xT_b[:, ::-1])
    if need("cxT"):
        out["cxT"] = np.ascontiguousarray(cxT_b)
    if need("cond"):
        out["cond"] = np.ascontiguousarray(np.stack([chunked(inp["c"][b], 8), chunked(inp["c_ctx"], 8)], axis=-1))
    if need("w_mod"):
        out["w_mod"] = np.ascontiguousarray(inp["w_mod"][l])
    if need("b_mod2"):
        bm = chunked(inp["b_mod"][l], 48)
        out["b_mod2"] = np.ascontiguousarray(np.stack([bm, bm], axis=-1))
    for nm, key in (("n1g2", "norm1_g"), ("n2g2", "norm2_g")):
        if need(nm):
            g = chunked(inp[key][l], 8)
            out[nm] = np.ascontiguousarray(np.stack([g, g], axis=-1))
    if need("w_kvq"):
        w = np.zeros((D, 7 * 128), f)
        w[:, 0:256] = w_in[:, KV_START:KV_START + 256]
        ra = w_in[:, KV_START + 256:KV_START + 272]
        rb = w_in[:, KV_START + 272:KV_START + 288]
        w[:, 256 + 64:256 + 80] = ra; w[:, 256 + 80:256 + 96] = rb
        w[:, 384 + 64:384 + 80] = rb; w[:, 384 + 80:384 + 96] = ra
        w[:, 512:896] = w_in[:, 0:384]
        out["w_kvq"] = w
    if need("g_kv"):
        out["g_kv"] = chunked(inp["kv_norm_g"][l], 2)
    if need("g_q"):
        out["g_q"] = chunked(inp["q_norm_g"][l], 3)
    if need("ropeC") or need("ropeS"):
        C, S = rope_tables(hf)
        out["ropeC"] = C; out["ropeS"] = S
    if need("w_uqA"):
        wq = inp["w_uq"][l].reshape(QL, NH, 96)
        A = wq.copy()
        B = np.zeros_like(wq)
        B[:, :, 64:80] = wq[:, :, 80:96]
        B[:, :, 80:96] = wq[:, :, 64:80]
        out["w_uqA"] = np.ascontiguousarray(A.reshape(QL, NH * 96))
        out["w_uqB"] = np.ascontiguousarray(B.reshape(QL, NH * 96))
    if need("w_uk"):
        out["w_uk"] = np.ascontiguousarray(inp["w_uk"][l])
    if need("w_uv"):
        out["w_uv"] = np.ascontiguousarray(inp["w_uv"][l])
    return out


def zfeat(n):
    bands = 8
    t = np.linspace(0.0, 1.0, n, dtype=np.float32)[:, None]
    phase = (np.float32(2.0 * math.pi / n) * np.arange(n, dtype=np.float32)[:, None]
             * np.linspace(1e-4, bands - 1, bands, dtype=np.float32)).astype(np.float32)
    z = np.concatenate([t, np.cos(phase), -np.sin(phase)], -1).astype(np.float32)
    return z, t[:, 0]


_FFT = {}


def fft_tables():
    if _FFT:
        return _FFT
    N = 8192
    n1 = np.arange(128)[:, None].astype(np.float64)
    f1 = (np.arange(128)[None, :] + 0.5)
    th = 2 * np.pi * n1 * f1 / 256
    _FFT["CS1a"] = np.concatenate([np.cos(th), -np.sin(th)], 1).astype(np.float32)
    th = 2 * np.pi * (n1 + 128) * f1 / 256
    _FFT["CS1b"] = np.concatenate([np.cos(th), -np.sin(th)], 1).astype(np.float32)
    p = np.arange(128)
    n2 = (p // 4)[:, None].astype(np.float64)
    th = 2 * np.pi * n2 * f1 / N
    Tr, Ti = np.cos(th), -np.sin(th)
    _FFT["T1"] = np.concatenate([Tr, Tr], 1).astype(np.float32)
    _FFT["T2"] = np.concatenate([Ti, Ti], 1).astype(np.float32)
    a = (p // 4); c = p % 4
    same = (c[:, None] == c[None, :]).astype(np.float64)
    th = 2 * np.pi * a[:, None] * a[None, :] / 32
    _FFT["W3r"] = (same * np.cos(th)).astype(np.float32)
    _FFT["W3i"] = (same * -np.sin(th)).astype(np.float32)
    _FFT["W3n"] = (same * np.sin(th)).astype(np.float32)
    n1p = np.arange(64)[None, :].astype(np.float64)
    f1c = (np.arange(128)[:, None] + 0.5)
    th = 2 * np.pi * n1p * f1c / 256
    _FFT["ICr"] = (2.0 / N * np.cos(th)).astype(np.float32)
    _FFT["ICi"] = (-2.0 / N * np.sin(th)).astype(np.float32)
    return _FFT


def hyena_inputs(inp, l, hf, names):
    out = {}
    f = np.float32
    w_in = inp["w_in"][l]
    if "zfT_f" in names:
        z, t = zfeat(SEQ)
        out["zfT_f"] = np.ascontiguousarray(z.T)
        idx = (SEQ - np.arange(SEQ)) % SEQ
        out["zfT_b"] = np.ascontiguousarray(z[idx].T)
        out["t_f"] = np.ascontiguousarray(t[None, :])
        out["t_b"] = np.ascontiguousarray(t[idx][None, :])
    if "hy_w1" in names:
        out["hy_w1"] = np.ascontiguousarray(inp["hy_w1"][l])
        out["hy_w2"] = np.ascontiguousarray(inp["hy_w2"][l])
        w3 = inp["hy_w3"][l].reshape(64, 2, HYW)
        dec = inp["hy_decay"][l]
        if hf == 1:
            w3 = w3[:, ::-1, :]
            dec = dec[::-1]
        out["hy_w3"] = np.ascontiguousarray(w3.reshape(64, 1024))
        out["hy_decay"] = np.ascontiguousarray(dec.reshape(1, 1024))
        out["hy_fq"] = np.ascontiguousarray(np.stack([inp["hy_freq"][l], inp["hy_b1"][l], inp["hy_b2"][l]], -1))
        out["hy_bias"] = np.ascontiguousarray(inp["hy_bias"][l][None, :])
    if "w_hy" in names:
        out["w_hy"] = np.ascontiguousarray(w_in[:, HY_START:GATE_START])
        sw = inp["hy_short_w"][l]
        if hf == 1:
            sw = sw[::-1]
        sb = inp["hy_short_b"][l]
        arr = np.concatenate([sw, sb[None]], 0)
        out["hy_sw"] = np.ascontiguousarray(arr.reshape(4, 12, 128).transpose(2, 1, 0))
    T = fft_tables()
    for k in T:
        if k in names:
            out[k] = T[k]
    return out


def rest_inputs(inp, l, hf, names):
    out = {}
    w_in = inp["w_in"][l]
    if "zfT_c" in names:
        z, t = zfeat(CTX)
        out["zfT_c"] = np.ascontiguousarray(z.T)
        out["tc_bc"] = np.ascontiguousarray(np.broadcast_to(t[None, :], (128, CTX)))
        out["hyc_w3"] = np.ascontiguousarray(inp["hy_w3"][l])
        dec = inp["hy_decay"][l]
        out["hyc_dec"] = np.ascontiguousarray(dec.reshape(2, 4, 128).transpose(2, 0, 1).reshape(128, 8))
        out["hyc_bias"] = chunked(inp["hy_bias"][l], 4)
    if "hy_swc" in names:
        sw = inp["hy_short_w"][l]
        sb = inp["hy_short_b"][l]
        arr = np.concatenate([sw, sb[None]], 0)
        out["hy_swc"] = np.ascontiguousarray(arr.reshape(4, 12, 128).transpose(2, 1, 0))
    if "w_gate" in names:
        out["w_gate"] = np.ascontiguousarray(w_in[:, GATE_START:])
        out["w_br_attn"] = np.ascontiguousarray(inp["w_br_attn"][l])
        out["w_br_hy"] = np.ascontiguousarray(inp["w_br_hy"][l])
        out["w_out"] = np.ascontiguousarray(inp["w_out"][l])
    i = l // 2
    if "ffn_w_gate" in names:
        out["ffn_w_gate"] = inp["ffn_w_gate"][i:i + 1]
        out["ffn_w_up"] = inp["ffn_w_up"][i:i + 1]
        out["ffn_w_down"] = inp["ffn_w_down"][i:i + 1]
    if "moe_w_gate" in names:
        out["moe_w_gate"] = inp["moe_w_gate"][i]
        out["moe_w_up"] = inp["moe_w_up"][i]
        out["moe_w_down"] = inp["moe_w_down"][i]
        out["moe_router"] = np.ascontiguousarray(inp["moe_router"][i].reshape(8, 128, NE).transpose(1, 0, 2))
        out["final_g"] = chunked(inp["final_g"], 8)
    return out


def all_inputs(inp, l, b, hf, xT_b, cxT_b, names):
    im = layer_inputs(inp, l, b, hf, xT_b, cxT_b, names)
    im.update(hyena_inputs(inp, l, hf, names))
    im.update(rest_inputs(inp, l, hf, names))
    return im


_FCACHE = {}


def _get_fused():
    if "k" not in _FCACHE:
        _FCACHE["k"] = build_fused()
    return _FCACHE["k"]


def kernel(**inputs):
    inp = {k: np.asarray(v, dtype=np.float32) for k, v in inputs.items()}
    B = inp["x"].shape[0]
    K, nc = _get_fused()
    all_names = set(K.in_shapes)
    shared = {n for n in all_names if not (n.startswith("L0_") or n.startswith("L1_"))}
    lnames = {l: {n[3:] for n in all_names if n.startswith(f"L{l}_")} for l in range(2)}
    xT = [np.ascontiguousarray(inp["x"][b].T) for b in range(B)]
    cxT = [np.ascontiguousarray(inp["ctx"][b].T) for b in range(B)]
    in_maps = []
    for core in range(8):
        b, hf = core // 2, core % 2
        m = {}
        im = all_inputs(inp, 0, b, hf, xT[b], cxT[b], shared | lnames[0])
        for n in shared:
            if n == "smask":
                sm = np.zeros((128, 2), np.float32)
                sm[:, hf] = 1.0
                m[n] = sm
            else:
                m[n] = im[n]
        for n in lnames[0]:
            m["L0_" + n] = im[n]
        im1 = all_inputs(inp, 1, b, hf, xT[b], cxT[b], lnames[1])
        for n in lnames[1]:
            m["L1_" + n] = im1[n]
        for k, s in K.in_shapes.items():
            assert m[k].shape == s, (k, m[k].shape, s)
            m[k] = np.ascontiguousarray(m[k], dtype=np.float32)
        in_maps.append(m)
    res = run_bass_kernel_spmd(nc, in_maps, core_ids=list(range(8)))
    r1 = res.results
    out = np.empty((B, SEQ, D), np.float32)
    for b in range(B):
        lo = np.asarray(r1[2 * b]["xout"])
        hi = np.asarray(r1[2 * b + 1]["xout"])[:, ::-1]
        out[b] = np.concatenate([lo, hi], axis=1).T
    return out
```
